# Optimizing a Trainium2 kernel written in Bass

```python
import math
import jax, jax.numpy as jnp
from jax import lax
import numpy as np

D_MODEL = 1024
BATCH = 8
SEQ = 4096
DEPTH = 4

D_MIX = D_MODEL
M_HEADS = 4
M_DH = D_MODEL // 16
M_W = M_HEADS * M_DH
CONV_W = 4
CHUNK = 64
DF_HEADS = 4
DF_DQK = D_MODEL // 16
DF_DV = 2 * DF_DQK
DF_QK_W = DF_HEADS * 2 * DF_DQK
DF_W = DF_HEADS * DF_DV
H_HEADS = 4
H_DK = D_MODEL // 16
H_DV = D_MODEL // 16
H_W = H_HEADS * H_DV
D_FF = 2816
REL_BUCKETS = 32
REL_MAX_EXACT = 16
REL_MAX_DIST = 128
Q_BLOCK = 128
EPS = 1e-6
SPLIT_SIZES = (2 * M_W, M_W, M_W, M_HEADS, M_HEADS,
               DF_QK_W, DF_QK_W, DF_W,
               H_W, H_W, H_W, H_W)
N_IN = sum(SPLIT_SIZES)

kernel_name = 'hymba_mlstm_diffattn_hgrn2_macaron'


def rms_norm(x, w):
    xf = x.astype(jnp.float32)
    y = xf * lax.rsqrt(jnp.mean(xf * xf, axis=-1, keepdims=True) + EPS)
    return (y * w.astype(jnp.float32)).astype(x.dtype)


def head_layer_norm(x, w):
    xf = x.astype(jnp.float32)
    xc = xf - jnp.mean(xf, axis=-1, keepdims=True)
    y = xc * lax.rsqrt(jnp.mean(xc * xc, axis=-1, keepdims=True) + EPS)
    return (y * w.astype(jnp.float32)).astype(x.dtype)


def swiglu(x, wi, wo):
    g, u = jnp.split(x @ wi, 2, axis=-1)
    return (jax.nn.silu(g) * u) @ wo


def causal_short_conv(x, w, b):
    S = x.shape[1]
    K = w.shape[0]
    xp = jnp.pad(x, ((0, 0), (K - 1, 0), (0, 0)))
    y = b
    for j in range(K):
        y = y + xp[:, j:j + S, :] * w[j]
    return y


def to_heads(t, H):
    B, S, _ = t.shape
    return t.reshape(B, S, H, -1).transpose(0, 2, 1, 3)


def from_heads(t):
    return t.transpose(0, 2, 1, 3)


def to_chunks(t):
    B, H, S = t.shape[:3]
    return jnp.moveaxis(t.reshape(B, H, S // CHUNK, CHUNK, *t.shape[3:]), 2, 0)


def from_chunks(t):
    NC, B, H, L = t.shape[:4]
    return jnp.moveaxis(t, 0, 2).reshape(B, H, NC * L, *t.shape[4:])


def mlstm_chunkwise(q, k, v, log_i, log_f):
    B, H, S, d = q.shape
    tri = jnp.tril(jnp.ones((CHUNK, CHUNK), dtype=bool))

    def step(carry, xs):
        C, n, m = carry
        qc, kc, vc, li, lf = xs
        b = jnp.cumsum(lf, axis=-1)
        dmat = jnp.where(tri, b[..., :, None] - b[..., None, :] + li[..., None, :], -jnp.inf)
        m_inter = b + m[..., None]
        m_t = jnp.maximum(jnp.max(dmat, axis=-1), m_inter)
        w = jnp.exp(dmat - m_t[..., None])
        sc = jnp.einsum('bhtd,bhsd->bhts', qc, kc) * w
        g = jnp.exp(m_inter - m_t)
        num = jnp.einsum('bhts,bhse->bhte', sc, vc) + g[..., None] * jnp.einsum('bhtd,bhde->bhte', qc, C)
        den = jnp.sum(sc, axis=-1) + g * jnp.einsum('bhtd,bhd->bht', qc, n)
        h = num / jnp.maximum(jnp.abs(den), jnp.exp(-m_t))[..., None]
        bl = b[..., -1]
        a = bl[..., None] - b + li
        m_new = jnp.maximum(bl + m, jnp.max(a, axis=-1))
        decay = jnp.exp(bl + m - m_new)
        wa = jnp.exp(a - m_new[..., None])
        C = decay[..., None, None] * C + jnp.einsum('bhs,bhsd,bhse->bhde', wa, kc, vc)
        n = decay[..., None] * n + jnp.einsum('bhs,bhsd->bhd', wa, kc)
        return (C, n, m_new), h

    init = (jnp.zeros((B, H, d, v.shape[-1]), jnp.float32), jnp.zeros((B, H, d), jnp.float32),
            jnp.zeros((B, H), jnp.float32))
    _, h = lax.scan(step, init, (to_chunks(q), to_chunks(k), to_chunks(v), to_chunks(log_i), to_chunks(log_f)))
    return from_chunks(h)


def hgrn2_chunkwise(q, k, v, log_f):
    B, H, S, dk = q.shape
    tri = jnp.tril(jnp.ones((CHUNK, CHUNK), dtype=bool))[:, :, None]

    def step(St, xs):
        qc, kc, vc, lf = xs
        b = jnp.cumsum(lf, axis=2)
        diff = b[:, :, :, None, :] - b[:, :, None, :, :]
        decay = jnp.exp(jnp.where(tri, diff, -jnp.inf))
        A = jnp.einsum('bhtsc,bhsc->bhts', decay * qc[:, :, :, None, :], kc)
        o = jnp.einsum('bhts,bhsv->bhtv', A, vc) + jnp.einsum('bhtc,bhcv->bhtv', qc * jnp.exp(b), St)
        bl = b[:, :, -1:, :]
        St = jnp.exp(bl[:, :, 0, :])[..., None] * St + jnp.einsum('bhsc,bhsv->bhcv', kc * jnp.exp(bl - b), vc)
        return St, o

    init = jnp.zeros((B, H, dk, v.shape[-1]), jnp.float32)
    _, o = lax.scan(step, init, (to_chunks(q), to_chunks(k), to_chunks(v), to_chunks(log_f)))
    return from_chunks(o)


def t5_bucket(rel):
    n = jnp.maximum(rel, 0)
    is_small = n < REL_MAX_EXACT
    nf = jnp.maximum(n, 1).astype(jnp.float32)
    large = REL_MAX_EXACT + (jnp.log(nf / REL_MAX_EXACT) / math.log(REL_MAX_DIST / REL_MAX_EXACT)
                             * (REL_BUCKETS - REL_MAX_EXACT)).astype(jnp.int32)
    large = jnp.minimum(large, REL_BUCKETS - 1)
    return jnp.where(is_small, n, large)


def diff_attention(q1, q2, k1, k2, v, lam, rel_bias):
    B, H, S, d = q1.shape
    nb = S // Q_BLOCK
    scale = d ** -0.5
    kpos = jnp.arange(S, dtype=jnp.int32)
    table = rel_bias.astype(jnp.float32)

    def qblocks(t):
        return jnp.moveaxis(t.reshape(B, H, nb, Q_BLOCK, t.shape[-1]), 2, 0)

    def one_block(args):
        q1b, q2b, j = args
        qpos = j * Q_BLOCK + jnp.arange(Q_BLOCK, dtype=jnp.int32)
        rel = qpos[:, None] - kpos[None, :]
        causal = rel >= 0
        bias = jnp.transpose(table[t5_bucket(rel)], (2, 0, 1))

        def probs(qb, kk):
            s = jnp.einsum('bhqd,bhkd->bhqk', qb, kk) * scale + bias
            return jax.nn.softmax(jnp.where(causal, s, -jnp.inf), axis=-1)

        a = probs(q1b, k1) - lam * probs(q2b, k2)
        return jnp.einsum('bhqk,bhkd->bhqd', a, v)

    out = lax.map(one_block, (qblocks(q1), qblocks(q2), jnp.arange(nb, dtype=jnp.int32)))
    return jnp.moveaxis(out, 0, 2).reshape(B, H, S, v.shape[-1])


def hybrid_mixer(h, layer, w_in, w_out, conv_w, conv_b, ig_b, fg_b, m_norm_w,
                 lam_vecs, df_norm_w, rel_bias, lb, hg_norm_w):
    f32 = jnp.float32
    B, S, _ = h.shape
    u = (h @ w_in).astype(f32)
    idx = [int(i) for i in np.cumsum(SPLIT_SIZES)[:-1]]
    (qk_m, v_m, o_m, i_m, f_m, q_d, k_d, v_d, q_h, f_h, i_h, g_h) = jnp.split(u, idx, axis=-1)

    qk_m = jax.nn.silu(causal_short_conv(qk_m, conv_w.astype(f32), conv_b.astype(f32)))
    q_mc, k_mc = jnp.split(qk_m, 2, axis=-1)
    qm = to_heads(q_mc, M_HEADS)
    km = to_heads(k_mc, M_HEADS) * (M_DH ** -0.5)
    vm = to_heads(v_m, M_HEADS)
    log_i = (i_m + ig_b.astype(f32)).transpose(0, 2, 1)
    log_f = jax.nn.log_sigmoid(f_m + fg_b.astype(f32)).transpose(0, 2, 1)
    hm = from_heads(mlstm_chunkwise(qm, km, vm, log_i, log_f))
    y_m = head_layer_norm(hm, m_norm_w.reshape(M_HEADS, M_DH)).reshape(B, S, M_W) * jax.nn.sigmoid(o_m)

    lam_init = 0.8 - 0.6 * math.exp(-0.3 * layer)
    lv = lam_vecs.astype(f32)
    lam = jnp.exp(jnp.sum(lv[0] * lv[1])) - jnp.exp(jnp.sum(lv[2] * lv[3])) + lam_init
    qd = q_d.reshape(B, S, DF_HEADS, 2, DF_DQK).transpose(0, 2, 1, 3, 4)
    kd = k_d.reshape(B, S, DF_HEADS, 2, DF_DQK).transpose(0, 2, 1, 3, 4)
    vd = to_heads(v_d, DF_HEADS)
    od = diff_attention(qd[..., 0, :], qd[..., 1, :], kd[..., 0, :], kd[..., 1, :], vd, lam, rel_bias)
    y_d = (rms_norm(from_heads(od), df_norm_w.reshape(DF_HEADS, DF_DV)) * (1.0 - lam_init)).reshape(B, S, DF_W)

    qh = jax.nn.silu(to_heads(q_h, H_HEADS))
    fp = to_heads(f_h, H_HEADS)
    lbh = lb.astype(f32).reshape(H_HEADS, 1, H_DK)
    log_fh = jnp.logaddexp(jnp.log(lbh), jnp.log1p(-lbh) + jax.nn.log_sigmoid(fp))
    kh = (1.0 - lbh) * jax.nn.sigmoid(-fp)
    vh = to_heads(i_h, H_HEADS)
    oh = from_heads(hgrn2_chunkwise(qh, kh, vh, log_fh))
    y_h = rms_norm(oh, hg_norm_w.reshape(H_HEADS, H_DV)).reshape(B, S, H_W) * jax.nn.silu(g_h)

    y = jnp.concatenate([y_m, y_d, y_h], axis=-1).astype(h.dtype)
    return y @ w_out


def setup_inputs(seed: int = 0) -> dict:
    key = jax.random.key(seed)
    ks = jax.random.split(key, 20)
    nrm = lambda k, shape, s: jax.random.normal(k, shape, jnp.float32) * s
    fg_base = jnp.linspace(3.0, 6.0, M_HEADS, dtype=jnp.float32)
    return {
        'x': nrm(ks[0], (BATCH, SEQ, D_MODEL), 1.0),
        'norm_w': 1.0 + nrm(ks[1], (DEPTH, 6, D_MODEL), 0.05),
        'ffn1_wi': nrm(ks[2], (DEPTH, D_MODEL, 2 * D_FF), D_MODEL ** -0.5),
        'ffn1_wo': nrm(ks[3], (DEPTH, D_FF, D_MODEL), D_FF ** -0.5),
        'ffn2_wi': nrm(ks[4], (DEPTH, D_MODEL, 2 * D_FF), D_MODEL ** -0.5),
        'ffn2_wo': nrm(ks[5], (DEPTH, D_FF, D_MODEL), D_FF ** -0.5),
        'w_in': nrm(ks[6], (DEPTH, D_MODEL, N_IN), D_MODEL ** -0.5),
        'w_out': nrm(ks[7], (DEPTH, D_MIX, D_MODEL), D_MIX ** -0.5),
        'mlstm_conv_w': nrm(ks[8], (DEPTH, CONV_W, 2 * M_W), CONV_W ** -0.5),
        'mlstm_conv_b': nrm(ks[9], (DEPTH, 2 * M_W), 0.01),
        'mlstm_igate_b': nrm(ks[10], (DEPTH, M_HEADS), 0.1),
        'mlstm_fgate_b': fg_base + nrm(ks[11], (DEPTH, M_HEADS), 0.1),
        'mlstm_norm_w': 1.0 + nrm(ks[12], (DEPTH, M_W), 0.05),
        'diff_lambda': nrm(ks[13], (DEPTH, 4, DF_DQK), 0.1),
        'diff_norm_w': 1.0 + nrm(ks[14], (DEPTH, DF_W), 0.05),
        'rel_bias': nrm(ks[15], (REL_BUCKETS, DF_HEADS), 0.5),
        'hgrn_lb_logits': nrm(ks[16], (DEPTH, H_HEADS * H_DK), 0.5),
        'hgrn_norm_w': 1.0 + nrm(ks[17], (DEPTH, H_W), 0.05),
    }


def reference(x, norm_w, ffn1_wi, ffn1_wo, ffn2_wi, ffn2_wo, w_in, w_out, mlstm_conv_w, mlstm_conv_b,
              mlstm_igate_b, mlstm_fgate_b, mlstm_norm_w, diff_lambda, diff_norm_w, rel_bias,
              hgrn_lb_logits, hgrn_norm_w):
    lb_all = jnp.cumsum(jax.nn.softmax(hgrn_lb_logits.astype(jnp.float32), axis=0), axis=0)
    lb_all = jnp.maximum(lb_all - lb_all[0:1], 0.0)
    for l in range(DEPTH):
        nw = norm_w[l]
        h = swiglu(rms_norm(x, nw[0]), ffn1_wi[l], ffn1_wo[l])
        x = x + 0.5 * rms_norm(h, nw[1])
        h = hybrid_mixer(rms_norm(x, nw[2]), l, w_in[l], w_out[l], mlstm_conv_w[l], mlstm_conv_b[l],
                         mlstm_igate_b[l], mlstm_fgate_b[l], mlstm_norm_w[l], diff_lambda[l],
                         diff_norm_w[l], rel_bias, lb_all[l], hgrn_norm_w[l])
        x = x + rms_norm(h, nw[3])
        h = swiglu(rms_norm(x, nw[4]), ffn2_wi[l], ffn2_wo[l])
        x = x + 0.5 * rms_norm(h, nw[5])
    return x
```

```python
import math
import numpy as np
from contextlib import ExitStack
import concourse.bass as bass
import concourse.mybir as mybir
from concourse.bass_utils import run_bass_kernel_spmd

F32 = mybir.dt.float32
BF16 = mybir.dt.bfloat16
AF = mybir.ActivationFunctionType
ALU = mybir.AluOpType
AX = mybir.AxisListType

S = 4096
D = 1024
DFF = 2816
NIN = 3592
NT = 32
NG = 8
DEPTH = 4
EPS = 1e-6
LN8 = math.log(0.125)


class Trk:
    __slots__ = ("w", "r", "psum")

    def __init__(self, psum=False):
        self.w = None
        self.r = []
        self.psum = psum


class Eng:
    def __init__(self, name, raw, sem):
        self.name = name
        self.raw = raw
        self.sem = sem
        self.count = 0
        self.waited = {}


class FW:
    NDMA = 8

    def __init__(self, nc, stack):
        self.nc = nc
        self.stack = stack
        self.engs = {}
        for name, raw in [("pe", nc.tensor), ("dve", nc.vector), ("act", nc.scalar),
                          ("pool", nc.gpsimd), ("sp", nc.sync)]:
            sem = stack.enter_context(nc.semaphore("sem_" + name))
            self.engs[name] = Eng(name, raw, sem)
        self.dma_sems = {}
        self.dma_count = {}
        for q in ["sp", "pool", "act"]:
            self.dma_sems[q] = [stack.enter_context(nc.semaphore("dsem_%s_%d" % (q, i))) for i in range(self.NDMA)]
            self.dma_count[q] = 0
        self.uid = 0
        self.pe_sync = False

    def name(self, base):
        self.uid += 1
        return "%s_%d" % (base, self.uid)

    def _deps(self, e, reads, writes):
        deps = {}

        def add(d):
            if d is None:
                return
            sem, val, en = d
            if en == "pe" and e.name == "pe" and not self.pe_sync:
                return
            k = id(sem)
            if e.waited.get(k, 0) >= val:
                return
            if k not in deps or deps[k][1] < val:
                deps[k] = (sem, val)
        for t in reads:
            add(t.w)
        for t in writes:
            add(t.w)
            for r in t.r:
                add(r)
        return list(deps.values())

    def _apply_waits(self, e, deps, instr_fn):
        for sem, val in deps[:-1]:
            e.raw.wait_ge(sem, val)
            e.waited[id(sem)] = val
        ins = instr_fn()
        if deps:
            sem, val = deps[-1]
            ins.wait_op(sem, val, "sem-ge")
            e.waited[id(sem)] = val
        return ins

    def op(self, eng, fn, reads=(), writes=()):
        e = self.engs[eng]
        pr = [t for t in reads if t.psum]
        if pr:
            reads = [t for t in reads if not t.psum]
            writes = list(writes) + pr
        deps = self._deps(e, reads, writes)
        ins = self._apply_waits(e, deps, fn)
        e.count += 1
        ins.then_inc(e.sem, 1)
        tag = (e.sem, e.count, e.name)
        for t in reads:
            t.r.append(tag)
        for t in writes:
            t.w = tag
            t.r = []
        return ins

    def dma(self, q, out, in_, reads=(), writes=(), **kw):
        e = self.engs[q]
        i = self.dma_count[q]
        self.dma_count[q] = i + 1
        sem = self.dma_sems[q][i % self.NDMA]
        val = 16 * (i // self.NDMA + 1)
        deps = self._deps(e, reads, writes)
        if i >= self.NDMA:
            pv = val - 16
            if e.waited.get(id(sem), 0) < pv:
                mx = max([pv] + [d[1] for d in deps if d[0] is sem])
                deps = [d for d in deps if d[0] is not sem] + [(sem, mx)]
        ins = self._apply_waits(e, deps, lambda: e.raw.dma_start(out=out, in_=in_, **kw))
        ins.then_inc(sem, 16)
        tag = (sem, val, "dma_" + q)
        for t in reads:
            t.r.append(tag)
        for t in writes:
            t.w = tag
            t.r = []
        return ins

    def barrier(self):
        targets = [(e.sem, e.count) for e in self.engs.values() if e.count > 0]
        for q in self.dma_sems:
            n = self.dma_count[q]
            for j, sem in enumerate(self.dma_sems[q]):
                cnt = (n - j + self.NDMA - 1) // self.NDMA if n > j else 0
                if cnt > 0:
                    targets.append((sem, 16 * cnt))
        for e in self.engs.values():
            for sem, val in targets:
                if sem is e.sem and e.name == "pe":
                    continue
                if e.waited.get(id(sem), 0) < val:
                    e.raw.wait_ge(sem, val)
                    e.waited[id(sem)] = val


class Phase:
    def __init__(self, K):
        self.K = K
        self.st = ExitStack()

    def __enter__(self):
        self.st.__enter__()
        return self

    def __exit__(self, *a):
        self.K.fw.barrier()
        return self.st.__exit__(*a)

    def sb(self, name, shape, dt):
        return self.st.enter_context(self.K.nc.sbuf_tensor(self.K.fw.name(name), list(shape), dt))

    def ps(self, name, shape, dt):
        full = 512 if dt == F32 else 1024
        t = self.st.enter_context(self.K.nc.psum_tensor(self.K.fw.name(name), [128, full], dt))
        n = 1
        for d in shape[1:]:
            n *= d
        assert shape[0] == 128 and n <= full
        v = t[:, 0:n]
        if len(shape) == 3:
            v = v.rearrange("p (a b) -> p a b", a=shape[1])
        return v


class Rot:
    def __init__(self, ph, name, shape, dt, n, psum=False):
        mk = ph.ps if psum else ph.sb
        self.bufs = [(mk(name, shape, dt), Trk(psum)) for _ in range(n)]
        self.i = 0

    def next(self):
        b = self.bufs[self.i % len(self.bufs)]
        self.i += 1
        return b


def t5_bucket_np(rel):
    n = np.maximum(rel, 0)
    nf = np.maximum(n, 1).astype(np.float32)
    large = 16 + (np.log(nf / np.float32(16)) / np.float32(math.log(128 / 16)) * np.float32(16)).astype(np.int32)
    large = np.minimum(large, 31)
    return np.where(n < 16, n, large)


C_IDENT, C_TRIT, C_TRIU, C_ONES, C_MASK, C_TRITBD, C_TRIMID, C_TRIUBD, C_MASKBD = [i * 128 for i in range(9)]
C_CHI = 9 * 128
C_EEI = C_CHI + 2
NCONST = C_EEI + 768


def make_consts():
    c = np.zeros((128, NCONST), np.float32)
    r = np.arange(128)[:, None]
    t = np.arange(128)[None, :]
    same = (r // 64) == (t // 64)
    c[:, C_IDENT:C_IDENT + 128] = (r == t)
    c[:, C_TRIT:C_TRIT + 128] = (r <= t)
    c[:, C_TRIU:C_TRIU + 128] = (r > t)
    c[:, C_ONES:C_ONES + 128] = 1.0
    c[:, C_MASK:C_MASK + 128] = (r <= t)
    c[:, C_TRITBD:C_TRITBD + 128] = (r <= t) & same
    mid = (t // 64) * 64 + 31
    c[:, C_TRIMID:C_TRIMID + 128] = (((r <= t).astype(np.float32) - (r <= mid).astype(np.float32)) * same)
    c[:, C_TRIUBD:C_TRIUBD + 128] = (r > t) & same
    c[:, C_MASKBD:C_MASKBD + 128] = (r <= t) & same
    c[:, C_CHI] = (np.arange(128) < 64)
    c[:, C_CHI + 1] = (np.arange(128) >= 64)
    k = np.arange(128)[:, None]
    col = np.arange(128)[None, :]
    rel0 = col - k
    idx0 = np.where(rel0 >= 0, t5_bucket_np(rel0), -1)
    idx1 = t5_bucket_np(128 + col - k)
    c[:, C_EEI:C_EEI + 128] = idx0
    c[:, C_EEI + 128:C_EEI + 256] = idx1
    c[:, C_EEI + 256:C_EEI + 768] = 31
    return c


class KCtx:
    pass


def rms_rstd(K, ph, src_ap, src_trk, n, junk, rstd, eps=EPS):
    nc, fw = K.nc, K.fw
    jb, jt = junk
    rb, rt = rstd
    fw.op("act", lambda: nc.scalar.activation(out=jb, in_=src_ap, func=AF.Square, accum_out=rb),
          reads=[src_trk], writes=[jt, rt])
    fw.op("act", lambda: nc.scalar.activation(out=rb, in_=rb, func=AF.Sqrt, scale=1.0 / n, bias=eps),
          reads=[rt], writes=[rt])
    fw.op("dve", lambda: nc.vector.reciprocal(out=rb, in_=rb), reads=[rt], writes=[rt])


def load_row(K, ph, name, src_1d, n, q="sp"):
    t = ph.sb(name, [128, n], F32)
    tr = Trk()
    K.fw.dma(q, t[:], src_1d.partition_broadcast(128), writes=[tr])
    return t, tr


def norm_transpose_group(K, ph, g, xg, t_xg, nwrow, t_nw, xnT, t_xnT, R):
    nc, fw = K.nc, K.fw
    for i in range(4):
        junk = R["junk"].next()
        rstd = R["rstd"].next()
        rms_rstd(K, ph, xg[:, i, :], t_xg[i], D, (junk[0][:], junk[1]), (rstd[0][:], rstd[1]))
        xn, t_xn = R["xn"].next()
        fw.op("dve", lambda: nc.vector.scalar_tensor_tensor(out=xn[:], in0=xg[:, i, :], scalar=rstd[0][:], in1=nwrow[:],
                                                            op0=ALU.mult, op1=ALU.mult),
              reads=[t_xg[i], rstd[1], t_nw], writes=[t_xn])
        pT, t_pT = R["pT"].next()
        for k in range(8):
            fw.op("pe", lambda: nc.tensor.transpose(out=pT[:, k, :], in_=xn[:, k * 128:(k + 1) * 128], identity=K.identb[:]),
                  reads=[t_xn, K.t_const], writes=[t_pT])
        fw.op("act", lambda: nc.scalar.copy(out=xnT[:, :, i * 128:(i + 1) * 128], in_=pT[:]), reads=[t_pT], writes=[t_xnT])


def token_phase(K, l, which, xin, t_xin, xout, t_xout):
    nc, fw = K.nc, K.fw
    wi = (K.ffn1_wi if which == 1 else K.ffn2_wi)[l]
    wo = (K.ffn1_wo if which == 1 else K.ffn2_wo)[l]
    npre, npost = (0, 1) if which == 1 else (4, 5)
    with Phase(K) as ph:
        nw_pre, t_nwpre = load_row(K, ph, "nwpre", K.norm_w[l, npre], D)
        nw_post, t_nwpost = load_row(K, ph, "nwpost", K.norm_w[l, npost], D)
        wo_sb = ph.sb("wo", [128, 22, D], BF16)
        t_wo = Trk()
        wo_v = wo.rearrange("(k p) n -> p k n", p=128)
        for kk in range(0, 22, 2):
            fw.dma("pool", wo_sb[:, kk:kk + 2, :], wo_v[:, kk:kk + 2, :], writes=[t_wo])
        if which == 2:
            nw3, t_nw3 = load_row(K, ph, "nw3", K.norm_w[l, 3], D)
            wout_sb = ph.sb("wout", [128, 8, D], BF16)
            t_wout = Trk()
            wout_v = K.w_out[l].rearrange("(k p) n -> p k n", p=128)
            for kk in range(0, 8, 2):
                fw.dma("pool", wout_sb[:, kk:kk + 2, :], wout_v[:, kk:kk + 2, :], writes=[t_wout])
            Ry = Rot(ph, "ytile", [128, D], BF16, 2)
            RyT = Rot(ph, "yT", [128, 8, 128], BF16, 2)
        R = {"junk": Rot(ph, "junk", [128, D], F32, 1), "rstd": Rot(ph, "rstd", [128, 1], F32, 4),
             "xn": Rot(ph, "xn", [128, D], BF16, 2), "pT": Rot(ph, "pT", [128, 8, 128], BF16, 2, psum=True)}
        nxg = 2 if which == 1 else 1
        xg_bufs = [(ph.sb("xg", [128, 4, D], F32), [Trk() for _ in range(4)]) for _ in range(nxg)]
        xnT = ph.sb("xnT", [128, 8, 512], BF16)
        t_xnT = Trk()
        aT = ph.sb("aT", [128, 22, 512], BF16)
        t_aT = Trk()
        Rw = Rot(ph, "wipiece", [128, 8, 2, 256], BF16, 3)
        Rpg = Rot(ph, "pg", [128, 512], F32, 2, psum=True)
        Rpu = Rot(ph, "pu", [128, 512], F32, 2, psum=True)
        Rpo = Rot(ph, "po", [128, 512], F32, 2, psum=True)
        Rsg = Rot(ph, "sg", [128, 512], F32, 2)
        Rh = Rot(ph, "h", [128, D], F32, 2)
        wi_v = wi.rearrange("(k p) n -> p k n", p=128)

        def load_piece(n):
            j = n % 11
            wb, t_wb = Rw.next()
            fw.dma("pool", wb[:, :, 0, :], wi_v[:, :, j * 256:(j + 1) * 256], writes=[t_wb])
            fw.dma("pool", wb[:, :, 1, :], wi_v[:, :, DFF + j * 256:DFF + (j + 1) * 256], writes=[t_wb])
            return wb, t_wb

        pieces = {}
        NP = NG * 11
        pieces[0] = load_piece(0)
        pieces[1] = load_piece(1)

        def load_x(g):
            xg, t_tiles = xg_bufs[g % nxg]
            for i in range(4):
                r0 = g * 512 + i * 128
                fw.dma("sp", xg[:, i, :], xin[r0:r0 + 128, :], reads=[t_xin[g]], writes=[t_tiles[i]])
            return xg, t_tiles

        cur = load_x(0)
        for g in range(NG):
            xg, t_xg = cur
            if which == 2:
                for i in range(4):
                    r0 = g * 512 + i * 128
                    yt, t_yt = Ry.next()
                    fw.dma("sp", yt[:], K.Y[r0:r0 + 128, :], reads=[K.t_Y[g]], writes=[t_yt])
                    pT, t_pT = R["pT"].next()
                    for k in range(8):
                        fw.op("pe", lambda: nc.tensor.transpose(out=pT[:, k, :], in_=yt[:, k * 128:(k + 1) * 128], identity=K.identb[:]),
                              reads=[t_yt, K.t_const], writes=[t_pT])
                    yT, t_yT = RyT.next()
                    fw.op("act", lambda: nc.scalar.copy(out=yT[:], in_=pT[:]), reads=[t_pT], writes=[t_yT])
                    h, t_h = Rh.next()
                    for hf in range(2):
                        po, t_po = Rpo.next()
                        for k in range(8):
                            fw.op("pe", lambda: nc.tensor.matmul(out=po[:], lhsT=yT[:, k, :], rhs=wout_sb[:, k, hf * 512:(hf + 1) * 512],
                                                                 start=(k == 0), stop=(k == 7)),
                                  reads=[t_yT, t_wout], writes=[t_po])
                        fw.op("act", lambda: nc.scalar.copy(out=h[:, hf * 512:(hf + 1) * 512], in_=po[:]), reads=[t_po], writes=[t_h])
                    junk = R["junk"].next()
                    rstd = R["rstd"].next()
                    rms_rstd(K, ph, h[:], t_h, D, (junk[0][:], junk[1]), (rstd[0][:], rstd[1]))
                    fw.op("dve", lambda: nc.vector.scalar_tensor_tensor(out=h[:], in0=h[:], scalar=rstd[0][:], in1=nw3[:],
                                                                        op0=ALU.mult, op1=ALU.mult),
                          reads=[t_h, rstd[1], t_nw3], writes=[t_h])
                    fw.op("dve", lambda: nc.vector.tensor_tensor(out=xg[:, i, :], in0=h[:], in1=xg[:, i, :], op=ALU.add),
                          reads=[t_h, t_xg[i]], writes=[t_xg[i]])
            norm_transpose_group(K, ph, g, xg, t_xg, nw_pre, t_nwpre, xnT, t_xnT, R)
            if g + 1 < NG and nxg == 2:
                cur = load_x(g + 1)
            for j in range(11):
                n = g * 11 + j
                if n + 2 < NP:
                    pieces[n + 2] = load_piece(n + 2)
                wb, t_wb = pieces.pop(n)
                for c in range(2):
                    pg, t_pg = Rpg.next()
                    pu, t_pu = Rpu.next()
                    for k in range(8):
                        fw.op("pe", lambda: nc.tensor.matmul(out=pg[:], lhsT=wb[:, k, 0, c * 128:(c + 1) * 128], rhs=xnT[:, k, :],
                                                             start=(k == 0), stop=(k == 7)),
                              reads=[t_wb, t_xnT], writes=[t_pg])
                    for k in range(8):
                        fw.op("pe", lambda: nc.tensor.matmul(out=pu[:], lhsT=wb[:, k, 1, c * 128:(c + 1) * 128], rhs=xnT[:, k, :],
                                                             start=(k == 0), stop=(k == 7)),
                              reads=[t_wb, t_xnT], writes=[t_pu])
                    sg, t_sg = Rsg.next()
                    fw.op("act", lambda: nc.scalar.activation(out=sg[:], in_=pg[:], func=AF.Silu), reads=[t_pg], writes=[t_sg])
                    fw.op("dve", lambda: nc.vector.tensor_tensor(out=aT[:, j * 2 + c, :], in0=sg[:], in1=pu[:], op=ALU.mult),
                          reads=[t_sg, t_pu], writes=[t_aT])
            for i in range(4):
                r0 = g * 512 + i * 128
                h, t_h = Rh.next()
                for hf in range(2):
                    po, t_po = Rpo.next()
                    for k in range(22):
                        fw.op("pe", lambda: nc.tensor.matmul(out=po[:], lhsT=aT[:, k, i * 128:(i + 1) * 128], rhs=wo_sb[:, k, hf * 512:(hf + 1) * 512],
                                                             start=(k == 0), stop=(k == 21)),
                              reads=[t_aT, t_wo], writes=[t_po])
                    fw.op("act", lambda: nc.scalar.copy(out=h[:, hf * 512:(hf + 1) * 512], in_=po[:]), reads=[t_po], writes=[t_h])
                junk = R["junk"].next()
                rstd = R["rstd"].next()
                rms_rstd(K, ph, h[:], t_h, D, (junk[0][:], junk[1]), (rstd[0][:], rstd[1]))
                fw.op("dve", lambda: nc.vector.scalar_tensor_tensor(out=h[:], in0=h[:], scalar=rstd[0][:], in1=nw_post[:],
                                                                    op0=ALU.mult, op1=ALU.mult),
                      reads=[t_h, rstd[1], t_nwpost], writes=[t_h])
                fw.op("dve", lambda: nc.vector.scalar_tensor_tensor(out=h[:], in0=h[:], scalar=0.5, in1=xg[:, i, :],
                                                                    op0=ALU.mult, op1=ALU.add),
                      reads=[t_h, t_xg[i]], writes=[t_h])
                fw.dma("sp", xout[r0:r0 + 128, :], h[:], reads=[t_h], writes=[t_xout[g]])
            if g + 1 < NG and nxg == 1:
                cur = load_x(g + 1)


def proj_phase(K, l, xsrc=None, t_xsrc=None):
    nc, fw = K.nc, K.fw
    if xsrc is None:
        xsrc, t_xsrc = K.X, K.t_X
    with Phase(K) as ph:
        nw2, t_nw2 = load_row(K, ph, "nw2", K.norm_w[l, 2], D)
        win_sb = ph.sb("win", [128, 8, NIN], BF16)
        t_win = Trk()
        win_v = K.w_in[l].rearrange("(k p) n -> p k n", p=128)
        for k in range(8):
            fw.dma("pool", win_sb[:, k, :], win_v[:, k, :], writes=[t_win])
        pp = ph.sb("pp", [128, 28], F32)
        t_pp = Trk()
        fw.dma("sp", pp[:], K.pp[l], writes=[t_pp])
        gb = ph.sb("gb", [128, 8], F32)
        t_gb = Trk()
        fw.dma("sp", gb[:, 0:4], K.ig_b[l].partition_broadcast(128), writes=[t_gb])
        fw.dma("sp", gb[:, 4:8], K.fg_b[l].partition_broadcast(128), writes=[t_gb])
        fw.op("dve", lambda: nc.vector.tensor_scalar(out=gb[:, 0:4], in0=gb[:, 0:4], scalar1=LN8, scalar2=None, op0=ALU.add),
              reads=[t_gb], writes=[t_gb])
        lgr = ph.sb("lgr", [128, 4, 256], F32)
        t_lgr = Trk()
        fw.dma("sp", lgr[:].rearrange("p a b -> p (a b)"), K.lb_logits.rearrange("a b -> (a b)").partition_broadcast(128), writes=[t_lgr])
        fw.op("act", lambda: nc.scalar.activation(out=lgr[:], in_=lgr[:], func=AF.Exp), reads=[t_lgr], writes=[t_lgr])
        lb_row = ph.sb("lb_row", [128, 256], F32)
        oml_row = ph.sb("oml_row", [128, 256], F32)
        tmp_row = ph.sb("tmp_row", [128, 256], F32)
        t_lb = Trk()
        fw.op("dve", lambda: nc.vector.tensor_tensor(out=tmp_row[:], in0=lgr[:, 0, :], in1=lgr[:, 1, :], op=ALU.add), reads=[t_lgr], writes=[t_lb])
        fw.op("dve", lambda: nc.vector.tensor_tensor(out=tmp_row[:], in0=tmp_row[:], in1=lgr[:, 2, :], op=ALU.add), reads=[t_lgr, t_lb], writes=[t_lb])
        fw.op("dve", lambda: nc.vector.tensor_tensor(out=tmp_row[:], in0=tmp_row[:], in1=lgr[:, 3, :], op=ALU.add), reads=[t_lgr, t_lb], writes=[t_lb])
        fw.op("dve", lambda: nc.vector.reciprocal(out=tmp_row[:], in_=tmp_row[:]), reads=[t_lb], writes=[t_lb])
        fw.op("dve", lambda: nc.vector.memset(lb_row[:], 0.0), writes=[t_lb])
        for j in range(1, l + 1):
            fw.op("dve", lambda: nc.vector.tensor_tensor(out=lb_row[:], in0=lb_row[:], in1=lgr[:, j, :], op=ALU.add), reads=[t_lgr, t_lb], writes=[t_lb])
        fw.op("dve", lambda: nc.vector.tensor_tensor(out=lb_row[:], in0=lb_row[:], in1=tmp_row[:], op=ALU.mult), reads=[t_lb], writes=[t_lb])
        fw.op("dve", lambda: nc.vector.tensor_scalar(out=oml_row[:], in0=lb_row[:], scalar1=-1.0, scalar2=1.0, op0=ALU.mult, op1=ALU.add),
              reads=[t_lb], writes=[t_lb])
        lgf = ph.sb("lgf", [128, 2, 4], F32)
        oml_fm = ph.sb("oml_fm", [128, 2], F32)
        tmpf = ph.sb("tmpf", [128, 2], F32)
        t_lf = Trk()
        fw.op("act", lambda: nc.scalar.activation(out=lgf[:], in_=pp[:, 20:28].rearrange("p (a b) -> p a b", a=2), func=AF.Exp),
              reads=[t_pp], writes=[t_lf])
        fw.op("dve", lambda: nc.vector.tensor_reduce(out=tmpf[:], in_=lgf[:], axis=AX.X, op=ALU.add), reads=[t_lf], writes=[t_lf])
        fw.op("dve", lambda: nc.vector.reciprocal(out=tmpf[:], in_=tmpf[:]), reads=[t_lf], writes=[t_lf])
        fw.op("dve", lambda: nc.vector.memset(oml_fm[:], 0.0), writes=[t_lf])
        for j in range(1, l + 1):
            fw.op("dve", lambda: nc.vector.tensor_tensor(out=oml_fm[:], in0=oml_fm[:], in1=lgf[:, :, j], op=ALU.add), reads=[t_lf], writes=[t_lf])
        fw.op("dve", lambda: nc.vector.tensor_tensor(out=oml_fm[:], in0=oml_fm[:], in1=tmpf[:], op=ALU.mult), reads=[t_lf], writes=[t_lf])
        fw.op("dve", lambda: nc.vector.tensor_scalar(out=oml_fm[:], in0=oml_fm[:], scalar1=-1.0, scalar2=1.0, op0=ALU.mult, op1=ALU.add),
              reads=[t_lf], writes=[t_lf])

        R = {"junk": Rot(ph, "junk", [128, D], F32, 1), "rstd": Rot(ph, "rstd", [128, 1], F32, 4),
             "xn": Rot(ph, "xn", [128, D], BF16, 2), "pT": Rot(ph, "pT", [128, 8, 128], BF16, 2, psum=True)}
        xg_bufs = [(ph.sb("xg", [128, 4, D], F32), [Trk() for _ in range(4)]) for _ in range(2)]
        xnT = ph.sb("xnT", [128, 8, 512], BF16)
        t_xnT = Trk()
        xc = ph.sb("xc", [128, 4, 515], F32)
        t_xc = [Trk() for _ in range(4)]
        fw.op("dve", lambda: nc.vector.memset(xc[:], 0.0), writes=t_xc)
        Rpf = Rot(ph, "pf", [128, 512], F32, 2, psum=True)
        Rpt = Rot(ph, "pt", [128, 512], F32, 2, psum=True)
        Rpk = Rot(ph, "pk", [128, 4, 128], BF16, 1, psum=True)
        Racc = Rot(ph, "acc", [128, 512], F32, 2)
        Rob = Rot(ph, "ob", [128, 512], BF16, 3)
        Rkt = Rot(ph, "kt", [128, 4, 128], BF16, 2)
        Rtf = Rot(ph, "tf", [128, 512], F32, 3)
        Rtb = Rot(ph, "tb", [128, 512], BF16, 3)
        Rg8 = Rot(ph, "g8", [128, 8], F32, 2)
        Rg4 = Rot(ph, "g4", [128, 4], F32, 2)

        def load_x(g):
            xg, t_tiles = xg_bufs[g % 2]
            for i in range(4):
                r0 = g * 512 + i * 128
                fw.dma("sp", xg[:, i, :], xsrc[r0:r0 + 128, :], reads=[t_xsrc[g]], writes=[t_tiles[i]])
            return xg, t_tiles

        cur = load_x(0)
        fchunks = [(ci * 128, "qkm", ci) for ci in range(4)] + [(1032 + 128 * h, "qd", h) for h in range(4)] + \
                  [(1544 + 128 * h, "kd", h) for h in range(4)] + [(2568 + 128 * p, "qh", p) for p in range(2)] + \
                  [(2824 + 128 * p, "fh", p) for p in range(2)]
        for g in range(NG):
            xg, t_xg = cur
            norm_transpose_group(K, ph, g, xg, t_xg, nw2, t_nw2, xnT, t_xnT, R)
            if g + 1 < NG:
                cur = load_x(g + 1)
            cs = slice(g * 512, (g + 1) * 512)
            import os
            SK = os.environ.get('PROJ_SKIP', '')
            for (c0, kind, ci) in ([] if 'F' in SK else fchunks):
                pf, t_pf = Rpf.next()
                for k in range(8):
                    fw.op("pe", lambda: nc.tensor.matmul(out=pf[:], lhsT=win_sb[:, k, c0:c0 + 128], rhs=xnT[:, k, :],
                                                         start=(k == 0), stop=(k == 7)),
                          reads=[t_win, t_xnT], writes=[t_pf])
                if kind == "qkm":
                    fw.op("act", lambda: nc.scalar.copy(out=xc[:, ci, 3:515], in_=pf[:]), reads=[t_pf], writes=[t_xc[ci]])
                    acc, t_acc = Racc.next()
                    fw.op("dve", lambda: nc.vector.tensor_scalar(out=acc[:], in0=xc[:, ci, 3:515], scalar1=pp[:, ci * 4 + 3:ci * 4 + 4],
                                                                 scalar2=pp[:, 16 + ci:17 + ci], op0=ALU.mult, op1=ALU.add),
                          reads=[t_xc[ci], t_pp], writes=[t_acc])
                    for j in range(3):
                        fw.op("dve", lambda: nc.vector.scalar_tensor_tensor(out=acc[:], in0=xc[:, ci, j:j + 512], scalar=pp[:, ci * 4 + j:ci * 4 + j + 1],
                                                                            in1=acc[:], op0=ALU.mult, op1=ALU.add),
                              reads=[t_xc[ci], t_pp, t_acc], writes=[t_acc])
                    fw.op("dve", lambda: nc.vector.tensor_copy(out=xc[:, ci, 0:3], in_=xc[:, ci, 512:515]), reads=[t_xc[ci]], writes=[t_xc[ci]])
                    ob, t_ob = Rob.next()
                    fw.op("act", lambda: nc.scalar.activation(out=ob[:], in_=acc[:], func=AF.Silu), reads=[t_acc], writes=[t_ob])
                    fw.dma("sp", K.QKM[ci * 128:(ci + 1) * 128, cs], ob[:], reads=[t_ob], writes=[K.t_QKM[g]])
                    if ci >= 2:
                        pk, t_pk = Rpk.next()
                        for i in range(4):
                            fw.op("pe", lambda: nc.tensor.transpose(out=pk[:, i, :], in_=ob[:, i * 128:(i + 1) * 128], identity=K.identb[:]),
                                  reads=[t_ob, K.t_const], writes=[t_pk])
                        kt, t_kt = Rkt.next()
                        fw.op("dve", lambda: nc.vector.tensor_copy(out=kt[:], in_=pk[:]), reads=[t_pk], writes=[t_kt])
                        fw.dma("sp", K.KMT[cs, (ci - 2) * 128:(ci - 1) * 128].rearrange("(i p) c -> p i c", p=128), kt[:],
                               reads=[t_kt], writes=[K.t_KMT[g]])
                elif kind in ("qd", "kd"):
                    ob, t_ob = Rob.next()
                    if ci % 2 == 0:
                        fw.op("act", lambda: nc.scalar.copy(out=ob[:], in_=pf[:]), reads=[t_pf], writes=[t_ob])
                    else:
                        fw.op("dve", lambda: nc.vector.tensor_copy(out=ob[:], in_=pf[:]), reads=[t_pf], writes=[t_ob])
                    dst, tr = (K.QD, K.t_QD) if kind == "qd" else (K.KD, K.t_KD)
                    fw.dma("sp", dst[ci * 128:(ci + 1) * 128, cs], ob[:], reads=[t_ob], writes=[tr[g]])
                elif kind == "qh":
                    ob, t_ob = Rob.next()
                    fw.op("act", lambda: nc.scalar.activation(out=ob[:], in_=pf[:], func=AF.Silu), reads=[t_pf], writes=[t_ob])
                    fw.dma("sp", K.QH[ci * 128:(ci + 1) * 128, cs], ob[:], reads=[t_ob], writes=[K.t_QH[g]])
                else:
                    acc, t_acc = Racc.next()
                    fw.op("act", lambda: nc.scalar.activation(out=acc[:], in_=pf[:], func=AF.Sigmoid, scale=-1.0), reads=[t_pf], writes=[t_acc])
                    ob, t_ob = Rob.next()
                    fw.op("dve", lambda: nc.vector.tensor_scalar(out=ob[:], in0=acc[:], scalar1=oml_fm[:, ci:ci + 1], scalar2=None, op0=ALU.mult),
                          reads=[t_acc, t_lf], writes=[t_ob])
                    fw.dma("sp", K.KH[ci * 128:(ci + 1) * 128, cs], ob[:], reads=[t_ob], writes=[K.t_KH[g]])
            for i in ([] if 'T' in SK else range(4)):
                r0 = g * 512 + i * 128
                rs = slice(r0, r0 + 128)
                lt = xnT

                def tok_mm(c0, n):
                    pt, t_pt = Rpt.next()
                    for k in range(8):
                        fw.op("pe", lambda: nc.tensor.matmul(out=pt[:, 0:n], lhsT=xnT[:, k, i * 128:(i + 1) * 128], rhs=win_sb[:, k, c0:c0 + n],
                                                             start=(k == 0), stop=(k == 7)),
                              reads=[t_win, t_xnT], writes=[t_pt])
                    return pt, t_pt
                BL = os.environ.get('PROJ_BLK', 'ABCDE')
                if 'A' in BL:
                    pt, t_pt = tok_mm(512, 512)
                    tb, t_tb = Rtb.next()
                    fw.op("dve", lambda: nc.vector.tensor_copy(out=tb[:, 0:256], in_=pt[:, 0:256]), reads=[t_pt], writes=[t_tb])
                    fw.dma("sp", K.VM[rs, :], tb[:, 0:256], reads=[t_tb], writes=[K.t_VM[g]])
                    tf, t_tf = Rtf.next()
                    fw.op("act", lambda: nc.scalar.activation(out=tf[:, 0:256], in_=pt[:, 256:512], func=AF.Sigmoid), reads=[t_pt], writes=[t_tf])
                    fw.dma("sp", K.OM[rs, :], tf[:, 0:256], reads=[t_tf], writes=[K.t_OM[g]])
                if 'B' in BL:
                    pt, t_pt = tok_mm(1024, 8)
                    g8, t_g8 = Rg8.next()
                    fw.op("dve", lambda: nc.vector.tensor_tensor(out=g8[:], in0=pt[:, 0:8], in1=gb[:], op=ALU.add), reads=[t_pt, t_gb], writes=[t_g8])
                    g4, t_g4 = Rg4.next()
                    fw.op("act", lambda: nc.scalar.activation(out=g4[:], in_=g8[:, 4:8], func=AF.Exp, scale=-1.0), reads=[t_g8], writes=[t_g4])
                    fw.op("act", lambda: nc.scalar.activation(out=g4[:], in_=g4[:], func=AF.Ln, bias=1.0), reads=[t_g4], writes=[t_g4])
                    fw.op("dve", lambda: nc.vector.tensor_scalar(out=g8[:, 4:8], in0=g4[:], scalar1=-1.0, scalar2=None, op0=ALU.mult),
                          reads=[t_g4, t_g8], writes=[t_g8])
                    fw.dma("sp", K.GM[rs, :], g8[:], reads=[t_g8], writes=[K.t_GM[g]])
                if 'C' in BL:
                    pt, t_pt = tok_mm(2056, 512)
                    tb, t_tb = Rtb.next()
                    fw.op("act", lambda: nc.scalar.copy(out=tb[:], in_=pt[:]), reads=[t_pt], writes=[t_tb])
                    fw.dma("sp", K.VD[rs, :], tb[:], reads=[t_tb], writes=[K.t_VD[g]])
                if 'D' in BL:
                    pt, t_pt = tok_mm(2824, 512)
                    tf, t_tf = Rtf.next()
                    fw.op("act", lambda: nc.scalar.activation(out=tf[:, 0:256], in_=pt[:, 0:256], func=AF.Sigmoid), reads=[t_pt], writes=[t_tf])
                    fw.op("dve", lambda: nc.vector.tensor_tensor(out=tf[:, 0:256], in0=tf[:, 0:256], in1=oml_row[:], op=ALU.mult), reads=[t_tf, t_lb], writes=[t_tf])
                    fw.op("dve", lambda: nc.vector.tensor_tensor(out=tf[:, 0:256], in0=tf[:, 0:256], in1=lb_row[:], op=ALU.add), reads=[t_tf, t_lb], writes=[t_tf])
                    fw.op("dve", lambda: nc.vector.tensor_scalar(out=tf[:, 256:512], in0=tf[:, 0:256], scalar1=-1.0, scalar2=1.0, op0=ALU.mult, op1=ALU.add),
                          reads=[t_tf], writes=[t_tf])
                    fw.op("act", lambda: nc.scalar.activation(out=tf[:, 0:256], in_=tf[:, 0:256], func=AF.Ln), reads=[t_tf], writes=[t_tf])
                    fw.dma("sp", K.LFH[rs, :], tf[:, 0:256], reads=[t_tf], writes=[K.t_LFH[g]])
                    fw.dma("sp", K.KHT[rs, :], tf[:, 256:512], reads=[t_tf], writes=[K.t_KHT[g]])
                    tb, t_tb = Rtb.next()
                    fw.op("dve", lambda: nc.vector.tensor_copy(out=tb[:, 0:256], in_=pt[:, 256:512]), reads=[t_pt], writes=[t_tb])
                    fw.dma("sp", K.VH[rs, :], tb[:, 0:256], reads=[t_tb], writes=[K.t_VH[g]])
                if 'E' in BL:
                    pt, t_pt = tok_mm(3336, 256)
                    tf, t_tf = Rtf.next()
                    fw.op("act", lambda: nc.scalar.activation(out=tf[:, 0:256], in_=pt[:, 0:256], func=AF.Silu), reads=[t_pt], writes=[t_tf])
                    fw.dma("sp", K.GH[rs, :], tf[:, 0:256], reads=[t_tf], writes=[K.t_GH[g]])


def bview(ap, shape):
    return ap.unsqueeze(2).to_broadcast(shape)


def mlstm_phase(K, l):
    nc, fw = K.nc, K.fw
    cf = K.constf
    with Phase(K) as ph:
        qT = ph.sb("mqT", [128, 2, S], BF16)
        kT = ph.sb("mkT", [128, 2, S], BF16)
        ktok = ph.sb("mktok", [128, NT, 256], BF16)
        vaug = ph.sb("mvaug", [128, NT, 4, 66], BF16)
        gm = ph.sb("mgm", [128, NT, 8], F32)
        t_in = [Trk() for _ in range(NG)]
        t_gm = Trk()
        fw.op("pool", lambda: nc.gpsimd.memset(vaug[:], 1.0), writes=t_in)
        for g_ in range(NG):
            fw.dma("sp", gm[:, g_ * 4:(g_ + 1) * 4, :], K.GM[g_ * 512:(g_ + 1) * 512, :].rearrange("(c p) g -> p c g", p=128), reads=[K.t_GM[g_]], writes=[t_gm])
        mnw, t_mnw = load_row(K, ph, "mnw", K.m_nw[l], 256)
        for g in range(NG):
            cs = slice(g * 512, (g + 1) * 512)
            ts = slice(g * 4, (g + 1) * 4)
            for p in range(2):
                fw.dma("sp", qT[:, p, cs], K.QKM[p * 128:(p + 1) * 128, cs], reads=[K.t_QKM[g]], writes=[t_in[g]])
                fw.dma("sp", kT[:, p, cs], K.QKM[256 + p * 128:256 + (p + 1) * 128, cs], reads=[K.t_QKM[g]], writes=[t_in[g]])
            fw.dma("sp", ktok[:, ts, :], K.KMT[cs, :].rearrange("(c p) f -> p c f", p=128), reads=[K.t_KMT[g]], writes=[t_in[g]])
            for c4 in range(4):
                cc_ = g * 4 + c4
                fw.dma("sp", vaug[:, cc_, :, 0:64], K.VM[cc_ * 128:(cc_ + 1) * 128, :].rearrange("p (h e) -> p h e", h=4), reads=[K.t_VM[g]], writes=[t_in[g]])
        lfc = ph.sb("lfc", [128, 128], F32)
        lic = ph.sb("lic", [128, 128], F32)
        eb = ph.sb("eb", [128, 128], F32)
        al = ph.sb("al", [128, 128], F32)
        wa = ph.sb("wa", [128, 128], F32)
        ebL = ph.sb("ebL", [128, 128], F32)
        ebL2 = ph.sb("ebL2", [128, NT, 2], F32)
        t_g = Trk()
        fw.op("dve", lambda: nc.vector.tensor_copy(out=lfc[:].rearrange("p (c h) -> p c h", h=4), in_=gm[:, :, 4:8]), reads=[t_gm], writes=[t_g])
        fw.op("dve", lambda: nc.vector.tensor_copy(out=lic[:].rearrange("p (c h) -> p c h", h=4), in_=gm[:, :, 0:4]), reads=[t_gm], writes=[t_g])
        Rpp = Rot(ph, "mpp", [128, 128], F32, 1, psum=True)
        pp_, t_pp_ = Rpp.next()
        fw.op("pe", lambda: nc.tensor.matmul(out=pp_[:], lhsT=cf[:, C_TRIT:C_TRIT + 128], rhs=lfc[:], start=True, stop=True), reads=[K.t_const, t_g], writes=[t_pp_])
        fw.op("act", lambda: nc.scalar.activation(out=eb[:], in_=pp_[:], func=AF.Exp), reads=[t_pp_], writes=[t_g])
        fw.op("dve", lambda: nc.vector.tensor_tensor(out=al[:], in0=lic[:], in1=pp_[:], op=ALU.subtract), reads=[t_pp_, t_g], writes=[t_g])
        fw.op("act", lambda: nc.scalar.activation(out=al[:], in_=al[:], func=AF.Exp), reads=[t_g], writes=[t_g])
        fw.op("pe", lambda: nc.tensor.matmul(out=pp_[:], lhsT=cf[:, C_TRIU:C_TRIU + 128], rhs=lfc[:], start=True, stop=True), reads=[K.t_const, t_g], writes=[t_pp_])
        fw.op("dve", lambda: nc.vector.tensor_tensor(out=wa[:], in0=lic[:], in1=pp_[:], op=ALU.add), reads=[t_pp_, t_g], writes=[t_g])
        fw.op("act", lambda: nc.scalar.activation(out=wa[:], in_=wa[:], func=AF.Exp), reads=[t_g], writes=[t_g])
        fw.op("pe", lambda: nc.tensor.matmul(out=pp_[:], lhsT=cf[:, C_ONES:C_ONES + 128], rhs=lfc[:], start=True, stop=True), reads=[K.t_const, t_g], writes=[t_pp_])
        fw.op("act", lambda: nc.scalar.activation(out=ebL[:], in_=pp_[:], func=AF.Exp), reads=[t_pp_], writes=[t_g])
        ebL4 = ebL[:].rearrange("p (c q h) -> p c q h", q=2, h=2)
        fw.op("dve", lambda: nc.vector.tensor_copy(out=ebL2[0:64], in_=ebL4[0:64, :, :, 0]), reads=[t_g], writes=[t_g])
        fw.op("dve", lambda: nc.vector.tensor_copy(out=ebL2[64:128], in_=ebL4[64:128, :, :, 1]), reads=[t_g], writes=[t_g])
        Cst = [ph.sb("mC", [128, 132], F32) for _ in range(2)]
        t_C = [Trk() for _ in range(2)]
        RCb = [Rot(ph, "mCb", [128, 132], BF16, 2) for _ in range(2)]
        cb = []
        for p in range(2):
            fw.op("dve", lambda: nc.vector.memset(Cst[p][:], 0.0), writes=[t_C[p]])
            b_, t_b = RCb[p].next()
            fw.op("dve", lambda: nc.vector.memset(b_[:], 0.0), writes=[t_b])
            cb.append((b_, t_b))
        Rps = Rot(ph, "mps", [128, 4, 128], F32, 2, psum=True)
        Rpn = Rot(ph, "mpn", [128, 4, 65], F32, 2, psum=True)
        Rpc = Rot(ph, "mpc", [128, 132], F32, 2, psum=True)
        RscT = Rot(ph, "mscT", [128, 4, 128], BF16, 2)
        Rs4 = Rot(ph, "ms4", [128, 4], F32, 8)
        Rhm = Rot(ph, "mhm", [128, 4, 64], F32, 2)
        Rsq = Rot(ph, "msq", [128, 4, 64], F32, 2)
        Rom = Rot(ph, "mom", [128, 256], F32, 2)
        Ryb = Rot(ph, "myb", [128, 256], BF16, 2)
        Rkw = Rot(ph, "mkw", [128, 256], BF16, 2)
        for c in range(NT):
            g = c // 4
            cs = slice(c * 128, (c + 1) * 128)
            om, t_om = Rom.next()
            fw.dma("sp", om[:], K.OM[cs, :], reads=[K.t_OM[g]], writes=[t_om])
            ps, t_ps = Rps.next()
            for h in range(4):
                p, hp = h // 2, h % 2
                rr = slice(hp * 64, (hp + 1) * 64)
                fw.op("pe", lambda: nc.tensor.matmul(out=ps[:, h, :], lhsT=kT[rr, p, cs], rhs=qT[rr, p, cs], start=True, stop=True),
                      reads=[t_in[g]], writes=[t_ps])
            scT, t_scT = RscT.next()
            for h in range(4):
                fw.op("dve", lambda: nc.vector.scalar_tensor_tensor(out=scT[:, h, :], in0=ps[:, h, :], scalar=al[:, c * 4 + h:c * 4 + h + 1],
                                                                    in1=cf[:, C_MASK:C_MASK + 128], op0=ALU.mult, op1=ALU.mult),
                      reads=[t_ps, t_g, K.t_const], writes=[t_scT])
            pn, t_pn = Rpn.next()
            for h in range(4):
                p, hp = h // 2, h % 2
                rr = slice(hp * 64, (hp + 1) * 64)
                fw.op("pe", lambda: nc.tensor.matmul(out=pn[:, h, :], lhsT=scT[:, h, :], rhs=vaug[:, c, h, 0:65], start=True, stop=False),
                      reads=[t_scT, t_in[g]], writes=[t_pn])
                fw.op("pe", lambda: nc.tensor.matmul(out=pn[:, h, :], lhsT=qT[rr, p, cs], rhs=cb[p][0][rr, hp * 66:hp * 66 + 65], start=False, stop=True),
                      reads=[t_in[g], cb[p][1]], writes=[t_pn])
            eb4 = eb[:, c * 4:(c + 1) * 4]
            d4, t_d4 = Rs4.next()
            fw.op("dve", lambda: nc.vector.tensor_tensor(out=d4[:], in0=pn[:, :, 64], in1=eb4, op=ALU.mult), reads=[t_pn, t_g], writes=[t_d4])
            n4, t_n4 = Rs4.next()
            fw.op("dve", lambda: nc.vector.tensor_scalar(out=n4[:], in0=d4[:], scalar1=-1.0, scalar2=None, op0=ALU.mult), reads=[t_d4], writes=[t_n4])
            fw.op("dve", lambda: nc.vector.scalar_tensor_tensor(out=d4[:], in0=d4[:], scalar=1.0, in1=n4[:], op0=ALU.max, op1=ALU.max), reads=[t_d4, t_n4], writes=[t_d4])
            fw.op("dve", lambda: nc.vector.reciprocal(out=d4[:], in_=d4[:]), reads=[t_d4], writes=[t_d4])
            fw.op("dve", lambda: nc.vector.tensor_tensor(out=d4[:], in0=d4[:], in1=eb4, op=ALU.mult), reads=[t_d4, t_g], writes=[t_d4])
            hm, t_hm = Rhm.next()
            fw.op("dve", lambda: nc.vector.tensor_tensor(out=hm[:], in0=pn[:, :, 0:64], in1=bview(d4[:], [128, 4, 64]), op=ALU.mult),
                  reads=[t_pn, t_d4], writes=[t_hm])
            m4, t_m4 = Rs4.next()
            fw.op("dve", lambda: nc.vector.tensor_reduce(out=m4[:], in_=hm[:], axis=AX.X, op=ALU.add), reads=[t_hm], writes=[t_m4])
            fw.op("dve", lambda: nc.vector.tensor_scalar(out=m4[:], in0=m4[:], scalar1=-1.0 / 64, scalar2=None, op0=ALU.mult), reads=[t_m4], writes=[t_m4])
            fw.op("dve", lambda: nc.vector.tensor_tensor(out=hm[:], in0=hm[:], in1=bview(m4[:], [128, 4, 64]), op=ALU.add), reads=[t_hm, t_m4], writes=[t_hm])
            sq, t_sq = Rsq.next()
            fw.op("pool", lambda: nc.gpsimd.tensor_tensor(out=sq[:], in0=hm[:], in1=hm[:], op=ALU.mult), reads=[t_hm], writes=[t_sq])
            v4, t_v4 = Rs4.next()
            fw.op("dve", lambda: nc.vector.tensor_reduce(out=v4[:], in_=sq[:], axis=AX.X, op=ALU.add), reads=[t_sq], writes=[t_v4])
            fw.op("act", lambda: nc.scalar.activation(out=v4[:], in_=v4[:], func=AF.Sqrt, scale=1.0 / 64, bias=EPS), reads=[t_v4], writes=[t_v4])
            fw.op("dve", lambda: nc.vector.reciprocal(out=v4[:], in_=v4[:]), reads=[t_v4], writes=[t_v4])
            fw.op("dve", lambda: nc.vector.tensor_tensor(out=hm[:], in0=hm[:], in1=bview(v4[:], [128, 4, 64]), op=ALU.mult), reads=[t_hm, t_v4], writes=[t_hm])
            hm2 = hm[:].rearrange("p h e -> p (h e)")
            fw.op("pool", lambda: nc.gpsimd.tensor_tensor(out=hm2, in0=hm2, in1=mnw[:], op=ALU.mult), reads=[t_hm, t_mnw], writes=[t_hm])
            yb, t_yb = Ryb.next()
            fw.op("dve", lambda: nc.vector.tensor_tensor(out=yb[:], in0=hm2, in1=om[:], op=ALU.mult), reads=[t_hm, t_om], writes=[t_yb])
            fw.dma("sp", K.Y[cs, 0:256], yb[:], reads=[t_yb], writes=[K.t_Y[g]])
            if c + 1 < NT:
                kw, t_kw = Rkw.next()
                fw.op("dve", lambda: nc.vector.tensor_tensor(out=kw[:].rearrange("p (h e) -> p h e", h=4), in0=ktok[:, c, :].rearrange("p (h e) -> p h e", h=4),
                                                              in1=bview(wa[:, c * 4:(c + 1) * 4], [128, 4, 64]), op=ALU.mult),
                      reads=[t_in[g], t_g], writes=[t_kw])
                for p in range(2):
                    pc, t_pc = Rpc.next()
                    fw.op("pe", lambda: nc.tensor.matmul(out=pc[:], lhsT=kw[:, p * 128:(p + 1) * 128],
                                                         rhs=vaug[:, c, 2 * p:2 * p + 2, :].rearrange("p a b -> p (a b)"), start=True, stop=True),
                          reads=[t_kw, t_in[g]], writes=[t_pc])
                    fw.op("dve", lambda: nc.vector.scalar_tensor_tensor(out=Cst[p][:], in0=Cst[p][:], scalar=ebL2[:, c, p:p + 1], in1=pc[:],
                                                                        op0=ALU.mult, op1=ALU.add),
                          reads=[t_C[p], t_g, t_pc], writes=[t_C[p]])
                    b_, t_b = RCb[p].next()
                    fw.op("act", lambda: nc.scalar.copy(out=b_[:], in_=Cst[p][:]), reads=[t_C[p]], writes=[t_b])
                    cb[p] = (b_, t_b)


def hgrn_phase(K, l):
    nc, fw = K.nc, K.fw
    cf = K.constf
    with Phase(K) as ph:
        qT = ph.sb("hqT", [128, 2, S], BF16)
        kT = ph.sb("hkT", [128, 2, S], BF16)
        lf = ph.sb("hlf", [128, NT, 256], F32)
        kht = ph.sb("hkht", [128, NT, 256], F32)
        vh = ph.sb("hvh", [128, NT, 256], BF16)
        t_in = [Trk() for _ in range(NG)]
        hnw, t_hnw = load_row(K, ph, "hnw", K.h_nw[l], 256)
        for g in range(NG):
            cs = slice(g * 512, (g + 1) * 512)
            ts = slice(g * 4, (g + 1) * 4)
            for p in range(2):
                fw.dma("sp", qT[:, p, cs], K.QH[p * 128:(p + 1) * 128, cs], reads=[K.t_QH[g]], writes=[t_in[g]])
                fw.dma("sp", kT[:, p, cs], K.KH[p * 128:(p + 1) * 128, cs], reads=[K.t_KH[g]], writes=[t_in[g]])
            fw.dma("sp", lf[:, ts, :], K.LFH[cs, :].rearrange("(c p) f -> p c f", p=128), reads=[K.t_LFH[g]], writes=[t_in[g]])
            fw.dma("sp", kht[:, ts, :], K.KHT[cs, :].rearrange("(c p) f -> p c f", p=128), reads=[K.t_KHT[g]], writes=[t_in[g]])
            fw.dma("sp", vh[:, ts, :], K.VH[cs, :].rearrange("(c p) f -> p c f", p=128), reads=[K.t_VH[g]], writes=[t_in[g]])
        Sst = [ph.sb("hS", [128, 128], F32) for _ in range(2)]
        t_S = [Trk() for _ in range(2)]
        RSb = [Rot(ph, "hSb", [128, 128], BF16, 3) for _ in range(2)]
        sb_ = []
        for p in range(2):
            fw.op("dve", lambda: nc.vector.memset(Sst[p][:], 0.0), writes=[t_S[p]])
            b_, t_b = RSb[p].next()
            fw.op("dve", lambda: nc.vector.memset(b_[:], 0.0), writes=[t_b])
            sb_.append((b_, t_b))
        q01 = [[ph.sb("hq01", [128, 2, 128], BF16) for _ in range(2)] for _ in range(2)]
        t_q01 = [[Trk() for _ in range(2)] for _ in range(2)]
        for r in range(2):
            for j in range(2):
                fw.op("pool", lambda: nc.gpsimd.memset(q01[r][j][:], 0.0), writes=[t_q01[r][j]])
        Rpm = Rot(ph, "hpm", [128, 128], F32, 2, psum=True)
        Rpl = Rot(ph, "hpl", [128, 2], F32, 1, psum=True)
        Rprb = Rot(ph, "hprb", [128, 256], F32, 1, psum=True)
        Rpa = Rot(ph, "hpa", [128, 4, 128], F32, 1, psum=True)
        Rpo = Rot(ph, "hpo", [128, 4, 64], F32, 1, psum=True)
        Rpss = Rot(ph, "hpss", [128, 128], F32, 2, psum=True)
        RE = Rot(ph, "hE", [128, 128], F32, 4)
        Rqt = Rot(ph, "hqt", [128, 2, 128], BF16, 2)
        Rkt = Rot(ph, "hkt", [128, 2, 128], BF16, 2)
        Rebl = Rot(ph, "hebl", [128, 2, 2], F32, 2)
        Rwk = Rot(ph, "hwk", [128, 256], F32, 2)
        Rkp = Rot(ph, "hkp", [128, 256], BF16, 2)
        RAT = Rot(ph, "hAT", [128, 4, 128], BF16, 2)
        Ros = Rot(ph, "hos", [128, 4, 64], F32, 2)
        Rsq = Rot(ph, "hsq", [128, 4, 64], F32, 2)
        Rs4 = Rot(ph, "hs4", [128, 4], F32, 4)
        Rgh = Rot(ph, "hgh", [128, 256], F32, 2)
        Ryb = Rot(ph, "hyb", [128, 256], BF16, 2)
        for i in range(NT):
            g = i // 4
            cs = slice(i * 128, (i + 1) * 128)
            gh, t_gh = Rgh.next()
            fw.dma("sp", gh[:], K.GH[cs, :], reads=[K.t_GH[g]], writes=[t_gh])
            qt, t_qt = Rqt.next()
            kt, t_kt = Rkt.next()
            ebl, t_ebl = Rebl.next()
            q0, q1 = q01[i % 2]
            t_q0, t_q1 = t_q01[i % 2]
            for p in range(2):
                lfp = lf[:, i, p * 128:(p + 1) * 128]
                pm, t_pm = Rpm.next()
                fw.op("pe", lambda: nc.tensor.matmul(out=pm[:], lhsT=lfp, rhs=cf[:, C_TRIMID:C_TRIMID + 128], start=True, stop=True),
                      reads=[t_in[g], K.t_const], writes=[t_pm])
                E1, t_E1 = RE.next()
                E2, t_E2 = RE.next()
                fw.op("act", lambda: nc.scalar.activation(out=E1[:], in_=pm[:], func=AF.Exp), reads=[t_pm], writes=[t_E1])
                fw.op("act", lambda: nc.scalar.activation(out=E2[:], in_=pm[:], func=AF.Exp, scale=-1.0), reads=[t_pm], writes=[t_E2])
                fw.op("dve", lambda: nc.vector.tensor_tensor(out=qt[:, p, :], in0=qT[:, p, cs], in1=E1[:], op=ALU.mult), reads=[t_in[g], t_E1], writes=[t_qt])
                fw.op("pool", lambda: nc.gpsimd.tensor_tensor(out=kt[:, p, :], in0=kT[:, p, cs], in1=E2[:], op=ALU.mult), reads=[t_in[g], t_E2], writes=[t_kt])
                pb, t_pb = Rpm.next()
                fw.op("pe", lambda: nc.tensor.matmul(out=pb[:], lhsT=lfp, rhs=cf[:, C_TRITBD:C_TRITBD + 128], start=True, stop=True),
                      reads=[t_in[g], K.t_const], writes=[t_pb])
                E3, t_E3 = RE.next()
                fw.op("act", lambda: nc.scalar.activation(out=E3[:], in_=pb[:], func=AF.Exp), reads=[t_pb], writes=[t_E3])
                fw.op("dve", lambda: nc.vector.tensor_tensor(out=q0[:, p, 0:64], in0=qT[:, p, i * 128:i * 128 + 64], in1=E3[:, 0:64], op=ALU.mult),
                      reads=[t_in[g], t_E3], writes=[t_q0])
                fw.op("dve", lambda: nc.vector.tensor_tensor(out=q1[:, p, 64:128], in0=qT[:, p, i * 128 + 64:(i + 1) * 128], in1=E3[:, 64:128], op=ALU.mult),
                      reads=[t_in[g], t_E3], writes=[t_q1])
                pl, t_pl = Rpl.next()
                fw.op("pe", lambda: nc.tensor.matmul(out=pl[:], lhsT=lfp, rhs=cf[:, C_CHI:C_CHI + 2], start=True, stop=True),
                      reads=[t_in[g], K.t_const], writes=[t_pl])
                fw.op("act", lambda: nc.scalar.activation(out=ebl[:, p, :], in_=pl[:], func=AF.Exp), reads=[t_pl], writes=[t_ebl])
            prb, t_prb = Rprb.next()
            fw.op("pe", lambda: nc.tensor.matmul(out=prb[:], lhsT=cf[:, C_TRIUBD:C_TRIUBD + 128], rhs=lf[:, i, :], start=True, stop=True),
                  reads=[t_in[g], K.t_const], writes=[t_prb])
            wk, t_wk = Rwk.next()
            fw.op("act", lambda: nc.scalar.activation(out=wk[:], in_=prb[:], func=AF.Exp), reads=[t_prb], writes=[t_wk])
            kp, t_kp = Rkp.next()
            fw.op("pool", lambda: nc.gpsimd.tensor_tensor(out=kp[:], in0=kht[:, i, :], in1=wk[:], op=ALU.mult), reads=[t_in[g], t_wk], writes=[t_kp])
            pa, t_pa = Rpa.next()
            for h in range(4):
                p, hp = h // 2, h % 2
                rr = slice(hp * 64, (hp + 1) * 64)
                fw.op("pe", lambda: nc.tensor.matmul(out=pa[:, h, :], lhsT=kt[rr, p, :], rhs=qt[rr, p, :], start=True, stop=True),
                      reads=[t_kt, t_qt], writes=[t_pa])
            AT, t_AT = RAT.next()
            fw.op("dve", lambda: nc.vector.tensor_tensor(out=AT[:], in0=pa[:], in1=cf[:, C_MASKBD:C_MASKBD + 128].unsqueeze(1).to_broadcast([128, 4, 128]), op=ALU.mult),
                  reads=[t_pa, K.t_const], writes=[t_AT])

            def state_update(j):
                rj = slice(j * 64, (j + 1) * 64)
                for p in range(2):
                    pss, t_pss = Rpss.next()
                    fw.op("pe", lambda: nc.tensor.matmul(out=pss[:], lhsT=kp[rj, p * 128:(p + 1) * 128], rhs=vh[rj, i, p * 128:(p + 1) * 128], start=True, stop=True),
                          reads=[t_kp, t_in[g]], writes=[t_pss])
                    fw.op("dve", lambda: nc.vector.scalar_tensor_tensor(out=Sst[p][:], in0=Sst[p][:], scalar=ebl[:, p, j:j + 1], in1=pss[:],
                                                                        op0=ALU.mult, op1=ALU.add),
                          reads=[t_S[p], t_ebl, t_pss], writes=[t_S[p]])
                    b_, t_b = RSb[p].next()
                    fw.op("act", lambda: nc.scalar.copy(out=b_[:], in_=Sst[p][:]), reads=[t_S[p]], writes=[t_b])
                    yield (b_, t_b)
            s0 = list(sb_)
            s1 = list(state_update(0))
            po, t_po = Rpo.next()
            for h in range(4):
                p, hp = h // 2, h % 2
                rr = slice(hp * 64, (hp + 1) * 64)
                cc = slice(hp * 64, (hp + 1) * 64)
                fw.op("pe", lambda: nc.tensor.matmul(out=po[:, h, :], lhsT=AT[:, h, :], rhs=vh[:, i, h * 64:(h + 1) * 64], start=True, stop=False),
                      reads=[t_AT, t_in[g]], writes=[t_po])
                fw.op("pe", lambda: nc.tensor.matmul(out=po[:, h, :], lhsT=q0[rr, p, :], rhs=s0[p][0][rr, cc], start=False, stop=False),
                      reads=[t_q0, s0[p][1]], writes=[t_po])
                fw.op("pe", lambda: nc.tensor.matmul(out=po[:, h, :], lhsT=q1[rr, p, :], rhs=s1[p][0][rr, cc], start=False, stop=True),
                      reads=[t_q1, s1[p][1]], writes=[t_po])
            if i + 1 < NT:
                sb_ = list(state_update(1))
            os_, t_os = Ros.next()
            fw.op("act", lambda: nc.scalar.copy(out=os_[:], in_=po[:]), reads=[t_po], writes=[t_os])
            sq, t_sq = Rsq.next()
            fw.op("pool", lambda: nc.gpsimd.tensor_tensor(out=sq[:], in0=os_[:], in1=os_[:], op=ALU.mult), reads=[t_os], writes=[t_sq])
            v4, t_v4 = Rs4.next()
            fw.op("dve", lambda: nc.vector.tensor_reduce(out=v4[:], in_=sq[:], axis=AX.X, op=ALU.add), reads=[t_sq], writes=[t_v4])
            fw.op("act", lambda: nc.scalar.activation(out=v4[:], in_=v4[:], func=AF.Sqrt, scale=1.0 / 64, bias=EPS), reads=[t_v4], writes=[t_v4])
            fw.op("dve", lambda: nc.vector.reciprocal(out=v4[:], in_=v4[:]), reads=[t_v4], writes=[t_v4])
            fw.op("dve", lambda: nc.vector.tensor_tensor(out=os_[:], in0=os_[:], in1=bview(v4[:], [128, 4, 64]), op=ALU.mult), reads=[t_os, t_v4], writes=[t_os])
            o2 = os_[:].rearrange("p h e -> p (h e)")
            fw.op("pool", lambda: nc.gpsimd.tensor_tensor(out=o2, in0=o2, in1=hnw[:], op=ALU.mult), reads=[t_os, t_hnw], writes=[t_os])
            yb, t_yb = Ryb.next()
            fw.op("dve", lambda: nc.vector.tensor_tensor(out=yb[:], in0=o2, in1=gh[:], op=ALU.mult), reads=[t_os, t_gh], writes=[t_yb])
            fw.dma("sp", K.Y[cs, 768:1024], yb[:], reads=[t_yb], writes=[K.t_Y[g]])


def attn_phase(K, l):
    nc, fw = K.nc, K.fw
    cf = K.constf
    lam_init = 0.8 - 0.6 * math.exp(-0.3 * l)
    with Phase(K) as ph:
        lv, t_lv = load_row(K, ph, "lv", K.dlam[l], 256)
        dnw, t_dnw = load_row(K, ph, "dnw", K.d_nw[l], 512)
        rb, t_rb = load_row(K, ph, "rb", K.rel_bias, 128)
        j64 = ph.sb("j64", [128, 2, 64], F32)
        s12 = ph.sb("s12", [128, 2], F32)
        nlam = ph.sb("nlam", [128, 1], F32)
        t_lam = Trk()
        lv4 = lv[:].rearrange("p (a b) -> p a b", a=4)
        fw.op("dve", lambda: nc.vector.tensor_tensor(out=j64[:, 0, :], in0=lv4[:, 0, :], in1=lv4[:, 1, :], op=ALU.mult), reads=[t_lv], writes=[t_lam])
        fw.op("dve", lambda: nc.vector.tensor_tensor(out=j64[:, 1, :], in0=lv4[:, 2, :], in1=lv4[:, 3, :], op=ALU.mult), reads=[t_lv, t_lam], writes=[t_lam])
        fw.op("dve", lambda: nc.vector.tensor_reduce(out=s12[:], in_=j64[:], axis=AX.X, op=ALU.add), reads=[t_lam], writes=[t_lam])
        fw.op("act", lambda: nc.scalar.activation(out=s12[:], in_=s12[:], func=AF.Exp), reads=[t_lam], writes=[t_lam])
        fw.op("dve", lambda: nc.vector.tensor_tensor(out=nlam[:], in0=s12[:, 1:2], in1=s12[:, 0:1], op=ALU.subtract), reads=[t_lam], writes=[t_lam])
        fw.op("dve", lambda: nc.vector.tensor_scalar(out=nlam[:], in0=nlam[:], scalar1=-lam_init, scalar2=None, op0=ALU.add), reads=[t_lam], writes=[t_lam])
        fw.op("dve", lambda: nc.vector.tensor_scalar(out=dnw[:], in0=dnw[:], scalar1=1.0 - lam_init, scalar2=None, op0=ALU.mult), reads=[t_dnw], writes=[t_dnw])
        et = ph.sb("et", [128, 128], F32)
        t_et = Trk()
        fw.op("act", lambda: nc.scalar.activation(out=et[:], in_=rb[:], func=AF.Exp), reads=[t_rb], writes=[t_et])
        EE = ph.sb("EE", [128, 4, 768], F32)
        t_EE = Trk()
        etmp = ph.sb("etmp", [128, 768], F32)
        t_etmp = Trk()
        fw.op("dve", lambda: nc.vector.memset(EE[:], 0.0), writes=[t_EE])
        for b in range(32):
            w = 768 if b == 31 else 256
            for h in range(4):
                fw.op("dve", lambda: nc.vector.tensor_scalar(out=etmp[:, 0:w], in0=cf[:, C_EEI:C_EEI + w], scalar1=float(b), scalar2=et[:, b * 4 + h:b * 4 + h + 1],
                                                             op0=ALU.is_equal, op1=ALU.mult),
                      reads=[K.t_const, t_et, t_etmp], writes=[t_etmp])
                fw.op("dve", lambda: nc.vector.tensor_tensor(out=EE[:, h, 0:w], in0=EE[:, h, 0:w], in1=etmp[:, 0:w], op=ALU.add),
                      reads=[t_etmp, t_EE], writes=[t_EE])
        sets = []
        for r in range(2):
            QT = ph.sb("aQT", [128, S], BF16)
            KT = ph.sb("aKT", [128, S], BF16)
            V = ph.sb("aV", [128, NT, 130], BF16)
            tr = Trk()
            fw.op("pool", lambda: nc.gpsimd.memset(V[:], 1.0), writes=[tr])
            sets.append((QT, KT, V, tr))

        def load_head(h):
            QT, KT, V, tr = sets[h % 2]
            for g in range(NG):
                cs = slice(g * 512, (g + 1) * 512)
                fw.dma("sp", QT[:, cs], K.QD[h * 128:(h + 1) * 128, cs], reads=[K.t_QD[g]], writes=[tr])
                fw.dma("sp", KT[:, cs], K.KD[h * 128:(h + 1) * 128, cs], reads=[K.t_KD[g]], writes=[tr])
                fw.dma("sp", V[:, g * 4:(g + 1) * 4, 0:128], K.VD[cs, h * 128:(h + 1) * 128].rearrange("(c p) e -> p c e", p=128),
                       reads=[K.t_VD[g]], writes=[tr])
        Rs1 = Rot(ph, "as1", [128, 512], F32, 2, psum=True)
        Rs2 = Rot(ph, "as2", [128, 512], F32, 2, psum=True)
        PO = [(ph.ps("aPO", [128, 3, 129], F32), Trk(True)) for _ in range(3)]
        Rp1 = Rot(ph, "ap1", [128, 512], BF16, 3)
        Rp2 = Rot(ph, "ap2", [128, 512], BF16, 3)
        Re = Rot(ph, "ae", [128, 512], F32, 3)
        Rr = Rot(ph, "ar", [128, 2], F32, 4)
        Rt1 = Rot(ph, "at1", [128, 128], F32, 2)
        Rod = Rot(ph, "aod", [128, 128], F32, 2)
        Rsq = Rot(ph, "asq", [128, 128], F32, 2)
        Rss = Rot(ph, "ass", [128, 1], F32, 4)
        Ryb = Rot(ph, "ayb", [128, 128], BF16, 3)

        def acc(j, which):
            idx = which * 4 + j
            po, tp = PO[idx // 3]
            return po[:, idx % 3, :], tp

        load_head(0)
        for h in range(4):
            if h + 1 < 4:
                load_head(h + 1)
            QT, KT, V, t_hd = sets[h % 2]
            tiles = [(Q, kb) for Q in range(8) for kb in range(4 * Q + 4)]

            def S_step(Q, kb):
                j0 = kb - 4 * Q
                jlo = max(j0, 0)
                c0 = jlo * 128
                ks = slice(kb * 128, (kb + 1) * 128)
                qs = slice(Q * 512 + c0, (Q + 1) * 512)
                s1, t_s1 = Rs1.next()
                s2, t_s2 = Rs2.next()
                fw.op("pe", lambda: nc.tensor.matmul(out=s1[:, c0:512], lhsT=KT[0:64, ks], rhs=QT[0:64, qs], start=True, stop=True),
                      reads=[t_hd], writes=[t_s1])
                fw.op("pe", lambda: nc.tensor.matmul(out=s2[:, c0:512], lhsT=KT[64:128, ks], rhs=QT[64:128, qs], start=True, stop=True),
                      reads=[t_hd], writes=[t_s2])
                P1, t_P1 = Rp1.next()
                P2, t_P2 = Rp2.next()
                for (sx, t_sx, Px, t_Px, eng) in ((s1, t_s1, P1, t_P1, "dve"), (s2, t_s2, P2, t_P2, "pool")):
                    if j0 <= -2:
                        fw.op("act", lambda: nc.scalar.activation(out=Px[:], in_=sx[:], func=AF.Exp, scale=0.125, bias=rb[:, 124 + h:125 + h]),
                              reads=[t_sx, t_rb], writes=[t_Px])
                    else:
                        e, t_e = Re.next()
                        eoff = (jlo - j0) * 128
                        ncol = 512 - c0
                        fw.op("act", lambda: nc.scalar.activation(out=e[:, c0:512], in_=sx[:, c0:512], func=AF.Exp, scale=0.125), reads=[t_sx], writes=[t_e])
                        if eng == "dve":
                            fw.op("dve", lambda: nc.vector.tensor_tensor(out=Px[:, c0:512], in0=e[:, c0:512], in1=EE[:, h, eoff:eoff + ncol], op=ALU.mult),
                                  reads=[t_e, t_EE], writes=[t_Px])
                        else:
                            fw.op("pool", lambda: nc.gpsimd.tensor_tensor(out=Px[:, c0:512], in0=e[:, c0:512], in1=EE[:, h, eoff:eoff + ncol], op=ALU.mult),
                                  reads=[t_e, t_EE], writes=[t_Px])
                return (Q, kb, jlo, P1, t_P1, P2, t_P2)

            def A_step(info):
                Q, kb, jlo, P1, t_P1, P2, t_P2 = info
                if kb == 0:
                    for po, tp in PO:
                        fw.op("dve", lambda: nc.vector.memset(po[:], 0.0), writes=[tp])
                for j in range(jlo, 4):
                    for which, (Px, t_Px) in enumerate(((P1, t_P1), (P2, t_P2))):
                        o, t_o = acc(j, which)
                        fw.op("pe", lambda: nc.tensor.matmul(out=o, lhsT=Px[:, j * 128:(j + 1) * 128], rhs=V[:, kb, 0:129], start=False, stop=False,
                                                             skip_group_check=True),
                              reads=[t_Px, t_hd], writes=[t_o])

            def F_step(Q):
                for j in range(4):
                    i = 4 * Q + j
                    o1, t_o1 = acc(j, 0)
                    o2, t_o2 = acc(j, 1)
                    r, t_r = Rr.next()
                    fw.op("dve", lambda: nc.vector.reciprocal(out=r[:, 0:1], in_=o1[:, 128:129]), reads=[t_o1], writes=[t_r])
                    fw.op("dve", lambda: nc.vector.reciprocal(out=r[:, 1:2], in_=o2[:, 128:129]), reads=[t_o2, t_r], writes=[t_r])
                    fw.op("dve", lambda: nc.vector.tensor_tensor(out=r[:, 1:2], in0=r[:, 1:2], in1=nlam[:], op=ALU.mult), reads=[t_r, t_lam], writes=[t_r])
                    t1, t_t1 = Rt1.next()
                    fw.op("dve", lambda: nc.vector.tensor_scalar(out=t1[:], in0=o1[:, 0:128], scalar1=r[:, 0:1], scalar2=None, op0=ALU.mult),
                          reads=[t_o1, t_r], writes=[t_t1])
                    od, t_od = Rod.next()
                    fw.op("dve", lambda: nc.vector.scalar_tensor_tensor(out=od[:], in0=o2[:, 0:128], scalar=r[:, 1:2], in1=t1[:], op0=ALU.mult, op1=ALU.add),
                          reads=[t_o2, t_r, t_t1], writes=[t_od])
                    sq, t_sq = Rsq.next()
                    fw.op("pool", lambda: nc.gpsimd.tensor_tensor(out=sq[:], in0=od[:], in1=od[:], op=ALU.mult), reads=[t_od], writes=[t_sq])
                    ss, t_ss = Rss.next()
                    fw.op("dve", lambda: nc.vector.tensor_reduce(out=ss[:], in_=sq[:], axis=AX.X, op=ALU.add), reads=[t_sq], writes=[t_ss])
                    fw.op("act", lambda: nc.scalar.activation(out=ss[:], in_=ss[:], func=AF.Ln, scale=1.0 / 128, bias=EPS), reads=[t_ss], writes=[t_ss])
                    fw.op("act", lambda: nc.scalar.activation(out=ss[:], in_=ss[:], func=AF.Exp, scale=-0.5), reads=[t_ss], writes=[t_ss])
                    yb, t_yb = Ryb.next()
                    fw.op("dve", lambda: nc.vector.scalar_tensor_tensor(out=yb[:], in0=od[:], scalar=ss[:], in1=dnw[:, h * 128:(h + 1) * 128], op0=ALU.mult, op1=ALU.mult),
                          reads=[t_od, t_ss, t_dnw], writes=[t_yb])
                    fw.dma("sp", K.Y[i * 128:(i + 1) * 128, 256 + h * 128:256 + (h + 1) * 128], yb[:], reads=[t_yb], writes=[K.t_Y[i // 4]])

            prev = None
            for (Q, kb) in tiles:
                info = S_step(Q, kb)
                if prev is not None:
                    A_step(prev)
                    if prev[1] == 4 * prev[0] + 3:
                        F_step(prev[0])
                prev = info
            A_step(prev)
            F_step(prev[0])


KCtx.attn_phase = staticmethod(attn_phase)


def mixers(K, l, stop_after):
    import os
    MX = os.environ.get("MIXERS", "MHD")
    K.fw.pe_sync = True
    if "M" in MX:
        mlstm_phase(K, l)
    if "H" in MX:
        hgrn_phase(K, l)
    K.fw.pe_sync = False
    if "D" in MX and hasattr(K, "attn_phase"):
        K.attn_phase(K, l)


KCtx.mixers = staticmethod(mixers)


def build(n_layers=DEPTH, debug=False, stop_after=None):
    nc = bass.Bass("TRN2", target_bir_lowering=False)
    K = KCtx()
    K.nc = nc
    dt = nc.dram_tensor
    K.x = dt("x", [S, D], F32, kind="ExternalInput").ap()
    K.norm_w = dt("norm_w", [DEPTH, 6, D], F32, kind="ExternalInput").ap()
    K.ffn1_wi = dt("ffn1_wi", [DEPTH, D, 2 * DFF], F32, kind="ExternalInput").ap()
    K.ffn1_wo = dt("ffn1_wo", [DEPTH, DFF, D], F32, kind="ExternalInput").ap()
    K.ffn2_wi = dt("ffn2_wi", [DEPTH, D, 2 * DFF], F32, kind="ExternalInput").ap()
    K.ffn2_wo = dt("ffn2_wo", [DEPTH, DFF, D], F32, kind="ExternalInput").ap()
    K.w_in = dt("w_in", [DEPTH, D, NIN], F32, kind="ExternalInput").ap()
    K.w_out = dt("w_out", [DEPTH, D, D], F32, kind="ExternalInput").ap()
    K.consts = dt("consts", [128, NCONST], F32, kind="ExternalInput").ap()
    K.pp = dt("pp", [DEPTH, 128, 28], F32, kind="ExternalInput").ap()
    K.ig_b = dt("mlstm_igate_b", [DEPTH, 4], F32, kind="ExternalInput").ap()
    K.fg_b = dt("mlstm_fgate_b", [DEPTH, 4], F32, kind="ExternalInput").ap()
    K.m_nw = dt("mlstm_norm_w", [DEPTH, 256], F32, kind="ExternalInput").ap()
    K.dlam = dt("diff_lambda", [DEPTH, 256], F32, kind="ExternalInput").ap()
    K.d_nw = dt("diff_norm_w", [DEPTH, 512], F32, kind="ExternalInput").ap()
    K.rel_bias = dt("rel_bias", [128], F32, kind="ExternalInput").ap()
    K.lb_logits = dt("hgrn_lb_logits", [DEPTH, 256], F32, kind="ExternalInput").ap()
    K.h_nw = dt("hgrn_norm_w", [DEPTH, 256], F32, kind="ExternalInput").ap()
    K.out = dt("out", [S, D], F32, kind="ExternalOutput").ap()
    dbg = set(debug) if debug else set()

    def scratch(name, shape, dtp):
        kind = "ExternalOutput" if name in dbg else "Internal"
        ap = dt(name, shape, dtp, kind=kind).ap()
        return ap, [Trk() for _ in range(NG)]
    K.X, K.t_X = scratch("Xs", [S, D], F32)
    K.Y, K.t_Y = scratch("Ys", [S, D], BF16)
    K.QKM, K.t_QKM = scratch("QKM", [512, S], BF16)
    K.KMT, K.t_KMT = scratch("KMT", [S, 256], BF16)
    K.VM, K.t_VM = scratch("VM", [S, 256], BF16)
    K.OM, K.t_OM = scratch("OM", [S, 256], F32)
    K.GM, K.t_GM = scratch("GM", [S, 8], F32)
    K.QD, K.t_QD = scratch("QD", [512, S], BF16)
    K.KD, K.t_KD = scratch("KD", [512, S], BF16)
    K.VD, K.t_VD = scratch("VD", [S, 512], BF16)
    K.QH, K.t_QH = scratch("QH", [256, S], BF16)
    K.KH, K.t_KH = scratch("KH", [256, S], BF16)
    K.LFH, K.t_LFH = scratch("LFH", [S, 256], F32)
    K.KHT, K.t_KHT = scratch("KHT", [S, 256], F32)
    K.VH, K.t_VH = scratch("VH", [S, 256], BF16)
    K.GH, K.t_GH = scratch("GH", [S, 256], F32)
    K.t_x = [Trk() for _ in range(NG)]
    K.t_out = [Trk() for _ in range(NG)]
    with ExitStack() as st:
        fw = FW(nc, st)
        K.fw = fw
        K.constf = st.enter_context(nc.sbuf_tensor("constf", [128, NCONST], F32))
        K.identb = st.enter_context(nc.sbuf_tensor("identb", [128, 128], BF16))
        K.t_const = Trk()
        fw.dma("sp", K.constf[:], K.consts, writes=[K.t_const])
        fw.op("dve", lambda: nc.vector.tensor_copy(out=K.identb[:], in_=K.constf[:, C_IDENT:C_IDENT + 128]),
              reads=[K.t_const], writes=[K.t_const])
        for l in range(n_layers):
            xin, t_xin = (K.x, K.t_x) if l == 0 else (K.X, K.t_X)
            import os
            if os.environ.get('SIM_PROJ_ONLY'):
                proj_phase(K, l, K.x, K.t_x)
                if stop_after[1] >= 3:
                    K.mixers(K, l, stop_after)
                break
            token_phase(K, l, 1, xin, t_xin, K.X, K.t_X)
            if stop_after == (l, 1):
                break
            proj_phase(K, l)
            if stop_after == (l, 2):
                break
            if hasattr(K, "mixers"):
                K.mixers(K, l, stop_after)
            if stop_after is not None and stop_after[0] == l and stop_after[1] == 3:
                break
            last = (l == n_layers - 1)
            token_phase(K, l, 2, K.X, K.t_X, K.out if last else K.X, K.t_out if last else K.t_X)
            if stop_after == (l, 4):
                break
        fw.barrier()
    K.counts = {k: v.count for k, v in fw.engs.items()}
    print("instr counts", K.counts, "dmas", fw.dma_count)
    return nc


def make_in_map(inputs, c, consts):
    f = lambda a: np.ascontiguousarray(a, dtype=np.float32)
    m = {"x": f(inputs["x"][c]), "consts": consts}
    for k in ["norm_w", "ffn1_wi", "ffn1_wo", "ffn2_wi", "ffn2_wo", "w_in", "w_out", "mlstm_igate_b", "mlstm_fgate_b",
              "mlstm_norm_w", "diff_norm_w", "hgrn_lb_logits", "hgrn_norm_w"]:
        m[k] = f(inputs[k])
    m["diff_lambda"] = f(inputs["diff_lambda"]).reshape(DEPTH, 256)
    m["rel_bias"] = f(inputs["rel_bias"]).reshape(128)
    pp = np.zeros((DEPTH, 128, 28), np.float32)
    cw = f(inputs["mlstm_conv_w"])
    cb = f(inputs["mlstm_conv_b"])
    lg = f(inputs["hgrn_lb_logits"])
    for l in range(DEPTH):
        pp[l, :, 0:16] = cw[l].reshape(4, 4, 128).transpose(2, 1, 0).reshape(128, 16)
        pp[l, :, 16:20] = cb[l].reshape(4, 128).T
        pp[l, :, 20:28] = lg.reshape(DEPTH, 2, 128).transpose(2, 1, 0).reshape(128, 8)
    m["pp"] = pp
    return m


_NC_CACHE = {}


def kernel(**inputs):
    nc = _NC_CACHE.get("nc")
    if nc is None:
        nc = build()
        _NC_CACHE["nc"] = nc
    consts = make_consts()
    in_maps = [make_in_map(inputs, c, consts) for c in range(8)]
    res = run_bass_kernel_spmd(nc, in_maps, core_ids=list(range(8)))
    return np.stack([res.results[c]["out"] for c in range(8)], axis=0)
```

```python
import math
import os
import numpy as np
from contextlib import ExitStack
import concourse.bass as bass
import concourse.mybir as mybir
from concourse.bass_utils import run_bass_kernel_spmd

F32 = mybir.dt.float32
BF16 = mybir.dt.bfloat16
AF = mybir.ActivationFunctionType
ALU = mybir.AluOpType
AX = mybir.AxisListType

S = 4096
D = 1024
DFF = 2816
NIN = 3592
NT = 32
NG = 8
DEPTH = 4
EPS = 1e-6
LN8 = math.log(0.125)


class Trk:
    __slots__ = ("w", "r", "psum")

    def __init__(self, psum=False):
        self.w = None
        self.r = []
        self.psum = psum


class Eng:
    def __init__(self, name, raw, sem):
        self.name = name
        self.raw = raw
        self.sem = sem
        self.count = 0
        self.waited = {}


class FW:
    NDMA = 8

    def __init__(self, nc, stack):
        self.nc = nc
        self.stack = stack
        self.engs = {}
        for name, raw in [("pe", nc.tensor), ("dve", nc.vector), ("act", nc.scalar),
                          ("pool", nc.gpsimd), ("sp", nc.sync)]:
            sem = stack.enter_context(nc.semaphore("sem_" + name))
            self.engs[name] = Eng(name, raw, sem)
        self.dma_sems = {}
        self.dma_count = {}
        for q in ["sp", "pool", "act"]:
            self.dma_sems[q] = [stack.enter_context(nc.semaphore("dsem_%s_%d" % (q, i))) for i in range(self.NDMA)]
            self.dma_count[q] = 0
        self.uid = 0
        self.pe_sync = False

    def name(self, base):
        self.uid += 1
        return "%s_%d" % (base, self.uid)

    def _deps(self, e, reads, writes):
        deps = {}

        def add(d):
            if d is None:
                return
            sem, val, en = d
            if en == "pe" and e.name == "pe" and not self.pe_sync:
                return
            k = id(sem)
            if e.waited.get(k, 0) >= val:
                return
            if k not in deps or deps[k][1] < val:
                deps[k] = (sem, val)
        for t in reads:
            add(t.w)
        for t in writes:
            add(t.w)
            for r in t.r:
                add(r)
        return list(deps.values())

    def _apply_waits(self, e, deps, instr_fn):
        for sem, val in deps[:-1]:
            e.raw.wait_ge(sem, val)
            e.waited[id(sem)] = val
        ins = instr_fn()
        if deps:
            sem, val = deps[-1]
            ins.wait_op(sem, val, "sem-ge")
            e.waited[id(sem)] = val
        return ins

    def op(self, eng, fn, reads=(), writes=()):
        e = self.engs[eng]
        pr = [t for t in reads if t.psum]
        if pr:
            reads = [t for t in reads if not t.psum]
            writes = list(writes) + pr
        deps = self._deps(e, reads, writes)
        ins = self._apply_waits(e, deps, fn)
        e.count += 1
        ins.then_inc(e.sem, 1)
        tag = (e.sem, e.count, e.name)
        for t in reads:
            t.r.append(tag)
        for t in writes:
            t.w = tag
            t.r = []
        return ins

    def dma(self, q, out, in_, reads=(), writes=(), **kw):
        e = self.engs[q]
        i = self.dma_count[q]
        self.dma_count[q] = i + 1
        sem = self.dma_sems[q][i % self.NDMA]
        val = 16 * (i // self.NDMA + 1)
        deps = self._deps(e, reads, writes)
        if i >= self.NDMA:
            pv = val - 16
            if e.waited.get(id(sem), 0) < pv:
                mx = max([pv] + [d[1] for d in deps if d[0] is sem])
                deps = [d for d in deps if d[0] is not sem] + [(sem, mx)]
        ins = self._apply_waits(e, deps, lambda: e.raw.dma_start(out=out, in_=in_, **kw))
        ins.then_inc(sem, 16)
        tag = (sem, val, "dma_" + q)
        for t in reads:
            t.r.append(tag)
        for t in writes:
            t.w = tag
            t.r = []
        return ins

    def barrier(self):
        targets = [(e.sem, e.count) for e in self.engs.values() if e.count > 0]
        for q in self.dma_sems:
            n = self.dma_count[q]
            for j, sem in enumerate(self.dma_sems[q]):
                cnt = (n - j + self.NDMA - 1) // self.NDMA if n > j else 0
                if cnt > 0:
                    targets.append((sem, 16 * cnt))
        for e in self.engs.values():
            for sem, val in targets:
                if sem is e.sem and e.name == "pe":
                    continue
                if e.waited.get(id(sem), 0) < val:
                    e.raw.wait_ge(sem, val)
                    e.waited[id(sem)] = val


class Phase:
    def __init__(self, K):
        self.K = K
        self.st = ExitStack()

    def __enter__(self):
        self.st.__enter__()
        return self

    def __exit__(self, *a):
        self.K.fw.barrier()
        return self.st.__exit__(*a)

    def sb(self, name, shape, dt):
        return self.st.enter_context(self.K.nc.sbuf_tensor(self.K.fw.name(name), list(shape), dt))

    def ps(self, name, shape, dt):
        full = 512 if dt == F32 else 1024
        t = self.st.enter_context(self.K.nc.psum_tensor(self.K.fw.name(name), [128, full], dt))
        n = 1
        for d in shape[1:]:
            n *= d
        assert shape[0] == 128 and n <= full
        v = t[:, 0:n]
        if len(shape) == 3:
            v = v.rearrange("p (a b) -> p a b", a=shape[1])
        return v


class Rot:
    def __init__(self, ph, name, shape, dt, n, psum=False):
        mk = ph.ps if psum else ph.sb
        self.bufs = [(mk(name, shape, dt), Trk(psum)) for _ in range(n)]
        self.i = 0

    def next(self):
        b = self.bufs[self.i % len(self.bufs)]
        self.i += 1
        return b


def t5_bucket_np(rel):
    n = np.maximum(rel, 0)
    nf = np.maximum(n, 1).astype(np.float32)
    large = 16 + (np.log(nf / np.float32(16)) / np.float32(math.log(128 / 16)) * np.float32(16)).astype(np.int32)
    large = np.minimum(large, 31)
    return np.where(n < 16, n, large)


C_IDENT, C_TRIT, C_TRIU, C_ONES, C_MASK, C_TRITBD, C_TRIMID, C_TRIUBD, C_MASKBD = [i * 128 for i in range(9)]
C_CHI = 9 * 128
C_EEI = C_CHI + 2
NCONST = C_EEI + 768


def make_consts():
    c = np.zeros((128, NCONST), np.float32)
    r = np.arange(128)[:, None]
    t = np.arange(128)[None, :]
    same = (r // 64) == (t // 64)
    c[:, C_IDENT:C_IDENT + 128] = (r == t)
    c[:, C_TRIT:C_TRIT + 128] = (r <= t)
    c[:, C_TRIU:C_TRIU + 128] = (r > t)
    c[:, C_ONES:C_ONES + 128] = 1.0
    c[:, C_MASK:C_MASK + 128] = (r <= t)
    c[:, C_TRITBD:C_TRITBD + 128] = (r <= t) & same
    mid = (t // 64) * 64 + 31
    c[:, C_TRIMID:C_TRIMID + 128] = (((r <= t).astype(np.float32) - (r <= mid).astype(np.float32)) * same)
    c[:, C_TRIUBD:C_TRIUBD + 128] = (r > t) & same
    c[:, C_MASKBD:C_MASKBD + 128] = (r <= t) & same
    c[:, C_CHI] = (np.arange(128) < 64)
    c[:, C_CHI + 1] = (np.arange(128) >= 64)
    k = np.arange(128)[:, None]
    col = np.arange(128)[None, :]
    rel0 = col - k
    idx0 = np.where(rel0 >= 0, t5_bucket_np(rel0), -1)
    idx1 = t5_bucket_np(128 + col - k)
    c[:, C_EEI:C_EEI + 128] = idx0
    c[:, C_EEI + 128:C_EEI + 256] = idx1
    c[:, C_EEI + 256:C_EEI + 768] = 31
    return c


class KCtx:
    pass


def rms_rstd(K, ph, src_ap, src_trk, n, junk, rstd, eps=EPS):
    nc, fw = K.nc, K.fw
    jb, jt = junk
    rb, rt = rstd
    fw.op("act", lambda: nc.scalar.activation(out=jb, in_=src_ap, func=AF.Square, accum_out=rb),
          reads=[src_trk], writes=[jt, rt])
    fw.op("act", lambda: nc.scalar.activation(out=rb, in_=rb, func=AF.Sqrt, scale=1.0 / n, bias=eps),
          reads=[rt], writes=[rt])
    fw.op("dve", lambda: nc.vector.reciprocal(out=rb, in_=rb), reads=[rt], writes=[rt])


def load_row(K, ph, name, src_1d, n, q="sp"):
    t = ph.sb(name, [128, n], F32)
    tr = Trk()
    K.fw.dma(q, t[:], src_1d.partition_broadcast(128), writes=[tr])
    return t, tr


def norm_transpose_group(K, ph, g, xg, t_xg, nwrow, t_nw, xnT, t_xnT, R):
    nc, fw = K.nc, K.fw
    for i in range(4):
        junk = R["junk"].next()
        rstd = R["rstd"].next()
        rms_rstd(K, ph, xg[:, i, :], t_xg[i], D, (junk[0][:], junk[1]), (rstd[0][:], rstd[1]))
        xn, t_xn = R["xn"].next()
        fw.op("dve", lambda: nc.vector.scalar_tensor_tensor(out=xn[:], in0=xg[:, i, :], scalar=rstd[0][:], in1=nwrow[:],
                                                            op0=ALU.mult, op1=ALU.mult),
              reads=[t_xg[i], rstd[1], t_nw], writes=[t_xn])
        pT, t_pT = R["pT"].next()
        for k in range(8):
            fw.op("pe", lambda: nc.tensor.transpose(out=pT[:, k, :], in_=xn[:, k * 128:(k + 1) * 128], identity=K.identb[:]),
                  reads=[t_xn, K.t_const], writes=[t_pT])
        fw.op("act", lambda: nc.scalar.copy(out=xnT[:, :, i * 128:(i + 1) * 128], in_=pT[:]), reads=[t_pT], writes=[t_xnT])


def convert_wi(K, l, which):
    fw = K.fw
    slot = (which - 1) % 2
    wi = (K.ffn1_wi if which == 1 else K.ffn2_wi)[l]
    wi_v = wi.rearrange("(k p) n -> p k n", p=128)
    for j in range(11):
        dst = K.WIB[slot, j].rearrange("p (k a n) -> p k a n", k=8, a=2)
        fw.dma("pool", dst[:, :, 0, :], wi_v[:, :, j * 256:(j + 1) * 256], writes=[K.t_WIB[slot][j]])
        fw.dma("pool", dst[:, :, 1, :], wi_v[:, :, DFF + j * 256:DFF + (j + 1) * 256], writes=[K.t_WIB[slot][j]])
    K.wib_ready[(l, which)] = slot


def token_phase(K, l, which, xin, t_xin, xout, t_xout):
    nc, fw = K.nc, K.fw
    wi = (K.ffn1_wi if which == 1 else K.ffn2_wi)[l]
    wo = (K.ffn1_wo if which == 1 else K.ffn2_wo)[l]
    npre, npost = (0, 1) if which == 1 else (4, 5)
    with Phase(K) as ph:
        nw_pre, t_nwpre = load_row(K, ph, "nwpre", K.norm_w[l, npre], D)
        nw_post, t_nwpost = load_row(K, ph, "nwpost", K.norm_w[l, npost], D)
        wo_sb = ph.sb("wo", [128, 22, D], BF16)
        t_wo = Trk()
        wo_v = wo.rearrange("(k p) n -> p k n", p=128)
        for kk in range(0, 22, 2):
            fw.dma("pool", wo_sb[:, kk:kk + 2, :], wo_v[:, kk:kk + 2, :], writes=[t_wo])
        if which == 2:
            nw3, t_nw3 = load_row(K, ph, "nw3", K.norm_w[l, 3], D)
            wout_sb = ph.sb("wout", [128, 8, D], BF16)
            t_wout = Trk()
            wout_v = K.w_out[l].rearrange("(k p) n -> p k n", p=128)
            for kk in range(0, 8, 2):
                fw.dma("pool", wout_sb[:, kk:kk + 2, :], wout_v[:, kk:kk + 2, :], writes=[t_wout])
            Ry = Rot(ph, "ytile", [128, D], BF16, 2)
            RyT = Rot(ph, "yT", [128, 8, 128], BF16, 2)
        R = {"junk": Rot(ph, "junk", [128, D], F32, 1), "rstd": Rot(ph, "rstd", [128, 1], F32, 4),
             "xn": Rot(ph, "xn", [128, D], BF16, 2), "pT": Rot(ph, "pT", [128, 8, 128], BF16, 2, psum=True)}
        nxg = 2 if which == 1 else 1
        xg_bufs = [(ph.sb("xg", [128, 4, D], F32), [Trk() for _ in range(4)]) for _ in range(nxg)]
        xnT = ph.sb("xnT", [128, 8, 512], BF16)
        t_xnT = Trk()
        aT = ph.sb("aT", [128, 22, 512], BF16)
        t_aT = Trk()
        Rw = Rot(ph, "wipiece", [128, 8, 2, 256], BF16, 3)
        Rpg = Rot(ph, "pg", [128, 512], F32, 2, psum=True)
        Rpu = Rot(ph, "pu", [128, 512], F32, 2, psum=True)
        Rpo = Rot(ph, "po", [128, 512], F32, 2, psum=True)
        Rsg = Rot(ph, "sg", [128, 512], F32, 2)
        Rh = Rot(ph, "h", [128, D], F32, 2)
        wi_v = wi.rearrange("(k p) n -> p k n", p=128)

        wslot = K.wib_ready.get((l, which))

        def load_piece(n):
            j = n % 11
            wb, t_wb = Rw.next()
            if wslot is not None:
                fw.dma("sp", wb[:].rearrange("p k a n -> p (k a n)"), K.WIB[wslot, j], reads=[K.t_WIB[wslot][j]], writes=[t_wb])
            else:
                fw.dma("pool", wb[:, :, 0, :], wi_v[:, :, j * 256:(j + 1) * 256], writes=[t_wb])
                fw.dma("pool", wb[:, :, 1, :], wi_v[:, :, DFF + j * 256:DFF + (j + 1) * 256], writes=[t_wb])
            return wb, t_wb

        pieces = {}
        NP = NG * 11
        pieces[0] = load_piece(0)
        pieces[1] = load_piece(1)

        def load_x(g):
            xg, t_tiles = xg_bufs[g % nxg]
            for i in range(4):
                r0 = g * 512 + i * 128
                fw.dma("sp", xg[:, i, :], xin[r0:r0 + 128, :], reads=[t_xin[g]], writes=[t_tiles[i]])
            return xg, t_tiles

        cur = load_x(0)
        for g in range(NG):
            xg, t_xg = cur
            if which == 2:
                for i in range(4):
                    r0 = g * 512 + i * 128
                    yt, t_yt = Ry.next()
                    fw.dma("sp", yt[:], K.Y[r0:r0 + 128, :], reads=[K.t_Y[g]], writes=[t_yt])
                    pT, t_pT = R["pT"].next()
                    for k in range(8):
                        fw.op("pe", lambda: nc.tensor.transpose(out=pT[:, k, :], in_=yt[:, k * 128:(k + 1) * 128], identity=K.identb[:]),
                              reads=[t_yt, K.t_const], writes=[t_pT])
                    yT, t_yT = RyT.next()
                    fw.op("act", lambda: nc.scalar.copy(out=yT[:], in_=pT[:]), reads=[t_pT], writes=[t_yT])
                    h, t_h = Rh.next()
                    for hf in range(2):
                        po, t_po = Rpo.next()
                        for k in range(8):
                            fw.op("pe", lambda: nc.tensor.matmul(out=po[:], lhsT=yT[:, k, :], rhs=wout_sb[:, k, hf * 512:(hf + 1) * 512],
                                                                 start=(k == 0), stop=(k == 7)),
                                  reads=[t_yT, t_wout], writes=[t_po])
                        fw.op("act", lambda: nc.scalar.copy(out=h[:, hf * 512:(hf + 1) * 512], in_=po[:]), reads=[t_po], writes=[t_h])
                    junk = R["junk"].next()
                    rstd = R["rstd"].next()
                    rms_rstd(K, ph, h[:], t_h, D, (junk[0][:], junk[1]), (rstd[0][:], rstd[1]))
                    fw.op("dve", lambda: nc.vector.scalar_tensor_tensor(out=h[:], in0=h[:], scalar=rstd[0][:], in1=nw3[:],
                                                                        op0=ALU.mult, op1=ALU.mult),
                          reads=[t_h, rstd[1], t_nw3], writes=[t_h])
                    fw.op("dve", lambda: nc.vector.tensor_tensor(out=xg[:, i, :], in0=h[:], in1=xg[:, i, :], op=ALU.add),
                          reads=[t_h, t_xg[i]], writes=[t_xg[i]])
            norm_transpose_group(K, ph, g, xg, t_xg, nw_pre, t_nwpre, xnT, t_xnT, R)
            if g + 1 < NG and nxg == 2:
                cur = load_x(g + 1)
            for j in range(11):
                n = g * 11 + j
                if n + 2 < NP:
                    pieces[n + 2] = load_piece(n + 2)
                wb, t_wb = pieces.pop(n)
                for c in range(2):
                    pg, t_pg = Rpg.next()
                    pu, t_pu = Rpu.next()
                    for k in range(8):
                        fw.op("pe", lambda: nc.tensor.matmul(out=pg[:], lhsT=wb[:, k, 0, c * 128:(c + 1) * 128], rhs=xnT[:, k, :],
                                                             start=(k == 0), stop=(k == 7)),
                              reads=[t_wb, t_xnT], writes=[t_pg])
                    for k in range(8):
                        fw.op("pe", lambda: nc.tensor.matmul(out=pu[:], lhsT=wb[:, k, 1, c * 128:(c + 1) * 128], rhs=xnT[:, k, :],
                                                             start=(k == 0), stop=(k == 7)),
                              reads=[t_wb, t_xnT], writes=[t_pu])
                    sg, t_sg = Rsg.next()
                    fw.op("act", lambda: nc.scalar.activation(out=sg[:], in_=pg[:], func=AF.Silu), reads=[t_pg], writes=[t_sg])
                    fw.op("dve", lambda: nc.vector.tensor_tensor(out=aT[:, j * 2 + c, :], in0=sg[:], in1=pu[:], op=ALU.mult),
                          reads=[t_sg, t_pu], writes=[t_aT])
            for i in range(4):
                r0 = g * 512 + i * 128
                h, t_h = Rh.next()
                for hf in range(2):
                    po, t_po = Rpo.next()
                    for k in range(22):
                        fw.op("pe", lambda: nc.tensor.matmul(out=po[:], lhsT=aT[:, k, i * 128:(i + 1) * 128], rhs=wo_sb[:, k, hf * 512:(hf + 1) * 512],
                                                             start=(k == 0), stop=(k == 21)),
                              reads=[t_aT, t_wo], writes=[t_po])
                    fw.op("act", lambda: nc.scalar.copy(out=h[:, hf * 512:(hf + 1) * 512], in_=po[:]), reads=[t_po], writes=[t_h])
                junk = R["junk"].next()
                rstd = R["rstd"].next()
                rms_rstd(K, ph, h[:], t_h, D, (junk[0][:], junk[1]), (rstd[0][:], rstd[1]))
                fw.op("dve", lambda: nc.vector.scalar_tensor_tensor(out=h[:], in0=h[:], scalar=rstd[0][:], in1=nw_post[:],
                                                                    op0=ALU.mult, op1=ALU.mult),
                      reads=[t_h, rstd[1], t_nwpost], writes=[t_h])
                fw.op("dve", lambda: nc.vector.scalar_tensor_tensor(out=h[:], in0=h[:], scalar=0.5, in1=xg[:, i, :],
                                                                    op0=ALU.mult, op1=ALU.add),
                      reads=[t_h, t_xg[i]], writes=[t_h])
                fw.dma("sp", xout[r0:r0 + 128, :], h[:], reads=[t_h], writes=[t_xout[g]])
            if g + 1 < NG and nxg == 1:
                cur = load_x(g + 1)


def proj_phase(K, l, xsrc=None, t_xsrc=None):
    nc, fw = K.nc, K.fw
    if xsrc is None:
        xsrc, t_xsrc = K.X, K.t_X
    with Phase(K) as ph:
        nw2, t_nw2 = load_row(K, ph, "nw2", K.norm_w[l, 2], D)
        win_sb = ph.sb("win", [128, 8, NIN], BF16)
        t_win = Trk()
        win_v = K.w_in[l].rearrange("(k p) n -> p k n", p=128)
        for k in range(8):
            fw.dma("pool", win_sb[:, k, :], win_v[:, k, :], writes=[t_win])
        if not os.environ.get("NO_WIB"):
            convert_wi(K, l, 2)
            if l + 1 < K.n_layers:
                convert_wi(K, l + 1, 1)
        pp = ph.sb("pp", [128, 28], F32)
        t_pp = Trk()
        fw.dma("sp", pp[:], K.pp[l], writes=[t_pp])
        gb = ph.sb("gb", [128, 8], F32)
        t_gb = Trk()
        fw.dma("sp", gb[:, 0:4], K.ig_b[l].partition_broadcast(128), writes=[t_gb])
        fw.dma("sp", gb[:, 4:8], K.fg_b[l].partition_broadcast(128), writes=[t_gb])
        fw.op("dve", lambda: nc.vector.tensor_scalar(out=gb[:, 0:4], in0=gb[:, 0:4], scalar1=LN8, scalar2=None, op0=ALU.add),
              reads=[t_gb], writes=[t_gb])
        lgr = ph.sb("lgr", [128, 4, 256], F32)
        t_lgr = Trk()
        fw.dma("sp", lgr[:].rearrange("p a b -> p (a b)"), K.lb_logits.rearrange("a b -> (a b)").partition_broadcast(128), writes=[t_lgr])
        fw.op("act", lambda: nc.scalar.activation(out=lgr[:], in_=lgr[:], func=AF.Exp), reads=[t_lgr], writes=[t_lgr])
        lb_row = ph.sb("lb_row", [128, 256], F32)
        oml_row = ph.sb("oml_row", [128, 256], F32)
        tmp_row = ph.sb("tmp_row", [128, 256], F32)
        t_lb = Trk()
        fw.op("dve", lambda: nc.vector.tensor_tensor(out=tmp_row[:], in0=lgr[:, 0, :], in1=lgr[:, 1, :], op=ALU.add), reads=[t_lgr], writes=[t_lb])
        fw.op("dve", lambda: nc.vector.tensor_tensor(out=tmp_row[:], in0=tmp_row[:], in1=lgr[:, 2, :], op=ALU.add), reads=[t_lgr, t_lb], writes=[t_lb])
        fw.op("dve", lambda: nc.vector.tensor_tensor(out=tmp_row[:], in0=tmp_row[:], in1=lgr[:, 3, :], op=ALU.add), reads=[t_lgr, t_lb], writes=[t_lb])
        fw.op("dve", lambda: nc.vector.reciprocal(out=tmp_row[:], in_=tmp_row[:]), reads=[t_lb], writes=[t_lb])
        fw.op("dve", lambda: nc.vector.memset(lb_row[:], 0.0), writes=[t_lb])
        for j in range(1, l + 1):
            fw.op("dve", lambda: nc.vector.tensor_tensor(out=lb_row[:], in0=lb_row[:], in1=lgr[:, j, :], op=ALU.add), reads=[t_lgr, t_lb], writes=[t_lb])
        fw.op("dve", lambda: nc.vector.tensor_tensor(out=lb_row[:], in0=lb_row[:], in1=tmp_row[:], op=ALU.mult), reads=[t_lb], writes=[t_lb])
        fw.op("dve", lambda: nc.vector.tensor_scalar(out=oml_row[:], in0=lb_row[:], scalar1=-1.0, scalar2=1.0, op0=ALU.mult, op1=ALU.add),
              reads=[t_lb], writes=[t_lb])
        lgf = ph.sb("lgf", [128, 2, 4], F32)
        oml_fm = ph.sb("oml_fm", [128, 2], F32)
        tmpf = ph.sb("tmpf", [128, 2], F32)
        t_lf = Trk()
        fw.op("act", lambda: nc.scalar.activation(out=lgf[:], in_=pp[:, 20:28].rearrange("p (a b) -> p a b", a=2), func=AF.Exp),
              reads=[t_pp], writes=[t_lf])
        fw.op("dve", lambda: nc.vector.tensor_reduce(out=tmpf[:], in_=lgf[:], axis=AX.X, op=ALU.add), reads=[t_lf], writes=[t_lf])
        fw.op("dve", lambda: nc.vector.reciprocal(out=tmpf[:], in_=tmpf[:]), reads=[t_lf], writes=[t_lf])
        fw.op("dve", lambda: nc.vector.memset(oml_fm[:], 0.0), writes=[t_lf])
        for j in range(1, l + 1):
            fw.op("dve", lambda: nc.vector.tensor_tensor(out=oml_fm[:], in0=oml_fm[:], in1=lgf[:, :, j], op=ALU.add), reads=[t_lf], writes=[t_lf])
        fw.op("dve", lambda: nc.vector.tensor_tensor(out=oml_fm[:], in0=oml_fm[:], in1=tmpf[:], op=ALU.mult), reads=[t_lf], writes=[t_lf])
        fw.op("dve", lambda: nc.vector.tensor_scalar(out=oml_fm[:], in0=oml_fm[:], scalar1=-1.0, scalar2=1.0, op0=ALU.mult, op1=ALU.add),
              reads=[t_lf], writes=[t_lf])

        R = {"junk": Rot(ph, "junk", [128, D], F32, 1), "rstd": Rot(ph, "rstd", [128, 1], F32, 4),
             "xn": Rot(ph, "xn", [128, D], BF16, 2), "pT": Rot(ph, "pT", [128, 8, 128], BF16, 2, psum=True)}
        xg_bufs = [(ph.sb("xg", [128, 4, D], F32), [Trk() for _ in range(4)]) for _ in range(2)]
        xnT = ph.sb("xnT", [128, 8, 512], BF16)
        t_xnT = Trk()
        xc = ph.sb("xc", [128, 4, 515], F32)
        t_xc = [Trk() for _ in range(4)]
        fw.op("dve", lambda: nc.vector.memset(xc[:], 0.0), writes=t_xc)
        Rpf = Rot(ph, "pf", [128, 512], F32, 2, psum=True)
        Rpt = Rot(ph, "pt", [128, 512], F32, 2, psum=True)
        Rpk = Rot(ph, "pk", [128, 4, 128], BF16, 1, psum=True)
        Racc = Rot(ph, "acc", [128, 512], F32, 2)
        Rob = Rot(ph, "ob", [128, 512], BF16, 3)
        Rkt = Rot(ph, "kt", [128, 4, 128], BF16, 2)
        Rtf = Rot(ph, "tf", [128, 512], F32, 3)
        Rtb = Rot(ph, "tb", [128, 512], BF16, 3)
        Rg8 = Rot(ph, "g8", [128, 8], F32, 2)
        Rg4 = Rot(ph, "g4", [128, 4], F32, 2)

        def load_x(g):
            xg, t_tiles = xg_bufs[g % 2]
            for i in range(4):
                r0 = g * 512 + i * 128
                fw.dma("sp", xg[:, i, :], xsrc[r0:r0 + 128, :], reads=[t_xsrc[g]], writes=[t_tiles[i]])
            return xg, t_tiles

        cur = load_x(0)
        fchunks = [(ci * 128, "qkm", ci) for ci in range(4)] + [(1032 + 128 * h, "qd", h) for h in range(4)] + \
                  [(1544 + 128 * h, "kd", h) for h in range(4)] + [(2568 + 128 * p, "qh", p) for p in range(2)] + \
                  [(2824 + 128 * p, "fh", p) for p in range(2)]
        for g in range(NG):
            xg, t_xg = cur
            norm_transpose_group(K, ph, g, xg, t_xg, nw2, t_nw2, xnT, t_xnT, R)
            if g + 1 < NG:
                cur = load_x(g + 1)
            cs = slice(g * 512, (g + 1) * 512)
            SK = os.environ.get('PROJ_SKIP', '')
            for (c0, kind, ci) in ([] if 'F' in SK else fchunks):
                pf, t_pf = Rpf.next()
                for k in range(8):
                    fw.op("pe", lambda: nc.tensor.matmul(out=pf[:], lhsT=win_sb[:, k, c0:c0 + 128], rhs=xnT[:, k, :],
                                                         start=(k == 0), stop=(k == 7)),
                          reads=[t_win, t_xnT], writes=[t_pf])
                if kind == "qkm":
                    fw.op("act", lambda: nc.scalar.copy(out=xc[:, ci, 3:515], in_=pf[:]), reads=[t_pf], writes=[t_xc[ci]])
                    acc, t_acc = Racc.next()
                    fw.op("dve", lambda: nc.vector.tensor_scalar(out=acc[:], in0=xc[:, ci, 3:515], scalar1=pp[:, ci * 4 + 3:ci * 4 + 4],
                                                                 scalar2=pp[:, 16 + ci:17 + ci], op0=ALU.mult, op1=ALU.add),
                          reads=[t_xc[ci], t_pp], writes=[t_acc])
                    for j in range(3):
                        fw.op("dve", lambda: nc.vector.scalar_tensor_tensor(out=acc[:], in0=xc[:, ci, j:j + 512], scalar=pp[:, ci * 4 + j:ci * 4 + j + 1],
                                                                            in1=acc[:], op0=ALU.mult, op1=ALU.add),
                              reads=[t_xc[ci], t_pp, t_acc], writes=[t_acc])
                    fw.op("dve", lambda: nc.vector.tensor_copy(out=xc[:, ci, 0:3], in_=xc[:, ci, 512:515]), reads=[t_xc[ci]], writes=[t_xc[ci]])
                    ob, t_ob = Rob.next()
                    fw.op("act", lambda: nc.scalar.activation(out=ob[:], in_=acc[:], func=AF.Silu), reads=[t_acc], writes=[t_ob])
                    fw.dma("sp", K.QKM[ci * 128:(ci + 1) * 128, cs], ob[:], reads=[t_ob], writes=[K.t_QKM[g]])
                    if ci >= 2:
                        pk, t_pk = Rpk.next()
                        for i in range(4):
                            fw.op("pe", lambda: nc.tensor.transpose(out=pk[:, i, :], in_=ob[:, i * 128:(i + 1) * 128], identity=K.identb[:]),
                                  reads=[t_ob, K.t_const], writes=[t_pk])
                        kt, t_kt = Rkt.next()
                        fw.op("dve", lambda: nc.vector.tensor_copy(out=kt[:], in_=pk[:]), reads=[t_pk], writes=[t_kt])
                        fw.dma("sp", K.KMT[cs, (ci - 2) * 128:(ci - 1) * 128].rearrange("(i p) c -> p i c", p=128), kt[:],
                               reads=[t_kt], writes=[K.t_KMT[g]])
                elif kind in ("qd", "kd"):
                    ob, t_ob = Rob.next()
                    if ci % 2 == 0:
                        fw.op("act", lambda: nc.scalar.copy(out=ob[:], in_=pf[:]), reads=[t_pf], writes=[t_ob])
                    else:
                        fw.op("dve", lambda: nc.vector.tensor_copy(out=ob[:], in_=pf[:]), reads=[t_pf], writes=[t_ob])
                    dst, tr = (K.QD, K.t_QD) if kind == "qd" else (K.KD, K.t_KD)
                    fw.dma("sp", dst[ci * 128:(ci + 1) * 128, cs], ob[:], reads=[t_ob], writes=[tr[g]])
                elif kind == "qh":
                    ob, t_ob = Rob.next()
                    fw.op("act", lambda: nc.scalar.activation(out=ob[:], in_=pf[:], func=AF.Silu), reads=[t_pf], writes=[t_ob])
                    fw.dma("sp", K.QH[ci * 128:(ci + 1) * 128, cs], ob[:], reads=[t_ob], writes=[K.t_QH[g]])
                else:
                    acc, t_acc = Racc.next()
                    fw.op("act", lambda: nc.scalar.activation(out=acc[:], in_=pf[:], func=AF.Sigmoid, scale=-1.0), reads=[t_pf], writes=[t_acc])
                    ob, t_ob = Rob.next()
                    fw.op("dve", lambda: nc.vector.tensor_scalar(out=ob[:], in0=acc[:], scalar1=oml_fm[:, ci:ci + 1], scalar2=None, op0=ALU.mult),
                          reads=[t_acc, t_lf], writes=[t_ob])
                    fw.dma("sp", K.KH[ci * 128:(ci + 1) * 128, cs], ob[:], reads=[t_ob], writes=[K.t_KH[g]])
            for i in ([] if 'T' in SK else range(4)):
                r0 = g * 512 + i * 128
                rs = slice(r0, r0 + 128)
                lt = xnT

                def tok_mm(c0, n):
                    pt, t_pt = Rpt.next()
                    for k in range(8):
                        fw.op("pe", lambda: nc.tensor.matmul(out=pt[:, 0:n], lhsT=xnT[:, k, i * 128:(i + 1) * 128], rhs=win_sb[:, k, c0:c0 + n],
                                                             start=(k == 0), stop=(k == 7)),
                              reads=[t_win, t_xnT], writes=[t_pt])
                    return pt, t_pt
                BL = os.environ.get('PROJ_BLK', 'ABCDE')
                if 'A' in BL:
                    pt, t_pt = tok_mm(512, 512)
                    tb, t_tb = Rtb.next()
                    fw.op("dve", lambda: nc.vector.tensor_copy(out=tb[:, 0:256], in_=pt[:, 0:256]), reads=[t_pt], writes=[t_tb])
                    fw.dma("sp", K.VM[rs, :], tb[:, 0:256], reads=[t_tb], writes=[K.t_VM[g]])
                    tf, t_tf = Rtf.next()
                    fw.op("act", lambda: nc.scalar.activation(out=tf[:, 0:256], in_=pt[:, 256:512], func=AF.Sigmoid), reads=[t_pt], writes=[t_tf])
                    fw.dma("sp", K.OM[rs, :], tf[:, 0:256], reads=[t_tf], writes=[K.t_OM[g]])
                if 'B' in BL:
                    pt, t_pt = tok_mm(1024, 8)
                    g8, t_g8 = Rg8.next()
                    fw.op("dve", lambda: nc.vector.tensor_tensor(out=g8[:], in0=pt[:, 0:8], in1=gb[:], op=ALU.add), reads=[t_pt, t_gb], writes=[t_g8])
                    g4, t_g4 = Rg4.next()
                    fw.op("act", lambda: nc.scalar.activation(out=g4[:], in_=g8[:, 4:8], func=AF.Exp, scale=-1.0), reads=[t_g8], writes=[t_g4])
                    fw.op("act", lambda: nc.scalar.activation(out=g4[:], in_=g4[:], func=AF.Ln, bias=1.0), reads=[t_g4], writes=[t_g4])
                    fw.op("dve", lambda: nc.vector.tensor_scalar(out=g8[:, 4:8], in0=g4[:], scalar1=-1.0, scalar2=None, op0=ALU.mult),
                          reads=[t_g4, t_g8], writes=[t_g8])
                    fw.dma("sp", K.GM[rs, :], g8[:], reads=[t_g8], writes=[K.t_GM[g]])
                if 'C' in BL:
                    pt, t_pt = tok_mm(2056, 512)
                    tb, t_tb = Rtb.next()
                    fw.op("act", lambda: nc.scalar.copy(out=tb[:], in_=pt[:]), reads=[t_pt], writes=[t_tb])
                    fw.dma("sp", K.VD[rs, :], tb[:], reads=[t_tb], writes=[K.t_VD[g]])
                if 'D' in BL:
                    pt, t_pt = tok_mm(2824, 512)
                    tf, t_tf = Rtf.next()
                    fw.op("act", lambda: nc.scalar.activation(out=tf[:, 0:256], in_=pt[:, 0:256], func=AF.Sigmoid), reads=[t_pt], writes=[t_tf])
                    fw.op("dve", lambda: nc.vector.tensor_tensor(out=tf[:, 0:256], in0=tf[:, 0:256], in1=oml_row[:], op=ALU.mult), reads=[t_tf, t_lb], writes=[t_tf])
                    fw.op("dve", lambda: nc.vector.tensor_tensor(out=tf[:, 0:256], in0=tf[:, 0:256], in1=lb_row[:], op=ALU.add), reads=[t_tf, t_lb], writes=[t_tf])
                    fw.op("dve", lambda: nc.vector.tensor_scalar(out=tf[:, 256:512], in0=tf[:, 0:256], scalar1=-1.0, scalar2=1.0, op0=ALU.mult, op1=ALU.add),
                          reads=[t_tf], writes=[t_tf])
                    fw.op("act", lambda: nc.scalar.activation(out=tf[:, 0:256], in_=tf[:, 0:256], func=AF.Ln), reads=[t_tf], writes=[t_tf])
                    fw.dma("sp", K.LFH[rs, :], tf[:, 0:256], reads=[t_tf], writes=[K.t_LFH[g]])
                    fw.dma("sp", K.KHT[rs, :], tf[:, 256:512], reads=[t_tf], writes=[K.t_KHT[g]])
                    tb, t_tb = Rtb.next()
                    fw.op("dve", lambda: nc.vector.tensor_copy(out=tb[:, 0:256], in_=pt[:, 256:512]), reads=[t_pt], writes=[t_tb])
                    fw.dma("sp", K.VH[rs, :], tb[:, 0:256], reads=[t_tb], writes=[K.t_VH[g]])
                if 'E' in BL:
                    pt, t_pt = tok_mm(3336, 256)
                    tf, t_tf = Rtf.next()
                    fw.op("act", lambda: nc.scalar.activation(out=tf[:, 0:256], in_=pt[:, 0:256], func=AF.Silu), reads=[t_pt], writes=[t_tf])
                    fw.dma("sp", K.GH[rs, :], tf[:, 0:256], reads=[t_tf], writes=[K.t_GH[g]])


def bview(ap, shape):
    return ap.unsqueeze(2).to_broadcast(shape)


def mlstm_phase(K, l):
    nc, fw = K.nc, K.fw
    cf = K.constf
    with Phase(K) as ph:
        qT = ph.sb("mqT", [128, 2, S], BF16)
        kT = ph.sb("mkT", [128, 2, S], BF16)
        ktok = ph.sb("mktok", [128, NT, 256], BF16)
        vaug = ph.sb("mvaug", [128, NT, 4, 66], BF16)
        gm = ph.sb("mgm", [128, NT, 8], F32)
        t_in = [Trk() for _ in range(NG)]
        t_gm = Trk()
        fw.op("pool", lambda: nc.gpsimd.memset(vaug[:], 1.0), writes=t_in)
        for g_ in range(NG):
            fw.dma("sp", gm[:, g_ * 4:(g_ + 1) * 4, :], K.GM[g_ * 512:(g_ + 1) * 512, :].rearrange("(c p) g -> p c g", p=128), reads=[K.t_GM[g_]], writes=[t_gm])
        mnw, t_mnw = load_row(K, ph, "mnw", K.m_nw[l], 256)
        for g in range(NG):
            cs = slice(g * 512, (g + 1) * 512)
            ts = slice(g * 4, (g + 1) * 4)
            for p in range(2):
                fw.dma("sp", qT[:, p, cs], K.QKM[p * 128:(p + 1) * 128, cs], reads=[K.t_QKM[g]], writes=[t_in[g]])
                fw.dma("sp", kT[:, p, cs], K.QKM[256 + p * 128:256 + (p + 1) * 128, cs], reads=[K.t_QKM[g]], writes=[t_in[g]])
            fw.dma("sp", ktok[:, ts, :], K.KMT[cs, :].rearrange("(c p) f -> p c f", p=128), reads=[K.t_KMT[g]], writes=[t_in[g]])
            for c4 in range(4):
                cc_ = g * 4 + c4
                fw.dma("sp", vaug[:, cc_, :, 0:64], K.VM[cc_ * 128:(cc_ + 1) * 128, :].rearrange("p (h e) -> p h e", h=4), reads=[K.t_VM[g]], writes=[t_in[g]])
        lfc = ph.sb("lfc", [128, 128], F32)
        lic = ph.sb("lic", [128, 128], F32)
        eb = ph.sb("eb", [128, 128], F32)
        al = ph.sb("al", [128, 128], F32)
        wa = ph.sb("wa", [128, 128], F32)
        ebL = ph.sb("ebL", [128, 128], F32)
        ebL2 = ph.sb("ebL2", [128, NT, 2], F32)
        t_g = Trk()
        fw.op("dve", lambda: nc.vector.tensor_copy(out=lfc[:].rearrange("p (c h) -> p c h", h=4), in_=gm[:, :, 4:8]), reads=[t_gm], writes=[t_g])
        fw.op("dve", lambda: nc.vector.tensor_copy(out=lic[:].rearrange("p (c h) -> p c h", h=4), in_=gm[:, :, 0:4]), reads=[t_gm], writes=[t_g])
        Rpp = Rot(ph, "mpp", [128, 128], F32, 1, psum=True)
        pp_, t_pp_ = Rpp.next()
        fw.op("pe", lambda: nc.tensor.matmul(out=pp_[:], lhsT=cf[:, C_TRIT:C_TRIT + 128], rhs=lfc[:], start=True, stop=True), reads=[K.t_const, t_g], writes=[t_pp_])
        fw.op("act", lambda: nc.scalar.activation(out=eb[:], in_=pp_[:], func=AF.Exp), reads=[t_pp_], writes=[t_g])
        fw.op("dve", lambda: nc.vector.tensor_tensor(out=al[:], in0=lic[:], in1=pp_[:], op=ALU.subtract), reads=[t_pp_, t_g], writes=[t_g])
        fw.op("act", lambda: nc.scalar.activation(out=al[:], in_=al[:], func=AF.Exp), reads=[t_g], writes=[t_g])
        fw.op("pe", lambda: nc.tensor.matmul(out=pp_[:], lhsT=cf[:, C_TRIU:C_TRIU + 128], rhs=lfc[:], start=True, stop=True), reads=[K.t_const, t_g], writes=[t_pp_])
        fw.op("dve", lambda: nc.vector.tensor_tensor(out=wa[:], in0=lic[:], in1=pp_[:], op=ALU.add), reads=[t_pp_, t_g], writes=[t_g])
        fw.op("act", lambda: nc.scalar.activation(out=wa[:], in_=wa[:], func=AF.Exp), reads=[t_g], writes=[t_g])
        fw.op("pe", lambda: nc.tensor.matmul(out=pp_[:], lhsT=cf[:, C_ONES:C_ONES + 128], rhs=lfc[:], start=True, stop=True), reads=[K.t_const, t_g], writes=[t_pp_])
        fw.op("act", lambda: nc.scalar.activation(out=ebL[:], in_=pp_[:], func=AF.Exp), reads=[t_pp_], writes=[t_g])
        ebL4 = ebL[:].rearrange("p (c q h) -> p c q h", q=2, h=2)
        fw.op("dve", lambda: nc.vector.tensor_copy(out=ebL2[0:64], in_=ebL4[0:64, :, :, 0]), reads=[t_g], writes=[t_g])
        fw.op("dve", lambda: nc.vector.tensor_copy(out=ebL2[64:128], in_=ebL4[64:128, :, :, 1]), reads=[t_g], writes=[t_g])
        Cst = [ph.sb("mC", [128, 132], F32) for _ in range(2)]
        t_C = [Trk() for _ in range(2)]
        RCb = [Rot(ph, "mCb", [128, 132], BF16, 2) for _ in range(2)]
        cb = []
        for p in range(2):
            fw.op("dve", lambda: nc.vector.memset(Cst[p][:], 0.0), writes=[t_C[p]])
            b_, t_b = RCb[p].next()
            fw.op("dve", lambda: nc.vector.memset(b_[:], 0.0), writes=[t_b])
            cb.append((b_, t_b))
        Rps = Rot(ph, "mps", [128, 4, 128], F32, 2, psum=True)
        Rpn = Rot(ph, "mpn", [128, 4, 65], F32, 2, psum=True)
        Rpc = Rot(ph, "mpc", [128, 132], F32, 2, psum=True)
        RscT = Rot(ph, "mscT", [128, 4, 128], BF16, 2)
        Rs4 = Rot(ph, "ms4", [128, 4], F32, 8)
        Rhm = Rot(ph, "mhm", [128, 4, 64], F32, 2)
        Rsq = Rot(ph, "msq", [128, 4, 64], F32, 2)
        Rom = Rot(ph, "mom", [128, 256], F32, 2)
        Ryb = Rot(ph, "myb", [128, 256], BF16, 2)
        Rkw = Rot(ph, "mkw", [128, 256], BF16, 2)
        for c in range(NT):
            g = c // 4
            cs = slice(c * 128, (c + 1) * 128)
            om, t_om = Rom.next()
            fw.dma("sp", om[:], K.OM[cs, :], reads=[K.t_OM[g]], writes=[t_om])
            ps, t_ps = Rps.next()
            for h in range(4):
                p, hp = h // 2, h % 2
                rr = slice(hp * 64, (hp + 1) * 64)
                fw.op("pe", lambda: nc.tensor.matmul(out=ps[:, h, :], lhsT=kT[rr, p, cs], rhs=qT[rr, p, cs], start=True, stop=True),
                      reads=[t_in[g]], writes=[t_ps])
            scT, t_scT = RscT.next()
            for h in range(4):
                fw.op("dve", lambda: nc.vector.scalar_tensor_tensor(out=scT[:, h, :], in0=ps[:, h, :], scalar=al[:, c * 4 + h:c * 4 + h + 1],
                                                                    in1=cf[:, C_MASK:C_MASK + 128], op0=ALU.mult, op1=ALU.mult),
                      reads=[t_ps, t_g, K.t_const], writes=[t_scT])
            pn, t_pn = Rpn.next()
            for h in range(4):
                p, hp = h // 2, h % 2
                rr = slice(hp * 64, (hp + 1) * 64)
                fw.op("pe", lambda: nc.tensor.matmul(out=pn[:, h, :], lhsT=scT[:, h, :], rhs=vaug[:, c, h, 0:65], start=True, stop=False),
                      reads=[t_scT, t_in[g]], writes=[t_pn])
                fw.op("pe", lambda: nc.tensor.matmul(out=pn[:, h, :], lhsT=qT[rr, p, cs], rhs=cb[p][0][rr, hp * 66:hp * 66 + 65], start=False, stop=True),
                      reads=[t_in[g], cb[p][1]], writes=[t_pn])
            eb4 = eb[:, c * 4:(c + 1) * 4]
            d4, t_d4 = Rs4.next()
            fw.op("dve", lambda: nc.vector.tensor_tensor(out=d4[:], in0=pn[:, :, 64], in1=eb4, op=ALU.mult), reads=[t_pn, t_g], writes=[t_d4])
            n4, t_n4 = Rs4.next()
            fw.op("dve", lambda: nc.vector.tensor_scalar(out=n4[:], in0=d4[:], scalar1=-1.0, scalar2=None, op0=ALU.mult), reads=[t_d4], writes=[t_n4])
            fw.op("dve", lambda: nc.vector.scalar_tensor_tensor(out=d4[:], in0=d4[:], scalar=1.0, in1=n4[:], op0=ALU.max, op1=ALU.max), reads=[t_d4, t_n4], writes=[t_d4])
            fw.op("dve", lambda: nc.vector.reciprocal(out=d4[:], in_=d4[:]), reads=[t_d4], writes=[t_d4])
            fw.op("dve", lambda: nc.vector.tensor_tensor(out=d4[:], in0=d4[:], in1=eb4, op=ALU.mult), reads=[t_d4, t_g], writes=[t_d4])
            hm, t_hm = Rhm.next()
            fw.op("dve", lambda: nc.vector.tensor_tensor(out=hm[:], in0=pn[:, :, 0:64], in1=bview(d4[:], [128, 4, 64]), op=ALU.mult),
                  reads=[t_pn, t_d4], writes=[t_hm])
            m4, t_m4 = Rs4.next()
            fw.op("dve", lambda: nc.vector.tensor_reduce(out=m4[:], in_=hm[:], axis=AX.X, op=ALU.add), reads=[t_hm], writes=[t_m4])
            fw.op("dve", lambda: nc.vector.tensor_scalar(out=m4[:], in0=m4[:], scalar1=-1.0 / 64, scalar2=None, op0=ALU.mult), reads=[t_m4], writes=[t_m4])
            fw.op("dve", lambda: nc.vector.tensor_tensor(out=hm[:], in0=hm[:], in1=bview(m4[:], [128, 4, 64]), op=ALU.add), reads=[t_hm, t_m4], writes=[t_hm])
            sq, t_sq = Rsq.next()
            fw.op("pool", lambda: nc.gpsimd.tensor_tensor(out=sq[:], in0=hm[:], in1=hm[:], op=ALU.mult), reads=[t_hm], writes=[t_sq])
            v4, t_v4 = Rs4.next()
            fw.op("dve", lambda: nc.vector.tensor_reduce(out=v4[:], in_=sq[:], axis=AX.X, op=ALU.add), reads=[t_sq], writes=[t_v4])
            fw.op("act", lambda: nc.scalar.activation(out=v4[:], in_=v4[:], func=AF.Sqrt, scale=1.0 / 64, bias=EPS), reads=[t_v4], writes=[t_v4])
            fw.op("dve", lambda: nc.vector.reciprocal(out=v4[:], in_=v4[:]), reads=[t_v4], writes=[t_v4])
            fw.op("dve", lambda: nc.vector.tensor_tensor(out=hm[:], in0=hm[:], in1=bview(v4[:], [128, 4, 64]), op=ALU.mult), reads=[t_hm, t_v4], writes=[t_hm])
            hm2 = hm[:].rearrange("p h e -> p (h e)")
            fw.op("pool", lambda: nc.gpsimd.tensor_tensor(out=hm2, in0=hm2, in1=mnw[:], op=ALU.mult), reads=[t_hm, t_mnw], writes=[t_hm])
            yb, t_yb = Ryb.next()
            fw.op("dve", lambda: nc.vector.tensor_tensor(out=yb[:], in0=hm2, in1=om[:], op=ALU.mult), reads=[t_hm, t_om], writes=[t_yb])
            fw.dma("sp", K.Y[cs, 0:256], yb[:], reads=[t_yb], writes=[K.t_Y[g]])
            if c + 1 < NT:
                kw, t_kw = Rkw.next()
                fw.op("dve", lambda: nc.vector.tensor_tensor(out=kw[:].rearrange("p (h e) -> p h e", h=4), in0=ktok[:, c, :].rearrange("p (h e) -> p h e", h=4),
                                                              in1=bview(wa[:, c * 4:(c + 1) * 4], [128, 4, 64]), op=ALU.mult),
                      reads=[t_in[g], t_g], writes=[t_kw])
                for p in range(2):
                    pc, t_pc = Rpc.next()
                    fw.op("pe", lambda: nc.tensor.matmul(out=pc[:], lhsT=kw[:, p * 128:(p + 1) * 128],
                                                         rhs=vaug[:, c, 2 * p:2 * p + 2, :].rearrange("p a b -> p (a b)"), start=True, stop=True),
                          reads=[t_kw, t_in[g]], writes=[t_pc])
                    fw.op("dve", lambda: nc.vector.scalar_tensor_tensor(out=Cst[p][:], in0=Cst[p][:], scalar=ebL2[:, c, p:p + 1], in1=pc[:],
                                                                        op0=ALU.mult, op1=ALU.add),
                          reads=[t_C[p], t_g, t_pc], writes=[t_C[p]])
                    b_, t_b = RCb[p].next()
                    fw.op("act", lambda: nc.scalar.copy(out=b_[:], in_=Cst[p][:]), reads=[t_C[p]], writes=[t_b])
                    cb[p] = (b_, t_b)


def hgrn_phase(K, l):
    nc, fw = K.nc, K.fw
    cf = K.constf
    with Phase(K) as ph:
        qT = ph.sb("hqT", [128, 2, S], BF16)
        kT = ph.sb("hkT", [128, 2, S], BF16)
        lf = ph.sb("hlf", [128, NT, 256], F32)
        kht = ph.sb("hkht", [128, NT, 256], F32)
        vh = ph.sb("hvh", [128, NT, 256], BF16)
        t_in = [Trk() for _ in range(NG)]
        hnw, t_hnw = load_row(K, ph, "hnw", K.h_nw[l], 256)
        for g in range(NG):
            cs = slice(g * 512, (g + 1) * 512)
            ts = slice(g * 4, (g + 1) * 4)
            for p in range(2):
                fw.dma("sp", qT[:, p, cs], K.QH[p * 128:(p + 1) * 128, cs], reads=[K.t_QH[g]], writes=[t_in[g]])
                fw.dma("sp", kT[:, p, cs], K.KH[p * 128:(p + 1) * 128, cs], reads=[K.t_KH[g]], writes=[t_in[g]])
            fw.dma("sp", lf[:, ts, :], K.LFH[cs, :].rearrange("(c p) f -> p c f", p=128), reads=[K.t_LFH[g]], writes=[t_in[g]])
            fw.dma("sp", kht[:, ts, :], K.KHT[cs, :].rearrange("(c p) f -> p c f", p=128), reads=[K.t_KHT[g]], writes=[t_in[g]])
            fw.dma("sp", vh[:, ts, :], K.VH[cs, :].rearrange("(c p) f -> p c f", p=128), reads=[K.t_VH[g]], writes=[t_in[g]])
        Sst = [ph.sb("hS", [128, 128], F32) for _ in range(2)]
        t_S = [Trk() for _ in range(2)]
        RSb = [Rot(ph, "hSb", [128, 128], BF16, 3) for _ in range(2)]
        sb_ = []
        for p in range(2):
            fw.op("dve", lambda: nc.vector.memset(Sst[p][:], 0.0), writes=[t_S[p]])
            b_, t_b = RSb[p].next()
            fw.op("dve", lambda: nc.vector.memset(b_[:], 0.0), writes=[t_b])
            sb_.append((b_, t_b))
        q01 = [[ph.sb("hq01", [128, 2, 128], BF16) for _ in range(2)] for _ in range(2)]
        t_q01 = [[Trk() for _ in range(2)] for _ in range(2)]
        for r in range(2):
            for j in range(2):
                fw.op("pool", lambda: nc.gpsimd.memset(q01[r][j][:], 0.0), writes=[t_q01[r][j]])
        Rpm = Rot(ph, "hpm", [128, 128], F32, 2, psum=True)
        Rpl = Rot(ph, "hpl", [128, 2], F32, 1, psum=True)
        Rprb = Rot(ph, "hprb", [128, 256], F32, 1, psum=True)
        Rpa = Rot(ph, "hpa", [128, 4, 128], F32, 1, psum=True)
        Rpo = Rot(ph, "hpo", [128, 4, 64], F32, 1, psum=True)
        Rpss = Rot(ph, "hpss", [128, 128], F32, 2, psum=True)
        RE = Rot(ph, "hE", [128, 128], F32, 4)
        Rqt = Rot(ph, "hqt", [128, 2, 128], BF16, 2)
        Rkt = Rot(ph, "hkt", [128, 2, 128], BF16, 2)
        Rebl = Rot(ph, "hebl", [128, 2, 2], F32, 2)
        Rwk = Rot(ph, "hwk", [128, 256], F32, 2)
        Rkp = Rot(ph, "hkp", [128, 256], BF16, 2)
        RAT = Rot(ph, "hAT", [128, 4, 128], BF16, 2)
        Ros = Rot(ph, "hos", [128, 4, 64], F32, 2)
        Rsq = Rot(ph, "hsq", [128, 4, 64], F32, 2)
        Rs4 = Rot(ph, "hs4", [128, 4], F32, 4)
        Rgh = Rot(ph, "hgh", [128, 256], F32, 2)
        Ryb = Rot(ph, "hyb", [128, 256], BF16, 2)
        for i in range(NT):
            g = i // 4
            cs = slice(i * 128, (i + 1) * 128)
            gh, t_gh = Rgh.next()
            fw.dma("sp", gh[:], K.GH[cs, :], reads=[K.t_GH[g]], writes=[t_gh])
            qt, t_qt = Rqt.next()
            kt, t_kt = Rkt.next()
            ebl, t_ebl = Rebl.next()
            q0, q1 = q01[i % 2]
            t_q0, t_q1 = t_q01[i % 2]
            for p in range(2):
                lfp = lf[:, i, p * 128:(p + 1) * 128]
                pm, t_pm = Rpm.next()
                fw.op("pe", lambda: nc.tensor.matmul(out=pm[:], lhsT=lfp, rhs=cf[:, C_TRIMID:C_TRIMID + 128], start=True, stop=True),
                      reads=[t_in[g], K.t_const], writes=[t_pm])
                E1, t_E1 = RE.next()
                E2, t_E2 = RE.next()
                fw.op("act", lambda: nc.scalar.activation(out=E1[:], in_=pm[:], func=AF.Exp), reads=[t_pm], writes=[t_E1])
                fw.op("act", lambda: nc.scalar.activation(out=E2[:], in_=pm[:], func=AF.Exp, scale=-1.0), reads=[t_pm], writes=[t_E2])
                fw.op("dve", lambda: nc.vector.tensor_tensor(out=qt[:, p, :], in0=qT[:, p, cs], in1=E1[:], op=ALU.mult), reads=[t_in[g], t_E1], writes=[t_qt])
                fw.op("pool", lambda: nc.gpsimd.tensor_tensor(out=kt[:, p, :], in0=kT[:, p, cs], in1=E2[:], op=ALU.mult), reads=[t_in[g], t_E2], writes=[t_kt])
                pb, t_pb = Rpm.next()
                fw.op("pe", lambda: nc.tensor.matmul(out=pb[:], lhsT=lfp, rhs=cf[:, C_TRITBD:C_TRITBD + 128], start=True, stop=True),
                      reads=[t_in[g], K.t_const], writes=[t_pb])
                E3, t_E3 = RE.next()
                fw.op("act", lambda: nc.scalar.activation(out=E3[:], in_=pb[:], func=AF.Exp), reads=[t_pb], writes=[t_E3])
                fw.op("dve", lambda: nc.vector.tensor_tensor(out=q0[:, p, 0:64], in0=qT[:, p, i * 128:i * 128 + 64], in1=E3[:, 0:64], op=ALU.mult),
                      reads=[t_in[g], t_E3], writes=[t_q0])
                fw.op("dve", lambda: nc.vector.tensor_tensor(out=q1[:, p, 64:128], in0=qT[:, p, i * 128 + 64:(i + 1) * 128], in1=E3[:, 64:128], op=ALU.mult),
                      reads=[t_in[g], t_E3], writes=[t_q1])
                pl, t_pl = Rpl.next()
                fw.op("pe", lambda: nc.tensor.matmul(out=pl[:], lhsT=lfp, rhs=cf[:, C_CHI:C_CHI + 2], start=True, stop=True),
                      reads=[t_in[g], K.t_const], writes=[t_pl])
                fw.op("act", lambda: nc.scalar.activation(out=ebl[:, p, :], in_=pl[:], func=AF.Exp), reads=[t_pl], writes=[t_ebl])
            prb, t_prb = Rprb.next()
            fw.op("pe", lambda: nc.tensor.matmul(out=prb[:], lhsT=cf[:, C_TRIUBD:C_TRIUBD + 128], rhs=lf[:, i, :], start=True, stop=True),
                  reads=[t_in[g], K.t_const], writes=[t_prb])
            wk, t_wk = Rwk.next()
            fw.op("act", lambda: nc.scalar.activation(out=wk[:], in_=prb[:], func=AF.Exp), reads=[t_prb], writes=[t_wk])
            kp, t_kp = Rkp.next()
            fw.op("pool", lambda: nc.gpsimd.tensor_tensor(out=kp[:], in0=kht[:, i, :], in1=wk[:], op=ALU.mult), reads=[t_in[g], t_wk], writes=[t_kp])
            pa, t_pa = Rpa.next()
            for h in range(4):
                p, hp = h // 2, h % 2
                rr = slice(hp * 64, (hp + 1) * 64)
                fw.op("pe", lambda: nc.tensor.matmul(out=pa[:, h, :], lhsT=kt[rr, p, :], rhs=qt[rr, p, :], start=True, stop=True),
                      reads=[t_kt, t_qt], writes=[t_pa])
            AT, t_AT = RAT.next()
            fw.op("dve", lambda: nc.vector.tensor_tensor(out=AT[:], in0=pa[:], in1=cf[:, C_MASKBD:C_MASKBD + 128].unsqueeze(1).to_broadcast([128, 4, 128]), op=ALU.mult),
                  reads=[t_pa, K.t_const], writes=[t_AT])

            def state_update(j):
                rj = slice(j * 64, (j + 1) * 64)
                for p in range(2):
                    pss, t_pss = Rpss.next()
                    fw.op("pe", lambda: nc.tensor.matmul(out=pss[:], lhsT=kp[rj, p * 128:(p + 1) * 128], rhs=vh[rj, i, p * 128:(p + 1) * 128], start=True, stop=True),
                          reads=[t_kp, t_in[g]], writes=[t_pss])
                    fw.op("dve", lambda: nc.vector.scalar_tensor_tensor(out=Sst[p][:], in0=Sst[p][:], scalar=ebl[:, p, j:j + 1], in1=pss[:],
                                                                        op0=ALU.mult, op1=ALU.add),
                          reads=[t_S[p], t_ebl, t_pss], writes=[t_S[p]])
                    b_, t_b = RSb[p].next()
                    fw.op("act", lambda: nc.scalar.copy(out=b_[:], in_=Sst[p][:]), reads=[t_S[p]], writes=[t_b])
                    yield (b_, t_b)
            s0 = list(sb_)
            s1 = list(state_update(0))
            po, t_po = Rpo.next()
            for h in range(4):
                p, hp = h // 2, h % 2
                rr = slice(hp * 64, (hp + 1) * 64)
                cc = slice(hp * 64, (hp + 1) * 64)
                fw.op("pe", lambda: nc.tensor.matmul(out=po[:, h, :], lhsT=AT[:, h, :], rhs=vh[:, i, h * 64:(h + 1) * 64], start=True, stop=False),
                      reads=[t_AT, t_in[g]], writes=[t_po])
                fw.op("pe", lambda: nc.tensor.matmul(out=po[:, h, :], lhsT=q0[rr, p, :], rhs=s0[p][0][rr, cc], start=False, stop=False),
                      reads=[t_q0, s0[p][1]], writes=[t_po])
                fw.op("pe", lambda: nc.tensor.matmul(out=po[:, h, :], lhsT=q1[rr, p, :], rhs=s1[p][0][rr, cc], start=False, stop=True),
                      reads=[t_q1, s1[p][1]], writes=[t_po])
            if i + 1 < NT:
                sb_ = list(state_update(1))
            os_, t_os = Ros.next()
            fw.op("act", lambda: nc.scalar.copy(out=os_[:], in_=po[:]), reads=[t_po], writes=[t_os])
            sq, t_sq = Rsq.next()
            fw.op("pool", lambda: nc.gpsimd.tensor_tensor(out=sq[:], in0=os_[:], in1=os_[:], op=ALU.mult), reads=[t_os], writes=[t_sq])
            v4, t_v4 = Rs4.next()
            fw.op("dve", lambda: nc.vector.tensor_reduce(out=v4[:], in_=sq[:], axis=AX.X, op=ALU.add), reads=[t_sq], writes=[t_v4])
            fw.op("act", lambda: nc.scalar.activation(out=v4[:], in_=v4[:], func=AF.Sqrt, scale=1.0 / 64, bias=EPS), reads=[t_v4], writes=[t_v4])
            fw.op("dve", lambda: nc.vector.reciprocal(out=v4[:], in_=v4[:]), reads=[t_v4], writes=[t_v4])
            fw.op("dve", lambda: nc.vector.tensor_tensor(out=os_[:], in0=os_[:], in1=bview(v4[:], [128, 4, 64]), op=ALU.mult), reads=[t_os, t_v4], writes=[t_os])
            o2 = os_[:].rearrange("p h e -> p (h e)")
            fw.op("pool", lambda: nc.gpsimd.tensor_tensor(out=o2, in0=o2, in1=hnw[:], op=ALU.mult), reads=[t_os, t_hnw], writes=[t_os])
            yb, t_yb = Ryb.next()
            fw.op("dve", lambda: nc.vector.tensor_tensor(out=yb[:], in0=o2, in1=gh[:], op=ALU.mult), reads=[t_os, t_gh], writes=[t_yb])
            fw.dma("sp", K.Y[cs, 768:1024], yb[:], reads=[t_yb], writes=[K.t_Y[g]])


def attn_phase(K, l):
    nc, fw = K.nc, K.fw
    cf = K.constf
    lam_init = 0.8 - 0.6 * math.exp(-0.3 * l)
    with Phase(K) as ph:
        lv, t_lv = load_row(K, ph, "lv", K.dlam[l], 256)
        dnw, t_dnw = load_row(K, ph, "dnw", K.d_nw[l], 512)
        rb, t_rb = load_row(K, ph, "rb", K.rel_bias, 128)
        j64 = ph.sb("j64", [128, 2, 64], F32)
        s12 = ph.sb("s12", [128, 2], F32)
        nlam = ph.sb("nlam", [128, 1], F32)
        t_lam = Trk()
        lv4 = lv[:].rearrange("p (a b) -> p a b", a=4)
        fw.op("dve", lambda: nc.vector.tensor_tensor(out=j64[:, 0, :], in0=lv4[:, 0, :], in1=lv4[:, 1, :], op=ALU.mult), reads=[t_lv], writes=[t_lam])
        fw.op("dve", lambda: nc.vector.tensor_tensor(out=j64[:, 1, :], in0=lv4[:, 2, :], in1=lv4[:, 3, :], op=ALU.mult), reads=[t_lv, t_lam], writes=[t_lam])
        fw.op("dve", lambda: nc.vector.tensor_reduce(out=s12[:], in_=j64[:], axis=AX.X, op=ALU.add), reads=[t_lam], writes=[t_lam])
        fw.op("act", lambda: nc.scalar.activation(out=s12[:], in_=s12[:], func=AF.Exp), reads=[t_lam], writes=[t_lam])
        fw.op("dve", lambda: nc.vector.tensor_tensor(out=nlam[:], in0=s12[:, 1:2], in1=s12[:, 0:1], op=ALU.subtract), reads=[t_lam], writes=[t_lam])
        fw.op("dve", lambda: nc.vector.tensor_scalar(out=nlam[:], in0=nlam[:], scalar1=-lam_init, scalar2=None, op0=ALU.add), reads=[t_lam], writes=[t_lam])
        fw.op("dve", lambda: nc.vector.tensor_scalar(out=dnw[:], in0=dnw[:], scalar1=1.0 - lam_init, scalar2=None, op0=ALU.mult), reads=[t_dnw], writes=[t_dnw])
        et = ph.sb("et", [128, 128], F32)
        t_et = Trk()
        fw.op("act", lambda: nc.scalar.activation(out=et[:], in_=rb[:], func=AF.Exp), reads=[t_rb], writes=[t_et])
        EE = ph.sb("EE", [128, 4, 768], F32)
        t_EE = Trk()
        etmp = ph.sb("etmp", [128, 768], F32)
        t_etmp = Trk()
        fw.op("dve", lambda: nc.vector.memset(EE[:], 0.0), writes=[t_EE])
        for b in range(32):
            w = 768 if b == 31 else 256
            for h in range(4):
                fw.op("dve", lambda: nc.vector.tensor_scalar(out=etmp[:, 0:w], in0=cf[:, C_EEI:C_EEI + w], scalar1=float(b), scalar2=et[:, b * 4 + h:b * 4 + h + 1],
                                                             op0=ALU.is_equal, op1=ALU.mult),
                      reads=[K.t_const, t_et, t_etmp], writes=[t_etmp])
                fw.op("dve", lambda: nc.vector.tensor_tensor(out=EE[:, h, 0:w], in0=EE[:, h, 0:w], in1=etmp[:, 0:w], op=ALU.add),
                      reads=[t_etmp, t_EE], writes=[t_EE])
        sets = []
        for r in range(2):
            QT = ph.sb("aQT", [128, S], BF16)
            KT = ph.sb("aKT", [128, S], BF16)
            V = ph.sb("aV", [128, NT, 130], BF16)
            tr = Trk()
            fw.op("pool", lambda: nc.gpsimd.memset(V[:], 1.0), writes=[tr])
            sets.append((QT, KT, V, tr))

        def load_head(h):
            QT, KT, V, tr = sets[h % 2]
            for g in range(NG):
                cs = slice(g * 512, (g + 1) * 512)
                fw.dma("sp", QT[:, cs], K.QD[h * 128:(h + 1) * 128, cs], reads=[K.t_QD[g]], writes=[tr])
                fw.dma("sp", KT[:, cs], K.KD[h * 128:(h + 1) * 128, cs], reads=[K.t_KD[g]], writes=[tr])
                fw.dma("sp", V[:, g * 4:(g + 1) * 4, 0:128], K.VD[cs, h * 128:(h + 1) * 128].rearrange("(c p) e -> p c e", p=128),
                       reads=[K.t_VD[g]], writes=[tr])
        Rs1 = Rot(ph, "as1", [128, 512], F32, 2, psum=True)
        Rs2 = Rot(ph, "as2", [128, 512], F32, 2, psum=True)
        PO = [(ph.ps("aPO", [128, 3, 129], F32), Trk(True)) for _ in range(3)]
        Rp1 = Rot(ph, "ap1", [128, 512], BF16, 3)
        Rp2 = Rot(ph, "ap2", [128, 512], BF16, 3)
        Re = Rot(ph, "ae", [128, 512], F32, 3)
        Rr = Rot(ph, "ar", [128, 2], F32, 4)
        Rt1 = Rot(ph, "at1", [128, 128], F32, 2)
        Rod = Rot(ph, "aod", [128, 128], F32, 2)
        Rsq = Rot(ph, "asq", [128, 128], F32, 2)
        Rss = Rot(ph, "ass", [128, 1], F32, 4)
        Ryb = Rot(ph, "ayb", [128, 128], BF16, 3)

        def acc(j, which):
            idx = which * 4 + j
            po, tp = PO[idx // 3]
            return po[:, idx % 3, :], tp

        load_head(0)
        for h in range(4):
            if h + 1 < 4:
                load_head(h + 1)
            QT, KT, V, t_hd = sets[h % 2]
            tiles = [(Q, kb) for Q in range(8) for kb in range(4 * Q + 4)]

            def S_step(Q, kb):
                j0 = kb - 4 * Q
                jlo = max(j0, 0)
                c0 = jlo * 128
                ks = slice(kb * 128, (kb + 1) * 128)
                qs = slice(Q * 512 + c0, (Q + 1) * 512)
                s1, t_s1 = Rs1.next()
                s2, t_s2 = Rs2.next()
                fw.op("pe", lambda: nc.tensor.matmul(out=s1[:, c0:512], lhsT=KT[0:64, ks], rhs=QT[0:64, qs], start=True, stop=True),
                      reads=[t_hd], writes=[t_s1])
                fw.op("pe", lambda: nc.tensor.matmul(out=s2[:, c0:512], lhsT=KT[64:128, ks], rhs=QT[64:128, qs], start=True, stop=True),
                      reads=[t_hd], writes=[t_s2])
                P1, t_P1 = Rp1.next()
                P2, t_P2 = Rp2.next()
                for (sx, t_sx, Px, t_Px, eng) in ((s1, t_s1, P1, t_P1, "dve"), (s2, t_s2, P2, t_P2, "pool")):
                    if j0 <= -2:
                        fw.op("act", lambda: nc.scalar.activation(out=Px[:], in_=sx[:], func=AF.Exp, scale=0.125, bias=rb[:, 124 + h:125 + h]),
                              reads=[t_sx, t_rb], writes=[t_Px])
                    else:
                        e, t_e = Re.next()
                        eoff = (jlo - j0) * 128
                        ncol = 512 - c0
                        fw.op("act", lambda: nc.scalar.activation(out=e[:, c0:512], in_=sx[:, c0:512], func=AF.Exp, scale=0.125), reads=[t_sx], writes=[t_e])
                        if eng == "dve":
                            fw.op("dve", lambda: nc.vector.tensor_tensor(out=Px[:, c0:512], in0=e[:, c0:512], in1=EE[:, h, eoff:eoff + ncol], op=ALU.mult),
                                  reads=[t_e, t_EE], writes=[t_Px])
                        else:
                            fw.op("pool", lambda: nc.gpsimd.tensor_tensor(out=Px[:, c0:512], in0=e[:, c0:512], in1=EE[:, h, eoff:eoff + ncol], op=ALU.mult),
                                  reads=[t_e, t_EE], writes=[t_Px])
                return (Q, kb, jlo, P1, t_P1, P2, t_P2)

            def A_step(info):
                Q, kb, jlo, P1, t_P1, P2, t_P2 = info
                if kb == 0:
                    for po, tp in PO:
                        fw.op("dve", lambda: nc.vector.memset(po[:], 0.0), writes=[tp])
                for j in range(jlo, 4):
                    for which, (Px, t_Px) in enumerate(((P1, t_P1), (P2, t_P2))):
                        o, t_o = acc(j, which)
                        fw.op("pe", lambda: nc.tensor.matmul(out=o, lhsT=Px[:, j * 128:(j + 1) * 128], rhs=V[:, kb, 0:129], start=False, stop=False,
                                                             skip_group_check=True),
                              reads=[t_Px, t_hd], writes=[t_o])

            def F_step(Q):
                for j in range(4):
                    i = 4 * Q + j
                    o1, t_o1 = acc(j, 0)
                    o2, t_o2 = acc(j, 1)
                    r, t_r = Rr.next()
                    fw.op("dve", lambda: nc.vector.reciprocal(out=r[:, 0:1], in_=o1[:, 128:129]), reads=[t_o1], writes=[t_r])
                    fw.op("dve", lambda: nc.vector.reciprocal(out=r[:, 1:2], in_=o2[:, 128:129]), reads=[t_o2, t_r], writes=[t_r])
                    fw.op("dve", lambda: nc.vector.tensor_tensor(out=r[:, 1:2], in0=r[:, 1:2], in1=nlam[:], op=ALU.mult), reads=[t_r, t_lam], writes=[t_r])
                    t1, t_t1 = Rt1.next()
                    fw.op("dve", lambda: nc.vector.tensor_scalar(out=t1[:], in0=o1[:, 0:128], scalar1=r[:, 0:1], scalar2=None, op0=ALU.mult),
                          reads=[t_o1, t_r], writes=[t_t1])
                    od, t_od = Rod.next()
                    fw.op("dve", lambda: nc.vector.scalar_tensor_tensor(out=od[:], in0=o2[:, 0:128], scalar=r[:, 1:2], in1=t1[:], op0=ALU.mult, op1=ALU.add),
                          reads=[t_o2, t_r, t_t1], writes=[t_od])
                    sq, t_sq = Rsq.next()
                    fw.op("pool", lambda: nc.gpsimd.tensor_tensor(out=sq[:], in0=od[:], in1=od[:], op=ALU.mult), reads=[t_od], writes=[t_sq])
                    ss, t_ss = Rss.next()
                    fw.op("dve", lambda: nc.vector.tensor_reduce(out=ss[:], in_=sq[:], axis=AX.X, op=ALU.add), reads=[t_sq], writes=[t_ss])
                    fw.op("act", lambda: nc.scalar.activation(out=ss[:], in_=ss[:], func=AF.Ln, scale=1.0 / 128, bias=EPS), reads=[t_ss], writes=[t_ss])
                    fw.op("act", lambda: nc.scalar.activation(out=ss[:], in_=ss[:], func=AF.Exp, scale=-0.5), reads=[t_ss], writes=[t_ss])
                    yb, t_yb = Ryb.next()
                    fw.op("dve", lambda: nc.vector.scalar_tensor_tensor(out=yb[:], in0=od[:], scalar=ss[:], in1=dnw[:, h * 128:(h + 1) * 128], op0=ALU.mult, op1=ALU.mult),
                          reads=[t_od, t_ss, t_dnw], writes=[t_yb])
                    fw.dma("sp", K.Y[i * 128:(i + 1) * 128, 256 + h * 128:256 + (h + 1) * 128], yb[:], reads=[t_yb], writes=[K.t_Y[i // 4]])

            prev = None
            for (Q, kb) in tiles:
                info = S_step(Q, kb)
                if prev is not None:
                    A_step(prev)
                    if prev[1] == 4 * prev[0] + 3:
                        F_step(prev[0])
                prev = info
            A_step(prev)
            F_step(prev[0])


KCtx.attn_phase = staticmethod(attn_phase)


def mixers(K, l, stop_after):
    MX = os.environ.get("MIXERS", "MHD")
    K.fw.pe_sync = True
    if "M" in MX:
        mlstm_phase(K, l)
    if "H" in MX:
        hgrn_phase(K, l)
    K.fw.pe_sync = False
    if "D" in MX and hasattr(K, "attn_phase"):
        K.attn_phase(K, l)


KCtx.mixers = staticmethod(mixers)


def build(n_layers=DEPTH, debug=False, stop_after=None):
    nc = bass.Bass("TRN2", target_bir_lowering=False)
    K = KCtx()
    K.nc = nc
    dt = nc.dram_tensor
    K.x = dt("x", [S, D], F32, kind="ExternalInput").ap()
    K.norm_w = dt("norm_w", [DEPTH, 6, D], F32, kind="ExternalInput").ap()
    K.ffn1_wi = dt("ffn1_wi", [DEPTH, D, 2 * DFF], F32, kind="ExternalInput").ap()
    K.ffn1_wo = dt("ffn1_wo", [DEPTH, DFF, D], F32, kind="ExternalInput").ap()
    K.ffn2_wi = dt("ffn2_wi", [DEPTH, D, 2 * DFF], F32, kind="ExternalInput").ap()
    K.ffn2_wo = dt("ffn2_wo", [DEPTH, DFF, D], F32, kind="ExternalInput").ap()
    K.w_in = dt("w_in", [DEPTH, D, NIN], F32, kind="ExternalInput").ap()
    K.w_out = dt("w_out", [DEPTH, D, D], F32, kind="ExternalInput").ap()
    K.consts = dt("consts", [128, NCONST], F32, kind="ExternalInput").ap()
    K.pp = dt("pp", [DEPTH, 128, 28], F32, kind="ExternalInput").ap()
    K.ig_b = dt("mlstm_igate_b", [DEPTH, 4], F32, kind="ExternalInput").ap()
    K.fg_b = dt("mlstm_fgate_b", [DEPTH, 4], F32, kind="ExternalInput").ap()
    K.m_nw = dt("mlstm_norm_w", [DEPTH, 256], F32, kind="ExternalInput").ap()
    K.dlam = dt("diff_lambda", [DEPTH, 256], F32, kind="ExternalInput").ap()
    K.d_nw = dt("diff_norm_w", [DEPTH, 512], F32, kind="ExternalInput").ap()
    K.rel_bias = dt("rel_bias", [128], F32, kind="ExternalInput").ap()
    K.lb_logits = dt("hgrn_lb_logits", [DEPTH, 256], F32, kind="ExternalInput").ap()
    K.h_nw = dt("hgrn_norm_w", [DEPTH, 256], F32, kind="ExternalInput").ap()
    K.out = dt("out", [S, D], F32, kind="ExternalOutput").ap()
    dbg = set(debug) if debug else set()

    def scratch(name, shape, dtp):
        kind = "ExternalOutput" if name in dbg else "Internal"
        ap = dt(name, shape, dtp, kind=kind).ap()
        return ap, [Trk() for _ in range(NG)]
    K.X, K.t_X = scratch("Xs", [S, D], F32)
    K.Y, K.t_Y = scratch("Ys", [S, D], BF16)
    K.QKM, K.t_QKM = scratch("QKM", [512, S], BF16)
    K.KMT, K.t_KMT = scratch("KMT", [S, 256], BF16)
    K.VM, K.t_VM = scratch("VM", [S, 256], BF16)
    K.OM, K.t_OM = scratch("OM", [S, 256], F32)
    K.GM, K.t_GM = scratch("GM", [S, 8], F32)
    K.QD, K.t_QD = scratch("QD", [512, S], BF16)
    K.KD, K.t_KD = scratch("KD", [512, S], BF16)
    K.VD, K.t_VD = scratch("VD", [S, 512], BF16)
    K.QH, K.t_QH = scratch("QH", [256, S], BF16)
    K.KH, K.t_KH = scratch("KH", [256, S], BF16)
    K.LFH, K.t_LFH = scratch("LFH", [S, 256], F32)
    K.KHT, K.t_KHT = scratch("KHT", [S, 256], F32)
    K.VH, K.t_VH = scratch("VH", [S, 256], BF16)
    K.GH, K.t_GH = scratch("GH", [S, 256], F32)
    K.t_x = [Trk() for _ in range(NG)]
    K.t_out = [Trk() for _ in range(NG)]
    K.WIB = dt("WIB", [2, 11, 128, 4096], BF16, kind="Internal").ap()
    K.t_WIB = [[Trk() for _ in range(11)] for _ in range(2)]
    K.wib_ready = {}
    K.n_layers = n_layers
    with ExitStack() as st:
        fw = FW(nc, st)
        K.fw = fw
        K.constf = st.enter_context(nc.sbuf_tensor("constf", [128, NCONST], F32))
        K.identb = st.enter_context(nc.sbuf_tensor("identb", [128, 128], BF16))
        K.t_const = Trk()
        fw.dma("sp", K.constf[:], K.consts, writes=[K.t_const])
        fw.op("dve", lambda: nc.vector.tensor_copy(out=K.identb[:], in_=K.constf[:, C_IDENT:C_IDENT + 128]),
              reads=[K.t_const], writes=[K.t_const])
        for l in range(n_layers):
            xin, t_xin = (K.x, K.t_x) if l == 0 else (K.X, K.t_X)
            if os.environ.get('SIM_PROJ_ONLY'):
                proj_phase(K, l, K.x, K.t_x)
                if stop_after[1] >= 3:
                    K.mixers(K, l, stop_after)
                break
            token_phase(K, l, 1, xin, t_xin, K.X, K.t_X)
            if stop_after == (l, 1):
                break
            proj_phase(K, l)
            if stop_after == (l, 2):
                break
            if hasattr(K, "mixers"):
                K.mixers(K, l, stop_after)
            if stop_after is not None and stop_after[0] == l and stop_after[1] == 3:
                break
            last = (l == n_layers - 1)
            token_phase(K, l, 2, K.X, K.t_X, K.out if last else K.X, K.t_out if last else K.t_X)
            if stop_after == (l, 4):
                break
        fw.barrier()
    K.counts = {k: v.count for k, v in fw.engs.items()}
    print("instr counts", K.counts, "dmas", fw.dma_count)
    return nc


def make_in_map(inputs, c, consts):
    f = lambda a: np.ascontiguousarray(a, dtype=np.float32)
    m = {"x": f(inputs["x"][c]), "consts": consts}
    for k in ["norm_w", "ffn1_wi", "ffn1_wo", "ffn2_wi", "ffn2_wo", "w_in", "w_out", "mlstm_igate_b", "mlstm_fgate_b",
              "mlstm_norm_w", "diff_norm_w", "hgrn_lb_logits", "hgrn_norm_w"]:
        m[k] = f(inputs[k])
    m["diff_lambda"] = f(inputs["diff_lambda"]).reshape(DEPTH, 256)
    m["rel_bias"] = f(inputs["rel_bias"]).reshape(128)
    pp = np.zeros((DEPTH, 128, 28), np.float32)
    cw = f(inputs["mlstm_conv_w"])
    cb = f(inputs["mlstm_conv_b"])
    lg = f(inputs["hgrn_lb_logits"])
    for l in range(DEPTH):
        pp[l, :, 0:16] = cw[l].reshape(4, 4, 128).transpose(2, 1, 0).reshape(128, 16)
        pp[l, :, 16:20] = cb[l].reshape(4, 128).T
        pp[l, :, 20:28] = lg.reshape(DEPTH, 2, 128).transpose(2, 1, 0).reshape(128, 8)
    m["pp"] = pp
    return m


_NC_CACHE = {}


def kernel(**inputs):
    nc = _NC_CACHE.get("nc")
    if nc is None:
        nc = build()
        _NC_CACHE["nc"] = nc
    consts = make_consts()
    in_maps = [make_in_map(inputs, c, consts) for c in range(8)]
    res = run_bass_kernel_spmd(nc, in_maps, core_ids=list(range(8)))
    return np.stack([res.results[c]["out"] for c in range(8)], axis=0)
```

```python
import math
import os
import numpy as np
from contextlib import ExitStack
import concourse.bass as bass
import concourse.mybir as mybir
from concourse.bass_utils import run_bass_kernel_spmd

F32 = mybir.dt.float32
BF16 = mybir.dt.bfloat16
AF = mybir.ActivationFunctionType
ALU = mybir.AluOpType
AX = mybir.AxisListType

S = 4096
D = 1024
DFF = 2816
NIN = 3592
NT = 32
NG = 8
DEPTH = 4
EPS = 1e-6
LN8 = math.log(0.125)


class Trk:
    __slots__ = ("w", "r", "psum")

    def __init__(self, psum=False):
        self.w = None
        self.r = []
        self.psum = psum


class Eng:
    def __init__(self, name, raw, sem):
        self.name = name
        self.raw = raw
        self.sem = sem
        self.count = 0
        self.waited = {}


class FW:
    NDMA = 8

    def __init__(self, nc, stack):
        self.nc = nc
        self.stack = stack
        self.engs = {}
        for name, raw in [("pe", nc.tensor), ("dve", nc.vector), ("act", nc.scalar),
                          ("pool", nc.gpsimd), ("sp", nc.sync)]:
            sem = stack.enter_context(nc.semaphore("sem_" + name))
            self.engs[name] = Eng(name, raw, sem)
        self.dma_sems = {}
        self.dma_count = {}
        for q in ["sp", "pool", "act"]:
            self.dma_sems[q] = [stack.enter_context(nc.semaphore("dsem_%s_%d" % (q, i))) for i in range(self.NDMA)]
            self.dma_count[q] = 0
        self.uid = 0
        self.pe_sync = False

    def name(self, base):
        self.uid += 1
        return "%s_%d" % (base, self.uid)

    def _deps(self, e, reads, writes):
        deps = {}

        def add(d):
            if d is None:
                return
            sem, val, en = d
            if en == "pe" and e.name == "pe" and not self.pe_sync:
                return
            k = id(sem)
            if e.waited.get(k, 0) >= val:
                return
            if k not in deps or deps[k][1] < val:
                deps[k] = (sem, val)
        for t in reads:
            add(t.w)
        for t in writes:
            add(t.w)
            for r in t.r:
                add(r)
        return list(deps.values())

    def _apply_waits(self, e, deps, instr_fn):
        for sem, val in deps[:-1]:
            e.raw.wait_ge(sem, val)
            e.waited[id(sem)] = val
        ins = instr_fn()
        if deps:
            sem, val = deps[-1]
            ins.wait_op(sem, val, "sem-ge")
            e.waited[id(sem)] = val
        return ins

    def op(self, eng, fn, reads=(), writes=()):
        e = self.engs[eng]
        pr = [t for t in reads if t.psum]
        if pr:
            reads = [t for t in reads if not t.psum]
            writes = list(writes) + pr
        deps = self._deps(e, reads, writes)
        ins = self._apply_waits(e, deps, fn)
        e.count += 1
        ins.then_inc(e.sem, 1)
        tag = (e.sem, e.count, e.name)
        for t in reads:
            t.r.append(tag)
        for t in writes:
            t.w = tag
            t.r = []
        return ins

    def dma(self, q, out, in_, reads=(), writes=(), **kw):
        e = self.engs[q]
        i = self.dma_count[q]
        self.dma_count[q] = i + 1
        sem = self.dma_sems[q][i % self.NDMA]
        val = 16 * (i // self.NDMA + 1)
        deps = self._deps(e, reads, writes)
        if i >= self.NDMA:
            pv = val - 16
            if e.waited.get(id(sem), 0) < pv:
                mx = max([pv] + [d[1] for d in deps if d[0] is sem])
                deps = [d for d in deps if d[0] is not sem] + [(sem, mx)]
        ins = self._apply_waits(e, deps, lambda: e.raw.dma_start(out=out, in_=in_, **kw))
        ins.then_inc(sem, 16)
        tag = (sem, val, "dma_" + q)
        for t in reads:
            t.r.append(tag)
        for t in writes:
            t.w = tag
            t.r = []
        return ins

    def barrier(self):
        targets = [(e.sem, e.count) for e in self.engs.values() if e.count > 0]
        for q in self.dma_sems:
            n = self.dma_count[q]
            for j, sem in enumerate(self.dma_sems[q]):
                cnt = (n - j + self.NDMA - 1) // self.NDMA if n > j else 0
                if cnt > 0:
                    targets.append((sem, 16 * cnt))
        for e in self.engs.values():
            for sem, val in targets:
                if sem is e.sem and e.name == "pe":
                    continue
                if e.waited.get(id(sem), 0) < val:
                    e.raw.wait_ge(sem, val)
                    e.waited[id(sem)] = val


class Phase:
    def __init__(self, K):
        self.K = K
        self.st = ExitStack()

    def __enter__(self):
        self.st.__enter__()
        return self

    def __exit__(self, *a):
        self.K.fw.barrier()
        return self.st.__exit__(*a)

    def sb(self, name, shape, dt):
        return self.st.enter_context(self.K.nc.sbuf_tensor(self.K.fw.name(name), list(shape), dt))

    def ps(self, name, shape, dt):
        full = 512 if dt == F32 else 1024
        t = self.st.enter_context(self.K.nc.psum_tensor(self.K.fw.name(name), [128, full], dt))
        n = 1
        for d in shape[1:]:
            n *= d
        assert shape[0] == 128 and n <= full
        v = t[:, 0:n]
        if len(shape) == 3:
            v = v.rearrange("p (a b) -> p a b", a=shape[1])
        return v


class Rot:
    def __init__(self, ph, name, shape, dt, n, psum=False):
        mk = ph.ps if psum else ph.sb
        self.bufs = [(mk(name, shape, dt), Trk(psum)) for _ in range(n)]
        self.i = 0

    def next(self):
        b = self.bufs[self.i % len(self.bufs)]
        self.i += 1
        return b


def t5_bucket_np(rel):
    n = np.maximum(rel, 0)
    nf = np.maximum(n, 1).astype(np.float32)
    large = 16 + (np.log(nf / np.float32(16)) / np.float32(math.log(128 / 16)) * np.float32(16)).astype(np.int32)
    large = np.minimum(large, 31)
    return np.where(n < 16, n, large)


C_IDENT, C_TRIT, C_TRIU, C_ONES, C_MASK, C_TRITBD, C_TRIMID, C_TRIUBD, C_MASKBD = [i * 128 for i in range(9)]
C_CHI = 9 * 128
C_EEI = C_CHI + 2
NCONST = C_EEI + 768


def make_consts():
    c = np.zeros((128, NCONST), np.float32)
    r = np.arange(128)[:, None]
    t = np.arange(128)[None, :]
    same = (r // 64) == (t // 64)
    c[:, C_IDENT:C_IDENT + 128] = (r == t)
    c[:, C_TRIT:C_TRIT + 128] = (r <= t)
    c[:, C_TRIU:C_TRIU + 128] = (r > t)
    c[:, C_ONES:C_ONES + 128] = 1.0
    c[:, C_MASK:C_MASK + 128] = (r <= t)
    c[:, C_TRITBD:C_TRITBD + 128] = (r <= t) & same
    mid = (t // 64) * 64 + 31
    c[:, C_TRIMID:C_TRIMID + 128] = (((r <= t).astype(np.float32) - (r <= mid).astype(np.float32)) * same)
    c[:, C_TRIUBD:C_TRIUBD + 128] = (r > t) & same
    c[:, C_MASKBD:C_MASKBD + 128] = (r <= t) & same
    c[:, C_CHI] = (np.arange(128) < 64)
    c[:, C_CHI + 1] = (np.arange(128) >= 64)
    k = np.arange(128)[:, None]
    col = np.arange(128)[None, :]
    rel0 = col - k
    idx0 = np.where(rel0 >= 0, t5_bucket_np(rel0), -1)
    idx1 = t5_bucket_np(128 + col - k)
    c[:, C_EEI:C_EEI + 128] = idx0
    c[:, C_EEI + 128:C_EEI + 256] = idx1
    c[:, C_EEI + 256:C_EEI + 768] = 31
    return c


class KCtx:
    pass


def rms_rstd(K, ph, src_ap, src_trk, n, junk, rstd, eps=EPS):
    nc, fw = K.nc, K.fw
    jb, jt = junk
    rb, rt = rstd
    fw.op("act", lambda: nc.scalar.activation(out=jb, in_=src_ap, func=AF.Square, accum_out=rb),
          reads=[src_trk], writes=[jt, rt])
    fw.op("act", lambda: nc.scalar.activation(out=rb, in_=rb, func=AF.Sqrt, scale=1.0 / n, bias=eps),
          reads=[rt], writes=[rt])
    fw.op("dve", lambda: nc.vector.reciprocal(out=rb, in_=rb), reads=[rt], writes=[rt])


def load_row(K, ph, name, src_1d, n, q="sp"):
    t = ph.sb(name, [128, n], F32)
    tr = Trk()
    K.fw.dma(q, t[:], src_1d.partition_broadcast(128), writes=[tr])
    return t, tr


def norm_transpose_group(K, ph, g, xg, t_xg, nwrow, t_nw, xnT, t_xnT, R):
    nc, fw = K.nc, K.fw
    for i in range(4):
        junk = R["junk"].next()
        rstd = R["rstd"].next()
        rms_rstd(K, ph, xg[:, i, :], t_xg[i], D, (junk[0][:], junk[1]), (rstd[0][:], rstd[1]))
        xn, t_xn = R["xn"].next()
        fw.op("dve", lambda: nc.vector.scalar_tensor_tensor(out=xn[:], in0=xg[:, i, :], scalar=rstd[0][:], in1=nwrow[:],
                                                            op0=ALU.mult, op1=ALU.mult),
              reads=[t_xg[i], rstd[1], t_nw], writes=[t_xn])
        pT, t_pT = R["pT"].next()
        for k in range(8):
            fw.op("pe", lambda: nc.tensor.transpose(out=pT[:, k, :], in_=xn[:, k * 128:(k + 1) * 128], identity=K.identb[:]),
                  reads=[t_xn, K.t_const], writes=[t_pT])
        fw.op("act", lambda: nc.scalar.copy(out=xnT[:, :, i * 128:(i + 1) * 128], in_=pT[:]), reads=[t_pT], writes=[t_xnT])


def convert_wi(K, l, which):
    fw = K.fw
    slot = (which - 1) % 2
    wi = (K.ffn1_wi if which == 1 else K.ffn2_wi)[l]
    wi_v = wi.rearrange("(k p) n -> p k n", p=128)
    for j in range(11):
        dst = K.WIB[slot, j].rearrange("p (k a n) -> p k a n", k=8, a=2)
        fw.dma("pool", dst[:, :, 0, :], wi_v[:, :, j * 256:(j + 1) * 256], writes=[K.t_WIB[slot][j]])
        fw.dma("pool", dst[:, :, 1, :], wi_v[:, :, DFF + j * 256:DFF + (j + 1) * 256], writes=[K.t_WIB[slot][j]])
    K.wib_ready[(l, which)] = slot


def token_phase(K, l, which, xin, t_xin, xout, t_xout):
    nc, fw = K.nc, K.fw
    wi = (K.ffn1_wi if which == 1 else K.ffn2_wi)[l]
    wo = (K.ffn1_wo if which == 1 else K.ffn2_wo)[l]
    npre, npost = (0, 1) if which == 1 else (4, 5)
    with Phase(K) as ph:
        nw_pre, t_nwpre = load_row(K, ph, "nwpre", K.norm_w[l, npre], D)
        nw_post, t_nwpost = load_row(K, ph, "nwpost", K.norm_w[l, npost], D)
        wo_sb = ph.sb("wo", [128, 22, D], BF16)
        t_wo = Trk()
        wo_v = wo.rearrange("(k p) n -> p k n", p=128)
        for kk in range(0, 22, 2):
            fw.dma("pool", wo_sb[:, kk:kk + 2, :], wo_v[:, kk:kk + 2, :], writes=[t_wo])
        if which == 2:
            nw3, t_nw3 = load_row(K, ph, "nw3", K.norm_w[l, 3], D)
            wout_sb = ph.sb("wout", [128, 8, D], BF16)
            t_wout = Trk()
            wout_v = K.w_out[l].rearrange("(k p) n -> p k n", p=128)
            for kk in range(0, 8, 2):
                fw.dma("pool", wout_sb[:, kk:kk + 2, :], wout_v[:, kk:kk + 2, :], writes=[t_wout])
            Ry = Rot(ph, "ytile", [128, D], BF16, 2)
            RyT = Rot(ph, "yT", [128, 8, 128], BF16, 2)
        R = {"junk": Rot(ph, "junk", [128, D], F32, 1), "rstd": Rot(ph, "rstd", [128, 1], F32, 4),
             "xn": Rot(ph, "xn", [128, D], BF16, 2), "pT": Rot(ph, "pT", [128, 8, 128], BF16, 2, psum=True)}
        nxg = 2 if which == 1 else 1
        xg_bufs = [(ph.sb("xg", [128, 4, D], F32), [Trk() for _ in range(4)]) for _ in range(nxg)]
        xnT = ph.sb("xnT", [128, 8, 512], BF16)
        t_xnT = Trk()
        aT = ph.sb("aT", [128, 22, 512], BF16)
        t_aT = Trk()
        Rw = Rot(ph, "wipiece", [128, 8, 2, 256], BF16, 3)
        Rpg = Rot(ph, "pg", [128, 512], F32, 2, psum=True)
        Rpu = Rot(ph, "pu", [128, 512], F32, 2, psum=True)
        Rpo = Rot(ph, "po", [128, 512], F32, 2, psum=True)
        Rsg = Rot(ph, "sg", [128, 512], F32, 2)
        Rh = Rot(ph, "h", [128, D], F32, 2)
        wi_v = wi.rearrange("(k p) n -> p k n", p=128)

        wslot = K.wib_ready.get((l, which))

        def load_piece(n):
            j = n % 11
            wb, t_wb = Rw.next()
            if wslot is not None:
                fw.dma("sp", wb[:].rearrange("p k a n -> p (k a n)"), K.WIB[wslot, j], reads=[K.t_WIB[wslot][j]], writes=[t_wb])
            else:
                fw.dma("pool", wb[:, :, 0, :], wi_v[:, :, j * 256:(j + 1) * 256], writes=[t_wb])
                fw.dma("pool", wb[:, :, 1, :], wi_v[:, :, DFF + j * 256:DFF + (j + 1) * 256], writes=[t_wb])
            return wb, t_wb

        pieces = {}
        NP = NG * 11
        pieces[0] = load_piece(0)
        pieces[1] = load_piece(1)

        def load_x(g):
            xg, t_tiles = xg_bufs[g % nxg]
            for i in range(4):
                r0 = g * 512 + i * 128
                fw.dma("sp", xg[:, i, :], xin[r0:r0 + 128, :], reads=[t_xin[g]], writes=[t_tiles[i]])
            return xg, t_tiles

        cur = load_x(0)
        for g in range(NG):
            xg, t_xg = cur
            if which == 2:
                for i in range(4):
                    r0 = g * 512 + i * 128
                    yt, t_yt = Ry.next()
                    fw.dma("sp", yt[:], K.Y[r0:r0 + 128, :], reads=[K.t_Y[g]], writes=[t_yt])
                    pT, t_pT = R["pT"].next()
                    for k in range(8):
                        fw.op("pe", lambda: nc.tensor.transpose(out=pT[:, k, :], in_=yt[:, k * 128:(k + 1) * 128], identity=K.identb[:]),
                              reads=[t_yt, K.t_const], writes=[t_pT])
                    yT, t_yT = RyT.next()
                    fw.op("act", lambda: nc.scalar.copy(out=yT[:], in_=pT[:]), reads=[t_pT], writes=[t_yT])
                    h, t_h = Rh.next()
                    for hf in range(2):
                        po, t_po = Rpo.next()
                        for k in range(8):
                            fw.op("pe", lambda: nc.tensor.matmul(out=po[:], lhsT=yT[:, k, :], rhs=wout_sb[:, k, hf * 512:(hf + 1) * 512],
                                                                 start=(k == 0), stop=(k == 7)),
                                  reads=[t_yT, t_wout], writes=[t_po])
                        fw.op("act", lambda: nc.scalar.copy(out=h[:, hf * 512:(hf + 1) * 512], in_=po[:]), reads=[t_po], writes=[t_h])
                    junk = R["junk"].next()
                    rstd = R["rstd"].next()
                    rms_rstd(K, ph, h[:], t_h, D, (junk[0][:], junk[1]), (rstd[0][:], rstd[1]))
                    fw.op("dve", lambda: nc.vector.scalar_tensor_tensor(out=h[:], in0=h[:], scalar=rstd[0][:], in1=nw3[:],
                                                                        op0=ALU.mult, op1=ALU.mult),
                          reads=[t_h, rstd[1], t_nw3], writes=[t_h])
                    fw.op("dve", lambda: nc.vector.tensor_tensor(out=xg[:, i, :], in0=h[:], in1=xg[:, i, :], op=ALU.add),
                          reads=[t_h, t_xg[i]], writes=[t_xg[i]])
            norm_transpose_group(K, ph, g, xg, t_xg, nw_pre, t_nwpre, xnT, t_xnT, R)
            if g + 1 < NG and nxg == 2:
                cur = load_x(g + 1)
            for j in range(11):
                n = g * 11 + j
                if n + 2 < NP:
                    pieces[n + 2] = load_piece(n + 2)
                wb, t_wb = pieces.pop(n)
                for c in range(2):
                    pg, t_pg = Rpg.next()
                    pu, t_pu = Rpu.next()
                    for k in range(8):
                        fw.op("pe", lambda: nc.tensor.matmul(out=pg[:], lhsT=wb[:, k, 0, c * 128:(c + 1) * 128], rhs=xnT[:, k, :],
                                                             start=(k == 0), stop=(k == 7)),
                              reads=[t_wb, t_xnT], writes=[t_pg])
                    for k in range(8):
                        fw.op("pe", lambda: nc.tensor.matmul(out=pu[:], lhsT=wb[:, k, 1, c * 128:(c + 1) * 128], rhs=xnT[:, k, :],
                                                             start=(k == 0), stop=(k == 7)),
                              reads=[t_wb, t_xnT], writes=[t_pu])
                    sg, t_sg = Rsg.next()
                    fw.op("act", lambda: nc.scalar.activation(out=sg[:], in_=pg[:], func=AF.Silu), reads=[t_pg], writes=[t_sg])
                    fw.op("dve", lambda: nc.vector.tensor_tensor(out=aT[:, j * 2 + c, :], in0=sg[:], in1=pu[:], op=ALU.mult),
                          reads=[t_sg, t_pu], writes=[t_aT])
            for i in range(4):
                r0 = g * 512 + i * 128
                h, t_h = Rh.next()
                for hf in range(2):
                    po, t_po = Rpo.next()
                    for k in range(22):
                        fw.op("pe", lambda: nc.tensor.matmul(out=po[:], lhsT=aT[:, k, i * 128:(i + 1) * 128], rhs=wo_sb[:, k, hf * 512:(hf + 1) * 512],
                                                             start=(k == 0), stop=(k == 21)),
                              reads=[t_aT, t_wo], writes=[t_po])
                    fw.op("act", lambda: nc.scalar.copy(out=h[:, hf * 512:(hf + 1) * 512], in_=po[:]), reads=[t_po], writes=[t_h])
                junk = R["junk"].next()
                rstd = R["rstd"].next()
                rms_rstd(K, ph, h[:], t_h, D, (junk[0][:], junk[1]), (rstd[0][:], rstd[1]))
                fw.op("dve", lambda: nc.vector.scalar_tensor_tensor(out=h[:], in0=h[:], scalar=rstd[0][:], in1=nw_post[:],
                                                                    op0=ALU.mult, op1=ALU.mult),
                      reads=[t_h, rstd[1], t_nwpost], writes=[t_h])
                fw.op("dve", lambda: nc.vector.scalar_tensor_tensor(out=h[:], in0=h[:], scalar=0.5, in1=xg[:, i, :],
                                                                    op0=ALU.mult, op1=ALU.add),
                      reads=[t_h, t_xg[i]], writes=[t_h])
                fw.dma("sp", xout[r0:r0 + 128, :], h[:], reads=[t_h], writes=[t_xout[g]])
            if g + 1 < NG and nxg == 1:
                cur = load_x(g + 1)


def proj_phase(K, l, xsrc=None, t_xsrc=None):
    nc, fw = K.nc, K.fw
    if xsrc is None:
        xsrc, t_xsrc = K.X, K.t_X
    with Phase(K) as ph:
        nw2, t_nw2 = load_row(K, ph, "nw2", K.norm_w[l, 2], D)
        win_sb = ph.sb("win", [128, 8, NIN], BF16)
        t_win = Trk()
        win_v = K.w_in[l].rearrange("(k p) n -> p k n", p=128)
        for k in range(8):
            fw.dma("pool", win_sb[:, k, :], win_v[:, k, :], writes=[t_win])
        if not os.environ.get("NO_WIB"):
            convert_wi(K, l, 2)
            if l + 1 < K.n_layers:
                convert_wi(K, l + 1, 1)
        pp = ph.sb("pp", [128, 28], F32)
        t_pp = Trk()
        fw.dma("sp", pp[:], K.pp[l], writes=[t_pp])
        gb = ph.sb("gb", [128, 8], F32)
        t_gb = Trk()
        fw.dma("sp", gb[:, 0:4], K.ig_b[l].partition_broadcast(128), writes=[t_gb])
        fw.dma("sp", gb[:, 4:8], K.fg_b[l].partition_broadcast(128), writes=[t_gb])
        fw.op("dve", lambda: nc.vector.tensor_scalar(out=gb[:, 0:4], in0=gb[:, 0:4], scalar1=LN8, scalar2=None, op0=ALU.add),
              reads=[t_gb], writes=[t_gb])
        lgr = ph.sb("lgr", [128, 4, 256], F32)
        t_lgr = Trk()
        fw.dma("sp", lgr[:].rearrange("p a b -> p (a b)"), K.lb_logits.rearrange("a b -> (a b)").partition_broadcast(128), writes=[t_lgr])
        fw.op("act", lambda: nc.scalar.activation(out=lgr[:], in_=lgr[:], func=AF.Exp), reads=[t_lgr], writes=[t_lgr])
        lb_row = ph.sb("lb_row", [128, 256], F32)
        oml_row = ph.sb("oml_row", [128, 256], F32)
        tmp_row = ph.sb("tmp_row", [128, 256], F32)
        t_lb = Trk()
        fw.op("dve", lambda: nc.vector.tensor_tensor(out=tmp_row[:], in0=lgr[:, 0, :], in1=lgr[:, 1, :], op=ALU.add), reads=[t_lgr], writes=[t_lb])
        fw.op("dve", lambda: nc.vector.tensor_tensor(out=tmp_row[:], in0=tmp_row[:], in1=lgr[:, 2, :], op=ALU.add), reads=[t_lgr, t_lb], writes=[t_lb])
        fw.op("dve", lambda: nc.vector.tensor_tensor(out=tmp_row[:], in0=tmp_row[:], in1=lgr[:, 3, :], op=ALU.add), reads=[t_lgr, t_lb], writes=[t_lb])
        fw.op("dve", lambda: nc.vector.reciprocal(out=tmp_row[:], in_=tmp_row[:]), reads=[t_lb], writes=[t_lb])
        fw.op("dve", lambda: nc.vector.memset(lb_row[:], 0.0), writes=[t_lb])
        for j in range(1, l + 1):
            fw.op("dve", lambda: nc.vector.tensor_tensor(out=lb_row[:], in0=lb_row[:], in1=lgr[:, j, :], op=ALU.add), reads=[t_lgr, t_lb], writes=[t_lb])
        fw.op("dve", lambda: nc.vector.tensor_tensor(out=lb_row[:], in0=lb_row[:], in1=tmp_row[:], op=ALU.mult), reads=[t_lb], writes=[t_lb])
        fw.op("dve", lambda: nc.vector.tensor_scalar(out=oml_row[:], in0=lb_row[:], scalar1=-1.0, scalar2=1.0, op0=ALU.mult, op1=ALU.add),
              reads=[t_lb], writes=[t_lb])
        lgf = ph.sb("lgf", [128, 2, 4], F32)
        oml_fm = ph.sb("oml_fm", [128, 2], F32)
        tmpf = ph.sb("tmpf", [128, 2], F32)
        t_lf = Trk()
        fw.op("act", lambda: nc.scalar.activation(out=lgf[:], in_=pp[:, 20:28].rearrange("p (a b) -> p a b", a=2), func=AF.Exp),
              reads=[t_pp], writes=[t_lf])
        fw.op("dve", lambda: nc.vector.tensor_reduce(out=tmpf[:], in_=lgf[:], axis=AX.X, op=ALU.add), reads=[t_lf], writes=[t_lf])
        fw.op("dve", lambda: nc.vector.reciprocal(out=tmpf[:], in_=tmpf[:]), reads=[t_lf], writes=[t_lf])
        fw.op("dve", lambda: nc.vector.memset(oml_fm[:], 0.0), writes=[t_lf])
        for j in range(1, l + 1):
            fw.op("dve", lambda: nc.vector.tensor_tensor(out=oml_fm[:], in0=oml_fm[:], in1=lgf[:, :, j], op=ALU.add), reads=[t_lf], writes=[t_lf])
        fw.op("dve", lambda: nc.vector.tensor_tensor(out=oml_fm[:], in0=oml_fm[:], in1=tmpf[:], op=ALU.mult), reads=[t_lf], writes=[t_lf])
        fw.op("dve", lambda: nc.vector.tensor_scalar(out=oml_fm[:], in0=oml_fm[:], scalar1=-1.0, scalar2=1.0, op0=ALU.mult, op1=ALU.add),
              reads=[t_lf], writes=[t_lf])

        R = {"junk": Rot(ph, "junk", [128, D], F32, 1), "rstd": Rot(ph, "rstd", [128, 1], F32, 4),
             "xn": Rot(ph, "xn", [128, D], BF16, 2), "pT": Rot(ph, "pT", [128, 8, 128], BF16, 2, psum=True)}
        xg_bufs = [(ph.sb("xg", [128, 4, D], F32), [Trk() for _ in range(4)]) for _ in range(2)]
        xnT = ph.sb("xnT", [128, 8, 512], BF16)
        t_xnT = Trk()
        xc = ph.sb("xc", [128, 4, 515], F32)
        t_xc = [Trk() for _ in range(4)]
        fw.op("dve", lambda: nc.vector.memset(xc[:], 0.0), writes=t_xc)
        Rpf = Rot(ph, "pf", [128, 512], F32, 2, psum=True)
        Rpt = Rot(ph, "pt", [128, 512], F32, 2, psum=True)
        Rpk = Rot(ph, "pk", [128, 4, 128], BF16, 1, psum=True)
        Racc = Rot(ph, "acc", [128, 512], F32, 2)
        Rob = Rot(ph, "ob", [128, 512], BF16, 3)
        Rkt = Rot(ph, "kt", [128, 4, 128], BF16, 2)
        Rtf = Rot(ph, "tf", [128, 512], F32, 2)
        RtfD = Rot(ph, "tfD", [128, 512], F32, 4)
        Rtb = Rot(ph, "tb", [128, 512], BF16, 3)
        Rg8 = Rot(ph, "g8", [128, 8], F32, 2)
        Rg4 = Rot(ph, "g4", [128, 4], F32, 2)

        def load_x(g):
            xg, t_tiles = xg_bufs[g % 2]
            for i in range(4):
                r0 = g * 512 + i * 128
                fw.dma("sp", xg[:, i, :], xsrc[r0:r0 + 128, :], reads=[t_xsrc[g]], writes=[t_tiles[i]])
            return xg, t_tiles

        cur = load_x(0)
        fchunks = [(ci * 128, "qkm", ci) for ci in range(4)] + [(2568 + 128 * p, "qh", p) for p in range(2)] + \
                  [(1032 + 128 * h, "qd", h) for h in range(4)] + [(1544 + 128 * h, "kd", h) for h in range(4)] + \
                  [(2824 + 128 * p, "fh", p) for p in range(2)]
        for g in range(NG):
            xg, t_xg = cur
            norm_transpose_group(K, ph, g, xg, t_xg, nw2, t_nw2, xnT, t_xnT, R)
            if g + 1 < NG:
                cur = load_x(g + 1)
            cs = slice(g * 512, (g + 1) * 512)
            for (c0, kind, ci) in fchunks:
                pf, t_pf = Rpf.next()
                for k in range(8):
                    fw.op("pe", lambda: nc.tensor.matmul(out=pf[:], lhsT=win_sb[:, k, c0:c0 + 128], rhs=xnT[:, k, :],
                                                         start=(k == 0), stop=(k == 7)),
                          reads=[t_win, t_xnT], writes=[t_pf])
                if kind == "qkm":
                    fw.op("act", lambda: nc.scalar.copy(out=xc[:, ci, 3:515], in_=pf[:]), reads=[t_pf], writes=[t_xc[ci]])
                    acc, t_acc = Racc.next()
                    fw.op("dve", lambda: nc.vector.tensor_scalar(out=acc[:], in0=xc[:, ci, 3:515], scalar1=pp[:, ci * 4 + 3:ci * 4 + 4],
                                                                 scalar2=pp[:, 16 + ci:17 + ci], op0=ALU.mult, op1=ALU.add),
                          reads=[t_xc[ci], t_pp], writes=[t_acc])
                    for j in range(3):
                        fw.op("dve", lambda: nc.vector.scalar_tensor_tensor(out=acc[:], in0=xc[:, ci, j:j + 512], scalar=pp[:, ci * 4 + j:ci * 4 + j + 1],
                                                                            in1=acc[:], op0=ALU.mult, op1=ALU.add),
                              reads=[t_xc[ci], t_pp, t_acc], writes=[t_acc])
                    fw.op("dve", lambda: nc.vector.tensor_copy(out=xc[:, ci, 0:3], in_=xc[:, ci, 512:515]), reads=[t_xc[ci]], writes=[t_xc[ci]])
                    ob, t_ob = Rob.next()
                    fw.op("act", lambda: nc.scalar.activation(out=ob[:], in_=acc[:], func=AF.Silu), reads=[t_acc], writes=[t_ob])
                    fw.dma("sp", K.QKM[ci * 128:(ci + 1) * 128, cs], ob[:], reads=[t_ob], writes=[K.t_QKM[g]])
                    if ci >= 2:
                        pk, t_pk = Rpk.next()
                        for i in range(4):
                            fw.op("pe", lambda: nc.tensor.transpose(out=pk[:, i, :], in_=ob[:, i * 128:(i + 1) * 128], identity=K.identb[:]),
                                  reads=[t_ob, K.t_const], writes=[t_pk])
                        kt, t_kt = Rkt.next()
                        fw.op("dve", lambda: nc.vector.tensor_copy(out=kt[:], in_=pk[:]), reads=[t_pk], writes=[t_kt])
                        fw.dma("sp", K.KMT[cs, (ci - 2) * 128:(ci - 1) * 128].rearrange("(i p) c -> p i c", p=128), kt[:],
                               reads=[t_kt], writes=[K.t_KMT[g]])
                elif kind in ("qd", "kd"):
                    ob, t_ob = Rob.next()
                    if ci % 2 == 0:
                        fw.op("act", lambda: nc.scalar.copy(out=ob[:], in_=pf[:]), reads=[t_pf], writes=[t_ob])
                    else:
                        fw.op("dve", lambda: nc.vector.tensor_copy(out=ob[:], in_=pf[:]), reads=[t_pf], writes=[t_ob])
                    dst, tr = (K.QD, K.t_QD) if kind == "qd" else (K.KD, K.t_KD)
                    fw.dma("sp", dst[ci * 128:(ci + 1) * 128, cs], ob[:], reads=[t_ob], writes=[tr[g]])
                elif kind == "qh":
                    ob, t_ob = Rob.next()
                    fw.op("act", lambda: nc.scalar.activation(out=ob[:], in_=pf[:], func=AF.Silu), reads=[t_pf], writes=[t_ob])
                    fw.dma("sp", K.QH[ci * 128:(ci + 1) * 128, cs], ob[:], reads=[t_ob], writes=[K.t_QH[g]])
                else:
                    acc, t_acc = Racc.next()
                    fw.op("act", lambda: nc.scalar.activation(out=acc[:], in_=pf[:], func=AF.Sigmoid, scale=-1.0), reads=[t_pf], writes=[t_acc])
                    ob, t_ob = Rob.next()
                    fw.op("dve", lambda: nc.vector.tensor_scalar(out=ob[:], in0=acc[:], scalar1=oml_fm[:, ci:ci + 1], scalar2=None, op0=ALU.mult),
                          reads=[t_acc, t_lf], writes=[t_ob])
                    fw.dma("sp", K.KH[ci * 128:(ci + 1) * 128, cs], ob[:], reads=[t_ob], writes=[K.t_KH[g]])
            def tok_mm(i, c0, n):
                pt, t_pt = Rpt.next()
                for k in range(8):
                    fw.op("pe", lambda: nc.tensor.matmul(out=pt[:, 0:n], lhsT=xnT[:, k, i * 128:(i + 1) * 128], rhs=win_sb[:, k, c0:c0 + n],
                                                         start=(k == 0), stop=(k == 7)),
                          reads=[t_win, t_xnT], writes=[t_pt])
                return pt, t_pt
            tfD = []
            for i in range(4):
                rs = slice(g * 512 + i * 128, g * 512 + (i + 1) * 128)
                pt, t_pt = tok_mm(i, 512, 512)
                tb, t_tb = Rtb.next()
                fw.op("dve", lambda: nc.vector.tensor_copy(out=tb[:, 0:256], in_=pt[:, 0:256]), reads=[t_pt], writes=[t_tb])
                fw.dma("sp", K.VM[rs, :], tb[:, 0:256], reads=[t_tb], writes=[K.t_VM[g]])
                tf, t_tf = Rtf.next()
                fw.op("act", lambda: nc.scalar.activation(out=tf[:, 0:256], in_=pt[:, 256:512], func=AF.Sigmoid), reads=[t_pt], writes=[t_tf])
                fw.dma("sp", K.OM[rs, :], tf[:, 0:256], reads=[t_tf], writes=[K.t_OM[g]])
                pt, t_pt = tok_mm(i, 2824, 512)
                tf, t_tf = RtfD.next()
                fw.op("act", lambda: nc.scalar.activation(out=tf[:, 0:256], in_=pt[:, 0:256], func=AF.Sigmoid), reads=[t_pt], writes=[t_tf])
                fw.op("dve", lambda: nc.vector.tensor_tensor(out=tf[:, 0:256], in0=tf[:, 0:256], in1=oml_row[:], op=ALU.mult), reads=[t_tf, t_lb], writes=[t_tf])
                fw.op("dve", lambda: nc.vector.tensor_tensor(out=tf[:, 0:256], in0=tf[:, 0:256], in1=lb_row[:], op=ALU.add), reads=[t_tf, t_lb], writes=[t_tf])
                fw.op("dve", lambda: nc.vector.tensor_scalar(out=tf[:, 256:512], in0=tf[:, 0:256], scalar1=-1.0, scalar2=1.0, op0=ALU.mult, op1=ALU.add),
                      reads=[t_tf], writes=[t_tf])
                fw.dma("sp", K.KHT[rs, :], tf[:, 256:512], reads=[t_tf], writes=[K.t_KHT[g]])
                tb, t_tb = Rtb.next()
                fw.op("dve", lambda: nc.vector.tensor_copy(out=tb[:, 0:256], in_=pt[:, 256:512]), reads=[t_pt], writes=[t_tb])
                fw.dma("sp", K.VH[rs, :], tb[:, 0:256], reads=[t_tb], writes=[K.t_VH[g]])
                tfD.append((tf, t_tf))
            for i in range(4):
                rs = slice(g * 512 + i * 128, g * 512 + (i + 1) * 128)
                tf, t_tf = tfD[i]
                fw.op("act", lambda: nc.scalar.activation(out=tf[:, 0:256], in_=tf[:, 0:256], func=AF.Ln), reads=[t_tf], writes=[t_tf])
                fw.dma("sp", K.LFH[rs, :], tf[:, 0:256], reads=[t_tf], writes=[K.t_LFH[g]])
                pt, t_pt = tok_mm(i, 1024, 8)
                g8, t_g8 = Rg8.next()
                fw.op("dve", lambda: nc.vector.tensor_tensor(out=g8[:], in0=pt[:, 0:8], in1=gb[:], op=ALU.add), reads=[t_pt, t_gb], writes=[t_g8])
                g4, t_g4 = Rg4.next()
                fw.op("act", lambda: nc.scalar.activation(out=g4[:], in_=g8[:, 4:8], func=AF.Exp, scale=-1.0), reads=[t_g8], writes=[t_g4])
                fw.op("act", lambda: nc.scalar.activation(out=g4[:], in_=g4[:], func=AF.Ln, bias=1.0), reads=[t_g4], writes=[t_g4])
                fw.op("dve", lambda: nc.vector.tensor_scalar(out=g8[:, 4:8], in0=g4[:], scalar1=-1.0, scalar2=None, op0=ALU.mult),
                      reads=[t_g4, t_g8], writes=[t_g8])
                fw.dma("sp", K.GM[rs, :], g8[:], reads=[t_g8], writes=[K.t_GM[g]])
                pt, t_pt = tok_mm(i, 2056, 512)
                tb, t_tb = Rtb.next()
                fw.op("act", lambda: nc.scalar.copy(out=tb[:], in_=pt[:]), reads=[t_pt], writes=[t_tb])
                fw.dma("sp", K.VD[rs, :], tb[:], reads=[t_tb], writes=[K.t_VD[g]])
            for i in range(4):
                rs = slice(g * 512 + i * 128, g * 512 + (i + 1) * 128)
                pt, t_pt = tok_mm(i, 3336, 256)
                tf, t_tf = Rtf.next()
                fw.op("act", lambda: nc.scalar.activation(out=tf[:, 0:256], in_=pt[:, 0:256], func=AF.Silu), reads=[t_pt], writes=[t_tf])
                fw.dma("sp", K.GH[rs, :], tf[:, 0:256], reads=[t_tf], writes=[K.t_GH[g]])


def bview(ap, shape):
    return ap.unsqueeze(2).to_broadcast(shape)


def mlstm_phase(K, l):
    nc, fw = K.nc, K.fw
    cf = K.constf
    with Phase(K) as ph:
        qT = ph.sb("mqT", [128, 2, S], BF16)
        kT = ph.sb("mkT", [128, 2, S], BF16)
        ktok = ph.sb("mktok", [128, NT, 256], BF16)
        vaug = ph.sb("mvaug", [128, NT, 4, 66], BF16)
        gm = ph.sb("mgm", [128, NT, 8], F32)
        t_in = [Trk() for _ in range(NG)]
        t_gm = Trk()
        fw.op("pool", lambda: nc.gpsimd.memset(vaug[:], 1.0), writes=t_in)
        for g_ in range(NG):
            fw.dma("sp", gm[:, g_ * 4:(g_ + 1) * 4, :], K.GM[g_ * 512:(g_ + 1) * 512, :].rearrange("(c p) g -> p c g", p=128), reads=[K.t_GM[g_]], writes=[t_gm])
        mnw, t_mnw = load_row(K, ph, "mnw", K.m_nw[l], 256)
        for g in range(NG):
            cs = slice(g * 512, (g + 1) * 512)
            ts = slice(g * 4, (g + 1) * 4)
            for p in range(2):
                fw.dma("sp", qT[:, p, cs], K.QKM[p * 128:(p + 1) * 128, cs], reads=[K.t_QKM[g]], writes=[t_in[g]])
                fw.dma("sp", kT[:, p, cs], K.QKM[256 + p * 128:256 + (p + 1) * 128, cs], reads=[K.t_QKM[g]], writes=[t_in[g]])
            fw.dma("sp", ktok[:, ts, :], K.KMT[cs, :].rearrange("(c p) f -> p c f", p=128), reads=[K.t_KMT[g]], writes=[t_in[g]])
            for c4 in range(4):
                cc_ = g * 4 + c4
                fw.dma("sp", vaug[:, cc_, :, 0:64], K.VM[cc_ * 128:(cc_ + 1) * 128, :].rearrange("p (h e) -> p h e", h=4), reads=[K.t_VM[g]], writes=[t_in[g]])
        lfc = ph.sb("lfc", [128, 128], F32)
        lic = ph.sb("lic", [128, 128], F32)
        eb = ph.sb("eb", [128, 128], F32)
        al = ph.sb("al", [128, 128], F32)
        wa = ph.sb("wa", [128, 128], F32)
        ebL = ph.sb("ebL", [128, 128], F32)
        ebL2 = ph.sb("ebL2", [128, NT, 2], F32)
        t_g = Trk()
        fw.op("dve", lambda: nc.vector.tensor_copy(out=lfc[:].rearrange("p (c h) -> p c h", h=4), in_=gm[:, :, 4:8]), reads=[t_gm], writes=[t_g])
        fw.op("dve", lambda: nc.vector.tensor_copy(out=lic[:].rearrange("p (c h) -> p c h", h=4), in_=gm[:, :, 0:4]), reads=[t_gm], writes=[t_g])
        Rpp = Rot(ph, "mpp", [128, 128], F32, 1, psum=True)
        pp_, t_pp_ = Rpp.next()
        fw.op("pe", lambda: nc.tensor.matmul(out=pp_[:], lhsT=cf[:, C_TRIT:C_TRIT + 128], rhs=lfc[:], start=True, stop=True), reads=[K.t_const, t_g], writes=[t_pp_])
        fw.op("act", lambda: nc.scalar.activation(out=eb[:], in_=pp_[:], func=AF.Exp), reads=[t_pp_], writes=[t_g])
        fw.op("dve", lambda: nc.vector.tensor_tensor(out=al[:], in0=lic[:], in1=pp_[:], op=ALU.subtract), reads=[t_pp_, t_g], writes=[t_g])
        fw.op("act", lambda: nc.scalar.activation(out=al[:], in_=al[:], func=AF.Exp), reads=[t_g], writes=[t_g])
        fw.op("pe", lambda: nc.tensor.matmul(out=pp_[:], lhsT=cf[:, C_TRIU:C_TRIU + 128], rhs=lfc[:], start=True, stop=True), reads=[K.t_const, t_g], writes=[t_pp_])
        fw.op("dve", lambda: nc.vector.tensor_tensor(out=wa[:], in0=lic[:], in1=pp_[:], op=ALU.add), reads=[t_pp_, t_g], writes=[t_g])
        fw.op("act", lambda: nc.scalar.activation(out=wa[:], in_=wa[:], func=AF.Exp), reads=[t_g], writes=[t_g])
        fw.op("pe", lambda: nc.tensor.matmul(out=pp_[:], lhsT=cf[:, C_ONES:C_ONES + 128], rhs=lfc[:], start=True, stop=True), reads=[K.t_const, t_g], writes=[t_pp_])
        fw.op("act", lambda: nc.scalar.activation(out=ebL[:], in_=pp_[:], func=AF.Exp), reads=[t_pp_], writes=[t_g])
        ebL4 = ebL[:].rearrange("p (c q h) -> p c q h", q=2, h=2)
        fw.op("dve", lambda: nc.vector.tensor_copy(out=ebL2[0:64], in_=ebL4[0:64, :, :, 0]), reads=[t_g], writes=[t_g])
        fw.op("dve", lambda: nc.vector.tensor_copy(out=ebL2[64:128], in_=ebL4[64:128, :, :, 1]), reads=[t_g], writes=[t_g])
        Cst = [ph.sb("mC", [128, 132], F32) for _ in range(2)]
        t_C = [Trk() for _ in range(2)]
        RCb = [Rot(ph, "mCb", [128, 132], BF16, 2) for _ in range(2)]
        cb = []
        for p in range(2):
            fw.op("dve", lambda: nc.vector.memset(Cst[p][:], 0.0), writes=[t_C[p]])
            b_, t_b = RCb[p].next()
            fw.op("dve", lambda: nc.vector.memset(b_[:], 0.0), writes=[t_b])
            cb.append((b_, t_b))
        Rps = Rot(ph, "mps", [128, 4, 128], F32, 2, psum=True)
        Rpn = Rot(ph, "mpn", [128, 4, 65], F32, 2, psum=True)
        Rpc = Rot(ph, "mpc", [128, 132], F32, 2, psum=True)
        RscT = Rot(ph, "mscT", [128, 4, 128], BF16, 2)
        Rs4 = Rot(ph, "ms4", [128, 4], F32, 8)
        Rhm = Rot(ph, "mhm", [128, 4, 64], F32, 2)
        Rsq = Rot(ph, "msq", [128, 4, 64], F32, 2)
        Rom = Rot(ph, "mom", [128, 256], F32, 2)
        Ryb = Rot(ph, "myb", [128, 256], BF16, 2)
        Rkw = Rot(ph, "mkw", [128, 256], BF16, 2)
        for c in range(NT):
            g = c // 4
            cs = slice(c * 128, (c + 1) * 128)
            om, t_om = Rom.next()
            fw.dma("sp", om[:], K.OM[cs, :], reads=[K.t_OM[g]], writes=[t_om])
            ps, t_ps = Rps.next()
            for h in range(4):
                p, hp = h // 2, h % 2
                rr = slice(hp * 64, (hp + 1) * 64)
                fw.op("pe", lambda: nc.tensor.matmul(out=ps[:, h, :], lhsT=kT[rr, p, cs], rhs=qT[rr, p, cs], start=True, stop=True),
                      reads=[t_in[g]], writes=[t_ps])
            scT, t_scT = RscT.next()
            for h in range(4):
                fw.op("dve", lambda: nc.vector.scalar_tensor_tensor(out=scT[:, h, :], in0=ps[:, h, :], scalar=al[:, c * 4 + h:c * 4 + h + 1],
                                                                    in1=cf[:, C_MASK:C_MASK + 128], op0=ALU.mult, op1=ALU.mult),
                      reads=[t_ps, t_g, K.t_const], writes=[t_scT])
            pn, t_pn = Rpn.next()
            for h in range(4):
                p, hp = h // 2, h % 2
                rr = slice(hp * 64, (hp + 1) * 64)
                fw.op("pe", lambda: nc.tensor.matmul(out=pn[:, h, :], lhsT=scT[:, h, :], rhs=vaug[:, c, h, 0:65], start=True, stop=False),
                      reads=[t_scT, t_in[g]], writes=[t_pn])
                fw.op("pe", lambda: nc.tensor.matmul(out=pn[:, h, :], lhsT=qT[rr, p, cs], rhs=cb[p][0][rr, hp * 66:hp * 66 + 65], start=False, stop=True),
                      reads=[t_in[g], cb[p][1]], writes=[t_pn])
            eb4 = eb[:, c * 4:(c + 1) * 4]
            d4, t_d4 = Rs4.next()
            fw.op("dve", lambda: nc.vector.tensor_tensor(out=d4[:], in0=pn[:, :, 64], in1=eb4, op=ALU.mult), reads=[t_pn, t_g], writes=[t_d4])
            n4, t_n4 = Rs4.next()
            fw.op("dve", lambda: nc.vector.tensor_scalar(out=n4[:], in0=d4[:], scalar1=-1.0, scalar2=None, op0=ALU.mult), reads=[t_d4], writes=[t_n4])
            fw.op("dve", lambda: nc.vector.scalar_tensor_tensor(out=d4[:], in0=d4[:], scalar=1.0, in1=n4[:], op0=ALU.max, op1=ALU.max), reads=[t_d4, t_n4], writes=[t_d4])
            fw.op("dve", lambda: nc.vector.reciprocal(out=d4[:], in_=d4[:]), reads=[t_d4], writes=[t_d4])
            fw.op("dve", lambda: nc.vector.tensor_tensor(out=d4[:], in0=d4[:], in1=eb4, op=ALU.mult), reads=[t_d4, t_g], writes=[t_d4])
            hm, t_hm = Rhm.next()
            fw.op("dve", lambda: nc.vector.tensor_tensor(out=hm[:], in0=pn[:, :, 0:64], in1=bview(d4[:], [128, 4, 64]), op=ALU.mult),
                  reads=[t_pn, t_d4], writes=[t_hm])
            m4, t_m4 = Rs4.next()
            fw.op("dve", lambda: nc.vector.tensor_reduce(out=m4[:], in_=hm[:], axis=AX.X, op=ALU.add), reads=[t_hm], writes=[t_m4])
            fw.op("dve", lambda: nc.vector.tensor_scalar(out=m4[:], in0=m4[:], scalar1=-1.0 / 64, scalar2=None, op0=ALU.mult), reads=[t_m4], writes=[t_m4])
            fw.op("dve", lambda: nc.vector.tensor_tensor(out=hm[:], in0=hm[:], in1=bview(m4[:], [128, 4, 64]), op=ALU.add), reads=[t_hm, t_m4], writes=[t_hm])
            sq, t_sq = Rsq.next()
            fw.op("pool", lambda: nc.gpsimd.tensor_tensor(out=sq[:], in0=hm[:], in1=hm[:], op=ALU.mult), reads=[t_hm], writes=[t_sq])
            v4, t_v4 = Rs4.next()
            fw.op("dve", lambda: nc.vector.tensor_reduce(out=v4[:], in_=sq[:], axis=AX.X, op=ALU.add), reads=[t_sq], writes=[t_v4])
            fw.op("act", lambda: nc.scalar.activation(out=v4[:], in_=v4[:], func=AF.Ln, scale=1.0 / 64, bias=EPS), reads=[t_v4], writes=[t_v4])
            fw.op("act", lambda: nc.scalar.activation(out=v4[:], in_=v4[:], func=AF.Exp, scale=-0.5), reads=[t_v4], writes=[t_v4])
            fw.op("dve", lambda: nc.vector.tensor_tensor(out=hm[:], in0=hm[:], in1=bview(v4[:], [128, 4, 64]), op=ALU.mult), reads=[t_hm, t_v4], writes=[t_hm])
            hm2 = hm[:].rearrange("p h e -> p (h e)")
            fw.op("pool", lambda: nc.gpsimd.tensor_tensor(out=hm2, in0=hm2, in1=mnw[:], op=ALU.mult), reads=[t_hm, t_mnw], writes=[t_hm])
            yb, t_yb = Ryb.next()
            fw.op("dve", lambda: nc.vector.tensor_tensor(out=yb[:], in0=hm2, in1=om[:], op=ALU.mult), reads=[t_hm, t_om], writes=[t_yb])
            fw.dma("sp", K.Y[cs, 0:256], yb[:], reads=[t_yb], writes=[K.t_Y[g]])
            if c + 1 < NT:
                kw, t_kw = Rkw.next()
                fw.op("dve", lambda: nc.vector.tensor_tensor(out=kw[:].rearrange("p (h e) -> p h e", h=4), in0=ktok[:, c, :].rearrange("p (h e) -> p h e", h=4),
                                                              in1=bview(wa[:, c * 4:(c + 1) * 4], [128, 4, 64]), op=ALU.mult),
                      reads=[t_in[g], t_g], writes=[t_kw])
                for p in range(2):
                    pc, t_pc = Rpc.next()
                    fw.op("pe", lambda: nc.tensor.matmul(out=pc[:], lhsT=kw[:, p * 128:(p + 1) * 128],
                                                         rhs=vaug[:, c, 2 * p:2 * p + 2, :].rearrange("p a b -> p (a b)"), start=True, stop=True),
                          reads=[t_kw, t_in[g]], writes=[t_pc])
                    fw.op("dve", lambda: nc.vector.scalar_tensor_tensor(out=Cst[p][:], in0=Cst[p][:], scalar=ebL2[:, c, p:p + 1], in1=pc[:],
                                                                        op0=ALU.mult, op1=ALU.add),
                          reads=[t_C[p], t_g, t_pc], writes=[t_C[p]])
                    b_, t_b = RCb[p].next()
                    fw.op("act", lambda: nc.scalar.copy(out=b_[:], in_=Cst[p][:]), reads=[t_C[p]], writes=[t_b])
                    cb[p] = (b_, t_b)


def hgrn_phase(K, l):
    nc, fw = K.nc, K.fw
    cf = K.constf
    with Phase(K) as ph:
        qT = ph.sb("hqT", [128, 2, S], BF16)
        kT = ph.sb("hkT", [128, 2, S], BF16)
        lf = ph.sb("hlf", [128, NT, 256], F32)
        kht = ph.sb("hkht", [128, NT, 256], F32)
        vh = ph.sb("hvh", [128, NT, 256], BF16)
        t_in = [Trk() for _ in range(NG)]
        hnw, t_hnw = load_row(K, ph, "hnw", K.h_nw[l], 256)
        for g in range(NG):
            cs = slice(g * 512, (g + 1) * 512)
            ts = slice(g * 4, (g + 1) * 4)
            for p in range(2):
                fw.dma("sp", qT[:, p, cs], K.QH[p * 128:(p + 1) * 128, cs], reads=[K.t_QH[g]], writes=[t_in[g]])
                fw.dma("sp", kT[:, p, cs], K.KH[p * 128:(p + 1) * 128, cs], reads=[K.t_KH[g]], writes=[t_in[g]])
            fw.dma("sp", lf[:, ts, :], K.LFH[cs, :].rearrange("(c p) f -> p c f", p=128), reads=[K.t_LFH[g]], writes=[t_in[g]])
            fw.dma("sp", kht[:, ts, :], K.KHT[cs, :].rearrange("(c p) f -> p c f", p=128), reads=[K.t_KHT[g]], writes=[t_in[g]])
            fw.dma("sp", vh[:, ts, :], K.VH[cs, :].rearrange("(c p) f -> p c f", p=128), reads=[K.t_VH[g]], writes=[t_in[g]])
        Sst = [ph.sb("hS", [128, 128], F32) for _ in range(2)]
        t_S = [Trk() for _ in range(2)]
        RSb = [Rot(ph, "hSb", [128, 128], BF16, 3) for _ in range(2)]
        sb_ = []
        for p in range(2):
            fw.op("dve", lambda: nc.vector.memset(Sst[p][:], 0.0), writes=[t_S[p]])
            b_, t_b = RSb[p].next()
            fw.op("dve", lambda: nc.vector.memset(b_[:], 0.0), writes=[t_b])
            sb_.append((b_, t_b))
        q01 = [[ph.sb("hq01", [128, 2, 128], BF16) for _ in range(2)] for _ in range(2)]
        t_q01 = [[Trk() for _ in range(2)] for _ in range(2)]
        for r in range(2):
            for j in range(2):
                fw.op("pool", lambda: nc.gpsimd.memset(q01[r][j][:], 0.0), writes=[t_q01[r][j]])
        Rpm = Rot(ph, "hpm", [128, 128], F32, 2, psum=True)
        Rpl = Rot(ph, "hpl", [128, 2], F32, 1, psum=True)
        Rprb = Rot(ph, "hprb", [128, 256], F32, 1, psum=True)
        Rpa = Rot(ph, "hpa", [128, 4, 128], F32, 1, psum=True)
        Rpo = Rot(ph, "hpo", [128, 4, 64], F32, 1, psum=True)
        Rpss = Rot(ph, "hpss", [128, 128], F32, 2, psum=True)
        RE = Rot(ph, "hE", [128, 128], F32, 4)
        Rqt = Rot(ph, "hqt", [128, 2, 128], BF16, 2)
        Rkt = Rot(ph, "hkt", [128, 2, 128], BF16, 2)
        Rebl = Rot(ph, "hebl", [128, 2, 2], F32, 2)
        Rwk = Rot(ph, "hwk", [128, 256], F32, 2)
        Rkp = Rot(ph, "hkp", [128, 256], BF16, 2)
        RAT = Rot(ph, "hAT", [128, 4, 128], BF16, 2)
        Ros = Rot(ph, "hos", [128, 4, 64], F32, 2)
        Rsq = Rot(ph, "hsq", [128, 4, 64], F32, 2)
        Rs4 = Rot(ph, "hs4", [128, 4], F32, 4)
        Rgh = Rot(ph, "hgh", [128, 256], F32, 2)
        Ryb = Rot(ph, "hyb", [128, 256], BF16, 2)
        for i in range(NT):
            g = i // 4
            cs = slice(i * 128, (i + 1) * 128)
            gh, t_gh = Rgh.next()
            fw.dma("sp", gh[:], K.GH[cs, :], reads=[K.t_GH[g]], writes=[t_gh])
            qt, t_qt = Rqt.next()
            kt, t_kt = Rkt.next()
            ebl, t_ebl = Rebl.next()
            q0, q1 = q01[i % 2]
            t_q0, t_q1 = t_q01[i % 2]
            for p in range(2):
                lfp = lf[:, i, p * 128:(p + 1) * 128]
                pm, t_pm = Rpm.next()
                fw.op("pe", lambda: nc.tensor.matmul(out=pm[:], lhsT=lfp, rhs=cf[:, C_TRIMID:C_TRIMID + 128], start=True, stop=True),
                      reads=[t_in[g], K.t_const], writes=[t_pm])
                E1, t_E1 = RE.next()
                E2, t_E2 = RE.next()
                fw.op("act", lambda: nc.scalar.activation(out=E1[:], in_=pm[:], func=AF.Exp), reads=[t_pm], writes=[t_E1])
                fw.op("act", lambda: nc.scalar.activation(out=E2[:], in_=pm[:], func=AF.Exp, scale=-1.0), reads=[t_pm], writes=[t_E2])
                fw.op("dve", lambda: nc.vector.tensor_tensor(out=qt[:, p, :], in0=qT[:, p, cs], in1=E1[:], op=ALU.mult), reads=[t_in[g], t_E1], writes=[t_qt])
                fw.op("pool", lambda: nc.gpsimd.tensor_tensor(out=kt[:, p, :], in0=kT[:, p, cs], in1=E2[:], op=ALU.mult), reads=[t_in[g], t_E2], writes=[t_kt])
                pb, t_pb = Rpm.next()
                fw.op("pe", lambda: nc.tensor.matmul(out=pb[:], lhsT=lfp, rhs=cf[:, C_TRITBD:C_TRITBD + 128], start=True, stop=True),
                      reads=[t_in[g], K.t_const], writes=[t_pb])
                E3, t_E3 = RE.next()
                fw.op("act", lambda: nc.scalar.activation(out=E3[:], in_=pb[:], func=AF.Exp), reads=[t_pb], writes=[t_E3])
                fw.op("dve", lambda: nc.vector.tensor_tensor(out=q0[:, p, 0:64], in0=qT[:, p, i * 128:i * 128 + 64], in1=E3[:, 0:64], op=ALU.mult),
                      reads=[t_in[g], t_E3], writes=[t_q0])
                fw.op("dve", lambda: nc.vector.tensor_tensor(out=q1[:, p, 64:128], in0=qT[:, p, i * 128 + 64:(i + 1) * 128], in1=E3[:, 64:128], op=ALU.mult),
                      reads=[t_in[g], t_E3], writes=[t_q1])
                pl, t_pl = Rpl.next()
                fw.op("pe", lambda: nc.tensor.matmul(out=pl[:], lhsT=lfp, rhs=cf[:, C_CHI:C_CHI + 2], start=True, stop=True),
                      reads=[t_in[g], K.t_const], writes=[t_pl])
                fw.op("act", lambda: nc.scalar.activation(out=ebl[:, p, :], in_=pl[:], func=AF.Exp), reads=[t_pl], writes=[t_ebl])
            prb, t_prb = Rprb.next()
            fw.op("pe", lambda: nc.tensor.matmul(out=prb[:], lhsT=cf[:, C_TRIUBD:C_TRIUBD + 128], rhs=lf[:, i, :], start=True, stop=True),
                  reads=[t_in[g], K.t_const], writes=[t_prb])
            wk, t_wk = Rwk.next()
            fw.op("act", lambda: nc.scalar.activation(out=wk[:], in_=prb[:], func=AF.Exp), reads=[t_prb], writes=[t_wk])
            kp, t_kp = Rkp.next()
            fw.op("pool", lambda: nc.gpsimd.tensor_tensor(out=kp[:], in0=kht[:, i, :], in1=wk[:], op=ALU.mult), reads=[t_in[g], t_wk], writes=[t_kp])
            pa, t_pa = Rpa.next()
            for h in range(4):
                p, hp = h // 2, h % 2
                rr = slice(hp * 64, (hp + 1) * 64)
                fw.op("pe", lambda: nc.tensor.matmul(out=pa[:, h, :], lhsT=kt[rr, p, :], rhs=qt[rr, p, :], start=True, stop=True),
                      reads=[t_kt, t_qt], writes=[t_pa])
            AT, t_AT = RAT.next()
            fw.op("dve", lambda: nc.vector.tensor_tensor(out=AT[:], in0=pa[:], in1=cf[:, C_MASKBD:C_MASKBD + 128].unsqueeze(1).to_broadcast([128, 4, 128]), op=ALU.mult),
                  reads=[t_pa, K.t_const], writes=[t_AT])

            def state_update(j):
                rj = slice(j * 64, (j + 1) * 64)
                for p in range(2):
                    pss, t_pss = Rpss.next()
                    fw.op("pe", lambda: nc.tensor.matmul(out=pss[:], lhsT=kp[rj, p * 128:(p + 1) * 128], rhs=vh[rj, i, p * 128:(p + 1) * 128], start=True, stop=True),
                          reads=[t_kp, t_in[g]], writes=[t_pss])
                    fw.op("dve", lambda: nc.vector.scalar_tensor_tensor(out=Sst[p][:], in0=Sst[p][:], scalar=ebl[:, p, j:j + 1], in1=pss[:],
                                                                        op0=ALU.mult, op1=ALU.add),
                          reads=[t_S[p], t_ebl, t_pss], writes=[t_S[p]])
                    b_, t_b = RSb[p].next()
                    fw.op("act", lambda: nc.scalar.copy(out=b_[:], in_=Sst[p][:]), reads=[t_S[p]], writes=[t_b])
                    yield (b_, t_b)
            s0 = list(sb_)
            s1 = list(state_update(0))
            po, t_po = Rpo.next()
            for h in range(4):
                p, hp = h // 2, h % 2
                rr = slice(hp * 64, (hp + 1) * 64)
                cc = slice(hp * 64, (hp + 1) * 64)
                fw.op("pe", lambda: nc.tensor.matmul(out=po[:, h, :], lhsT=AT[:, h, :], rhs=vh[:, i, h * 64:(h + 1) * 64], start=True, stop=False),
                      reads=[t_AT, t_in[g]], writes=[t_po])
                fw.op("pe", lambda: nc.tensor.matmul(out=po[:, h, :], lhsT=q0[rr, p, :], rhs=s0[p][0][rr, cc], start=False, stop=False),
                      reads=[t_q0, s0[p][1]], writes=[t_po])
                fw.op("pe", lambda: nc.tensor.matmul(out=po[:, h, :], lhsT=q1[rr, p, :], rhs=s1[p][0][rr, cc], start=False, stop=True),
                      reads=[t_q1, s1[p][1]], writes=[t_po])
            if i + 1 < NT:
                sb_ = list(state_update(1))
            os_, t_os = Ros.next()
            fw.op("act", lambda: nc.scalar.copy(out=os_[:], in_=po[:]), reads=[t_po], writes=[t_os])
            sq, t_sq = Rsq.next()
            fw.op("pool", lambda: nc.gpsimd.tensor_tensor(out=sq[:], in0=os_[:], in1=os_[:], op=ALU.mult), reads=[t_os], writes=[t_sq])
            v4, t_v4 = Rs4.next()
            fw.op("dve", lambda: nc.vector.tensor_reduce(out=v4[:], in_=sq[:], axis=AX.X, op=ALU.add), reads=[t_sq], writes=[t_v4])
            fw.op("act", lambda: nc.scalar.activation(out=v4[:], in_=v4[:], func=AF.Ln, scale=1.0 / 64, bias=EPS), reads=[t_v4], writes=[t_v4])
            fw.op("act", lambda: nc.scalar.activation(out=v4[:], in_=v4[:], func=AF.Exp, scale=-0.5), reads=[t_v4], writes=[t_v4])
            fw.op("dve", lambda: nc.vector.tensor_tensor(out=os_[:], in0=os_[:], in1=bview(v4[:], [128, 4, 64]), op=ALU.mult), reads=[t_os, t_v4], writes=[t_os])
            o2 = os_[:].rearrange("p h e -> p (h e)")
            fw.op("pool", lambda: nc.gpsimd.tensor_tensor(out=o2, in0=o2, in1=hnw[:], op=ALU.mult), reads=[t_os, t_hnw], writes=[t_os])
            yb, t_yb = Ryb.next()
            fw.op("dve", lambda: nc.vector.tensor_tensor(out=yb[:], in0=o2, in1=gh[:], op=ALU.mult), reads=[t_os, t_gh], writes=[t_yb])
            fw.dma("sp", K.Y[cs, 768:1024], yb[:], reads=[t_yb], writes=[K.t_Y[g]])


def attn_phase(K, l):
    nc, fw = K.nc, K.fw
    cf = K.constf
    lam_init = 0.8 - 0.6 * math.exp(-0.3 * l)
    with Phase(K) as ph:
        lv, t_lv = load_row(K, ph, "lv", K.dlam[l], 256)
        dnw, t_dnw = load_row(K, ph, "dnw", K.d_nw[l], 512)
        rb, t_rb = load_row(K, ph, "rb", K.rel_bias, 128)
        j64 = ph.sb("j64", [128, 2, 64], F32)
        s12 = ph.sb("s12", [128, 2], F32)
        nlam = ph.sb("nlam", [128, 1], F32)
        t_lam = Trk()
        lv4 = lv[:].rearrange("p (a b) -> p a b", a=4)
        fw.op("dve", lambda: nc.vector.tensor_tensor(out=j64[:, 0, :], in0=lv4[:, 0, :], in1=lv4[:, 1, :], op=ALU.mult), reads=[t_lv], writes=[t_lam])
        fw.op("dve", lambda: nc.vector.tensor_tensor(out=j64[:, 1, :], in0=lv4[:, 2, :], in1=lv4[:, 3, :], op=ALU.mult), reads=[t_lv, t_lam], writes=[t_lam])
        fw.op("dve", lambda: nc.vector.tensor_reduce(out=s12[:], in_=j64[:], axis=AX.X, op=ALU.add), reads=[t_lam], writes=[t_lam])
        fw.op("act", lambda: nc.scalar.activation(out=s12[:], in_=s12[:], func=AF.Exp), reads=[t_lam], writes=[t_lam])
        fw.op("dve", lambda: nc.vector.tensor_tensor(out=nlam[:], in0=s12[:, 1:2], in1=s12[:, 0:1], op=ALU.subtract), reads=[t_lam], writes=[t_lam])
        fw.op("dve", lambda: nc.vector.tensor_scalar(out=nlam[:], in0=nlam[:], scalar1=-lam_init, scalar2=None, op0=ALU.add), reads=[t_lam], writes=[t_lam])
        fw.op("dve", lambda: nc.vector.tensor_scalar(out=dnw[:], in0=dnw[:], scalar1=1.0 - lam_init, scalar2=None, op0=ALU.mult), reads=[t_dnw], writes=[t_dnw])
        et = ph.sb("et", [128, 128], F32)
        t_et = Trk()
        fw.op("act", lambda: nc.scalar.activation(out=et[:], in_=rb[:], func=AF.Exp), reads=[t_rb], writes=[t_et])
        EE = ph.sb("EE", [128, 4, 768], F32)
        t_EE = Trk()
        etmp = ph.sb("etmp", [128, 768], F32)
        t_etmp = Trk()
        fw.op("dve", lambda: nc.vector.memset(EE[:], 0.0), writes=[t_EE])
        for b in range(32):
            w = 768 if b == 31 else 256
            for h in range(4):
                fw.op("dve", lambda: nc.vector.tensor_scalar(out=etmp[:, 0:w], in0=cf[:, C_EEI:C_EEI + w], scalar1=float(b), scalar2=et[:, b * 4 + h:b * 4 + h + 1],
                                                             op0=ALU.is_equal, op1=ALU.mult),
                      reads=[K.t_const, t_et, t_etmp], writes=[t_etmp])
                fw.op("dve", lambda: nc.vector.tensor_tensor(out=EE[:, h, 0:w], in0=EE[:, h, 0:w], in1=etmp[:, 0:w], op=ALU.add),
                      reads=[t_etmp, t_EE], writes=[t_EE])
        sets = []
        for r in range(2):
            QT = ph.sb("aQT", [128, S], BF16)
            KT = ph.sb("aKT", [128, S], BF16)
            V = ph.sb("aV", [128, NT, 130], BF16)
            tr = Trk()
            fw.op("pool", lambda: nc.gpsimd.memset(V[:], 1.0), writes=[tr])
            sets.append((QT, KT, V, tr))

        def load_head(h):
            QT, KT, V, tr = sets[h % 2]
            for g in range(NG):
                cs = slice(g * 512, (g + 1) * 512)
                fw.dma("sp", QT[:, cs], K.QD[h * 128:(h + 1) * 128, cs], reads=[K.t_QD[g]], writes=[tr])
                fw.dma("sp", KT[:, cs], K.KD[h * 128:(h + 1) * 128, cs], reads=[K.t_KD[g]], writes=[tr])
                fw.dma("sp", V[:, g * 4:(g + 1) * 4, 0:128], K.VD[cs, h * 128:(h + 1) * 128].rearrange("(c p) e -> p c e", p=128),
                       reads=[K.t_VD[g]], writes=[tr])
        Rs1 = Rot(ph, "as1", [128, 512], F32, 2, psum=True)
        Rs2 = Rot(ph, "as2", [128, 512], F32, 2, psum=True)
        PO = [(ph.ps("aPO", [128, 3, 129], F32), Trk(True)) for _ in range(3)]
        Rp1 = Rot(ph, "ap1", [128, 512], BF16, 3)
        Rp2 = Rot(ph, "ap2", [128, 512], BF16, 3)
        Re = Rot(ph, "ae", [128, 512], F32, 3)
        Rr = Rot(ph, "ar", [128, 2], F32, 4)
        Rt1 = Rot(ph, "at1", [128, 128], F32, 2)
        Rod = Rot(ph, "aod", [128, 128], F32, 2)
        Rsq = Rot(ph, "asq", [128, 128], F32, 2)
        Rss = Rot(ph, "ass", [128, 1], F32, 4)
        Ryb = Rot(ph, "ayb", [128, 128], BF16, 3)

        def acc(j, which):
            idx = which * 4 + j
            po, tp = PO[idx // 3]
            return po[:, idx % 3, :], tp

        load_head(0)
        for h in range(4):
            if h + 1 < 4:
                load_head(h + 1)
            QT, KT, V, t_hd = sets[h % 2]
            tiles = [(Q, kb) for Q in range(8) for kb in range(4 * Q + 4)]

            def S_step(Q, kb):
                j0 = kb - 4 * Q
                jlo = max(j0, 0)
                c0 = jlo * 128
                ks = slice(kb * 128, (kb + 1) * 128)
                qs = slice(Q * 512 + c0, (Q + 1) * 512)
                s1, t_s1 = Rs1.next()
                s2, t_s2 = Rs2.next()
                fw.op("pe", lambda: nc.tensor.matmul(out=s1[:, c0:512], lhsT=KT[0:64, ks], rhs=QT[0:64, qs], start=True, stop=True),
                      reads=[t_hd], writes=[t_s1])
                fw.op("pe", lambda: nc.tensor.matmul(out=s2[:, c0:512], lhsT=KT[64:128, ks], rhs=QT[64:128, qs], start=True, stop=True),
                      reads=[t_hd], writes=[t_s2])
                P1, t_P1 = Rp1.next()
                P2, t_P2 = Rp2.next()
                for (sx, t_sx, Px, t_Px, eng) in ((s1, t_s1, P1, t_P1, "dve"), (s2, t_s2, P2, t_P2, "pool")):
                    if j0 <= -2:
                        fw.op("act", lambda: nc.scalar.activation(out=Px[:], in_=sx[:], func=AF.Exp, scale=0.125, bias=rb[:, 124 + h:125 + h]),
                              reads=[t_sx, t_rb], writes=[t_Px])
                    else:
                        e, t_e = Re.next()
                        eoff = (jlo - j0) * 128
                        ncol = 512 - c0
                        fw.op("act", lambda: nc.scalar.activation(out=e[:, c0:512], in_=sx[:, c0:512], func=AF.Exp, scale=0.125), reads=[t_sx], writes=[t_e])
                        if eng == "dve":
                            fw.op("dve", lambda: nc.vector.tensor_tensor(out=Px[:, c0:512], in0=e[:, c0:512], in1=EE[:, h, eoff:eoff + ncol], op=ALU.mult),
                                  reads=[t_e, t_EE], writes=[t_Px])
                        else:
                            fw.op("pool", lambda: nc.gpsimd.tensor_tensor(out=Px[:, c0:512], in0=e[:, c0:512], in1=EE[:, h, eoff:eoff + ncol], op=ALU.mult),
                                  reads=[t_e, t_EE], writes=[t_Px])
                return (Q, kb, jlo, P1, t_P1, P2, t_P2)

            def A_step(info):
                Q, kb, jlo, P1, t_P1, P2, t_P2 = info
                if kb == 0:
                    for po, tp in PO:
                        fw.op("dve", lambda: nc.vector.memset(po[:], 0.0), writes=[tp])
                for j in range(jlo, 4):
                    for which, (Px, t_Px) in enumerate(((P1, t_P1), (P2, t_P2))):
                        o, t_o = acc(j, which)
                        fw.op("pe", lambda: nc.tensor.matmul(out=o, lhsT=Px[:, j * 128:(j + 1) * 128], rhs=V[:, kb, 0:129], start=False, stop=False,
                                                             skip_group_check=True),
                              reads=[t_Px, t_hd], writes=[t_o])

            def F_step(Q):
                for j in range(4):
                    i = 4 * Q + j
                    o1, t_o1 = acc(j, 0)
                    o2, t_o2 = acc(j, 1)
                    r, t_r = Rr.next()
                    fw.op("dve", lambda: nc.vector.reciprocal(out=r[:, 0:1], in_=o1[:, 128:129]), reads=[t_o1], writes=[t_r])
                    fw.op("dve", lambda: nc.vector.reciprocal(out=r[:, 1:2], in_=o2[:, 128:129]), reads=[t_o2, t_r], writes=[t_r])
                    fw.op("dve", lambda: nc.vector.tensor_tensor(out=r[:, 1:2], in0=r[:, 1:2], in1=nlam[:], op=ALU.mult), reads=[t_r, t_lam], writes=[t_r])
                    t1, t_t1 = Rt1.next()
                    fw.op("dve", lambda: nc.vector.tensor_scalar(out=t1[:], in0=o1[:, 0:128], scalar1=r[:, 0:1], scalar2=None, op0=ALU.mult),
                          reads=[t_o1, t_r], writes=[t_t1])
                    od, t_od = Rod.next()
                    fw.op("dve", lambda: nc.vector.scalar_tensor_tensor(out=od[:], in0=o2[:, 0:128], scalar=r[:, 1:2], in1=t1[:], op0=ALU.mult, op1=ALU.add),
                          reads=[t_o2, t_r, t_t1], writes=[t_od])
                    sq, t_sq = Rsq.next()
                    fw.op("pool", lambda: nc.gpsimd.tensor_tensor(out=sq[:], in0=od[:], in1=od[:], op=ALU.mult), reads=[t_od], writes=[t_sq])
                    ss, t_ss = Rss.next()
                    fw.op("dve", lambda: nc.vector.tensor_reduce(out=ss[:], in_=sq[:], axis=AX.X, op=ALU.add), reads=[t_sq], writes=[t_ss])
                    fw.op("act", lambda: nc.scalar.activation(out=ss[:], in_=ss[:], func=AF.Ln, scale=1.0 / 128, bias=EPS), reads=[t_ss], writes=[t_ss])
                    fw.op("act", lambda: nc.scalar.activation(out=ss[:], in_=ss[:], func=AF.Exp, scale=-0.5), reads=[t_ss], writes=[t_ss])
                    yb, t_yb = Ryb.next()
                    fw.op("dve", lambda: nc.vector.scalar_tensor_tensor(out=yb[:], in0=od[:], scalar=ss[:], in1=dnw[:, h * 128:(h + 1) * 128], op0=ALU.mult, op1=ALU.mult),
                          reads=[t_od, t_ss, t_dnw], writes=[t_yb])
                    fw.dma("sp", K.Y[i * 128:(i + 1) * 128, 256 + h * 128:256 + (h + 1) * 128], yb[:], reads=[t_yb], writes=[K.t_Y[i // 4]])

            prev = None
            for (Q, kb) in tiles:
                info = S_step(Q, kb)
                if prev is not None:
                    A_step(prev)
                    if prev[1] == 4 * prev[0] + 3:
                        F_step(prev[0])
                prev = info
            A_step(prev)
            F_step(prev[0])


KCtx.attn_phase = staticmethod(attn_phase)


def mixers(K, l, stop_after):
    MX = os.environ.get("MIXERS", "MHD")
    K.fw.pe_sync = True
    if "M" in MX:
        mlstm_phase(K, l)
    if "H" in MX:
        hgrn_phase(K, l)
    K.fw.pe_sync = False
    if "D" in MX and hasattr(K, "attn_phase"):
        K.attn_phase(K, l)


KCtx.mixers = staticmethod(mixers)


def build(n_layers=DEPTH, debug=False, stop_after=None):
    nc = bass.Bass("TRN2", target_bir_lowering=False)
    K = KCtx()
    K.nc = nc
    dt = nc.dram_tensor
    K.x = dt("x", [S, D], F32, kind="ExternalInput").ap()
    K.norm_w = dt("norm_w", [DEPTH, 6, D], F32, kind="ExternalInput").ap()
    K.ffn1_wi = dt("ffn1_wi", [DEPTH, D, 2 * DFF], F32, kind="ExternalInput").ap()
    K.ffn1_wo = dt("ffn1_wo", [DEPTH, DFF, D], F32, kind="ExternalInput").ap()
    K.ffn2_wi = dt("ffn2_wi", [DEPTH, D, 2 * DFF], F32, kind="ExternalInput").ap()
    K.ffn2_wo = dt("ffn2_wo", [DEPTH, DFF, D], F32, kind="ExternalInput").ap()
    K.w_in = dt("w_in", [DEPTH, D, NIN], F32, kind="ExternalInput").ap()
    K.w_out = dt("w_out", [DEPTH, D, D], F32, kind="ExternalInput").ap()
    K.consts = dt("consts", [128, NCONST], F32, kind="ExternalInput").ap()
    K.pp = dt("pp", [DEPTH, 128, 28], F32, kind="ExternalInput").ap()
    K.ig_b = dt("mlstm_igate_b", [DEPTH, 4], F32, kind="ExternalInput").ap()
    K.fg_b = dt("mlstm_fgate_b", [DEPTH, 4], F32, kind="ExternalInput").ap()
    K.m_nw = dt("mlstm_norm_w", [DEPTH, 256], F32, kind="ExternalInput").ap()
    K.dlam = dt("diff_lambda", [DEPTH, 256], F32, kind="ExternalInput").ap()
    K.d_nw = dt("diff_norm_w", [DEPTH, 512], F32, kind="ExternalInput").ap()
    K.rel_bias = dt("rel_bias", [128], F32, kind="ExternalInput").ap()
    K.lb_logits = dt("hgrn_lb_logits", [DEPTH, 256], F32, kind="ExternalInput").ap()
    K.h_nw = dt("hgrn_norm_w", [DEPTH, 256], F32, kind="ExternalInput").ap()
    K.out = dt("out", [S, D], F32, kind="ExternalOutput").ap()
    dbg = set(debug) if debug else set()

    def scratch(name, shape, dtp):
        kind = "ExternalOutput" if name in dbg else "Internal"
        ap = dt(name, shape, dtp, kind=kind).ap()
        return ap, [Trk() for _ in range(NG)]
    K.X, K.t_X = scratch("Xs", [S, D], F32)
    K.Y, K.t_Y = scratch("Ys", [S, D], BF16)
    K.QKM, K.t_QKM = scratch("QKM", [512, S], BF16)
    K.KMT, K.t_KMT = scratch("KMT", [S, 256], BF16)
    K.VM, K.t_VM = scratch("VM", [S, 256], BF16)
    K.OM, K.t_OM = scratch("OM", [S, 256], F32)
    K.GM, K.t_GM = scratch("GM", [S, 8], F32)
    K.QD, K.t_QD = scratch("QD", [512, S], BF16)
    K.KD, K.t_KD = scratch("KD", [512, S], BF16)
    K.VD, K.t_VD = scratch("VD", [S, 512], BF16)
    K.QH, K.t_QH = scratch("QH", [256, S], BF16)
    K.KH, K.t_KH = scratch("KH", [256, S], BF16)
    K.LFH, K.t_LFH = scratch("LFH", [S, 256], F32)
    K.KHT, K.t_KHT = scratch("KHT", [S, 256], F32)
    K.VH, K.t_VH = scratch("VH", [S, 256], BF16)
    K.GH, K.t_GH = scratch("GH", [S, 256], F32)
    K.t_x = [Trk() for _ in range(NG)]
    K.t_out = [Trk() for _ in range(NG)]
    K.WIB = dt("WIB", [2, 11, 128, 4096], BF16, kind="Internal").ap()
    K.t_WIB = [[Trk() for _ in range(11)] for _ in range(2)]
    K.wib_ready = {}
    K.n_layers = n_layers
    with ExitStack() as st:
        fw = FW(nc, st)
        K.fw = fw
        K.constf = st.enter_context(nc.sbuf_tensor("constf", [128, NCONST], F32))
        K.identb = st.enter_context(nc.sbuf_tensor("identb", [128, 128], BF16))
        K.t_const = Trk()
        fw.dma("sp", K.constf[:], K.consts, writes=[K.t_const])
        fw.op("dve", lambda: nc.vector.tensor_copy(out=K.identb[:], in_=K.constf[:, C_IDENT:C_IDENT + 128]),
              reads=[K.t_const], writes=[K.t_const])
        for l in range(n_layers):
            xin, t_xin = (K.x, K.t_x) if l == 0 else (K.X, K.t_X)
            if os.environ.get('SIM_PROJ_ONLY'):
                proj_phase(K, l, K.x, K.t_x)
                if stop_after[1] >= 3:
                    K.mixers(K, l, stop_after)
                break
            token_phase(K, l, 1, xin, t_xin, K.X, K.t_X)
            if stop_after == (l, 1):
                break
            proj_phase(K, l)
            if stop_after == (l, 2):
                break
            if hasattr(K, "mixers"):
                K.mixers(K, l, stop_after)
            if stop_after is not None and stop_after[0] == l and stop_after[1] == 3:
                break
            last = (l == n_layers - 1)
            token_phase(K, l, 2, K.X, K.t_X, K.out if last else K.X, K.t_out if last else K.t_X)
            if stop_after == (l, 4):
                break
        fw.barrier()
    K.counts = {k: v.count for k, v in fw.engs.items()}
    print("instr counts", K.counts, "dmas", fw.dma_count)
    return nc


def make_in_map(inputs, c, consts):
    f = lambda a: np.ascontiguousarray(a, dtype=np.float32)
    m = {"x": f(inputs["x"][c]), "consts": consts}
    for k in ["norm_w", "ffn1_wi", "ffn1_wo", "ffn2_wi", "ffn2_wo", "w_in", "w_out", "mlstm_igate_b", "mlstm_fgate_b",
              "mlstm_norm_w", "diff_norm_w", "hgrn_lb_logits", "hgrn_norm_w"]:
        m[k] = f(inputs[k])
    m["diff_lambda"] = f(inputs["diff_lambda"]).reshape(DEPTH, 256)
    m["rel_bias"] = f(inputs["rel_bias"]).reshape(128)
    pp = np.zeros((DEPTH, 128, 28), np.float32)
    cw = f(inputs["mlstm_conv_w"])
    cb = f(inputs["mlstm_conv_b"])
    lg = f(inputs["hgrn_lb_logits"])
    for l in range(DEPTH):
        pp[l, :, 0:16] = cw[l].reshape(4, 4, 128).transpose(2, 1, 0).reshape(128, 16)
        pp[l, :, 16:20] = cb[l].reshape(4, 128).T
        pp[l, :, 20:28] = lg.reshape(DEPTH, 2, 128).transpose(2, 1, 0).reshape(128, 8)
    m["pp"] = pp
    return m


_NC_CACHE = {}


def kernel(**inputs):
    nc = _NC_CACHE.get("nc")
    if nc is None:
        nc = build()
        _NC_CACHE["nc"] = nc
    consts = make_consts()
    in_maps = [make_in_map(inputs, c, consts) for c in range(8)]
    res = run_bass_kernel_spmd(nc, in_maps, core_ids=list(range(8)))
    return np.stack([res.results[c]["out"] for c in range(8)], axis=0)
```

```python
import math
import os
import numpy as np
from contextlib import ExitStack
import concourse.bass as bass
import concourse.mybir as mybir
from concourse.bass_utils import run_bass_kernel_spmd

F32 = mybir.dt.float32
BF16 = mybir.dt.bfloat16
AF = mybir.ActivationFunctionType
ALU = mybir.AluOpType
AX = mybir.AxisListType

S = 4096
D = 1024
DFF = 2816
NIN = 3592
NT = 32
NG = 8
DEPTH = 4
EPS = 1e-6
LN8 = math.log(0.125)


class Trk:
    __slots__ = ("w", "r", "psum")

    def __init__(self, psum=False):
        self.w = None
        self.r = []
        self.psum = psum


class Eng:
    def __init__(self, name, raw, sem):
        self.name = name
        self.raw = raw
        self.sem = sem
        self.count = 0
        self.waited = {}


class FW:
    NDMA = 8

    def __init__(self, nc, stack):
        self.nc = nc
        self.stack = stack
        self.engs = {}
        for name, raw in [("pe", nc.tensor), ("dve", nc.vector), ("act", nc.scalar),
                          ("pool", nc.gpsimd), ("sp", nc.sync)]:
            sem = stack.enter_context(nc.semaphore("sem_" + name))
            self.engs[name] = Eng(name, raw, sem)
        self.dma_sems = {}
        self.dma_count = {}
        for q in ["sp", "pool", "act"]:
            self.dma_sems[q] = [stack.enter_context(nc.semaphore("dsem_%s_%d" % (q, i))) for i in range(self.NDMA)]
            self.dma_count[q] = 0
        self.uid = 0
        self.pe_sync = False

    def name(self, base):
        self.uid += 1
        return "%s_%d" % (base, self.uid)

    def _deps(self, e, reads, writes):
        deps = {}

        def add(d):
            if d is None:
                return
            sem, val, en = d
            if en == "pe" and e.name == "pe" and not self.pe_sync:
                return
            k = id(sem)
            if e.waited.get(k, 0) >= val:
                return
            if k not in deps or deps[k][1] < val:
                deps[k] = (sem, val)
        for t in reads:
            add(t.w)
        for t in writes:
            add(t.w)
            for r in t.r:
                add(r)
        return list(deps.values())

    def _apply_waits(self, e, deps, instr_fn):
        for sem, val in deps[:-1]:
            e.raw.wait_ge(sem, val)
            e.waited[id(sem)] = val
        ins = instr_fn()
        if deps:
            sem, val = deps[-1]
            ins.wait_op(sem, val, "sem-ge")
            e.waited[id(sem)] = val
        return ins

    def op(self, eng, fn, reads=(), writes=(), inc=True):
        e = self.engs[eng]
        pr = [t for t in reads if t.psum]
        if pr:
            reads = [t for t in reads if not t.psum]
            writes = list(writes) + pr
        deps = self._deps(e, reads, writes)
        ins = self._apply_waits(e, deps, fn)
        if inc:
            e.count += 1
            ins.then_inc(e.sem, 1)
            tag = (e.sem, e.count, e.name)
        else:
            tag = (e.sem, e.count + 1, e.name)
        for t in reads:
            t.r.append(tag)
        for t in writes:
            t.w = tag
            t.r = []
        return ins

    def dma(self, q, out, in_, reads=(), writes=(), **kw):
        e = self.engs[q]
        i = self.dma_count[q]
        self.dma_count[q] = i + 1
        sem = self.dma_sems[q][i % self.NDMA]
        val = 16 * (i // self.NDMA + 1)
        deps = self._deps(e, reads, writes)
        if i >= self.NDMA:
            pv = val - 16
            if e.waited.get(id(sem), 0) < pv:
                mx = max([pv] + [d[1] for d in deps if d[0] is sem])
                deps = [d for d in deps if d[0] is not sem] + [(sem, mx)]
        ins = self._apply_waits(e, deps, lambda: e.raw.dma_start(out=out, in_=in_, **kw))
        ins.then_inc(sem, 16)
        tag = (sem, val, "dma_" + q)
        for t in reads:
            t.r.append(tag)
        for t in writes:
            t.w = tag
            t.r = []
        return ins

    def barrier(self):
        targets = [(e.sem, e.count) for e in self.engs.values() if e.count > 0]
        for q in self.dma_sems:
            n = self.dma_count[q]
            for j, sem in enumerate(self.dma_sems[q]):
                cnt = (n - j + self.NDMA - 1) // self.NDMA if n > j else 0
                if cnt > 0:
                    targets.append((sem, 16 * cnt))
        for e in self.engs.values():
            for sem, val in targets:
                if sem is e.sem and e.name == "pe":
                    continue
                if e.waited.get(id(sem), 0) < val:
                    e.raw.wait_ge(sem, val)
                    e.waited[id(sem)] = val


class Phase:
    def __init__(self, K):
        self.K = K
        self.st = ExitStack()

    def __enter__(self):
        self.st.__enter__()
        return self

    def __exit__(self, *a):
        self.K.fw.barrier()
        return self.st.__exit__(*a)

    def sb(self, name, shape, dt):
        return self.st.enter_context(self.K.nc.sbuf_tensor(self.K.fw.name(name), list(shape), dt))

    def ps(self, name, shape, dt):
        full = 512 if dt == F32 else 1024
        t = self.st.enter_context(self.K.nc.psum_tensor(self.K.fw.name(name), [128, full], dt))
        n = 1
        for d in shape[1:]:
            n *= d
        assert shape[0] == 128 and n <= full
        v = t[:, 0:n]
        if len(shape) == 3:
            v = v.rearrange("p (a b) -> p a b", a=shape[1])
        return v


class Rot:
    def __init__(self, ph, name, shape, dt, n, psum=False):
        mk = ph.ps if psum else ph.sb
        self.bufs = [(mk(name, shape, dt), Trk(psum)) for _ in range(n)]
        self.i = 0

    def next(self):
        b = self.bufs[self.i % len(self.bufs)]
        self.i += 1
        return b


def t5_bucket_np(rel):
    n = np.maximum(rel, 0)
    nf = np.maximum(n, 1).astype(np.float32)
    large = 16 + (np.log(nf / np.float32(16)) / np.float32(math.log(128 / 16)) * np.float32(16)).astype(np.int32)
    large = np.minimum(large, 31)
    return np.where(n < 16, n, large)


C_IDENT, C_TRIT, C_TRIU, C_ONES, C_MASK, C_TRITBD, C_TRIMID, C_TRIUBD, C_MASKBD = [i * 128 for i in range(9)]
C_CHI = 9 * 128
C_EEI = C_CHI + 2
NCONST = C_EEI + 768


def make_consts():
    c = np.zeros((128, NCONST), np.float32)
    r = np.arange(128)[:, None]
    t = np.arange(128)[None, :]
    same = (r // 64) == (t // 64)
    c[:, C_IDENT:C_IDENT + 128] = (r == t)
    c[:, C_TRIT:C_TRIT + 128] = (r <= t)
    c[:, C_TRIU:C_TRIU + 128] = (r > t)
    c[:, C_ONES:C_ONES + 128] = 1.0
    c[:, C_MASK:C_MASK + 128] = (r <= t)
    c[:, C_TRITBD:C_TRITBD + 128] = (r <= t) & same
    mid = (t // 64) * 64 + 31
    c[:, C_TRIMID:C_TRIMID + 128] = (((r <= t).astype(np.float32) - (r <= mid).astype(np.float32)) * same)
    c[:, C_TRIUBD:C_TRIUBD + 128] = (r > t) & same
    c[:, C_MASKBD:C_MASKBD + 128] = (r <= t) & same
    c[:, C_CHI] = (np.arange(128) < 64)
    c[:, C_CHI + 1] = (np.arange(128) >= 64)
    k = np.arange(128)[:, None]
    col = np.arange(128)[None, :]
    rel0 = col - k
    idx0 = np.where(rel0 >= 0, t5_bucket_np(rel0), -1)
    idx1 = t5_bucket_np(128 + col - k)
    c[:, C_EEI:C_EEI + 128] = idx0
    c[:, C_EEI + 128:C_EEI + 256] = idx1
    c[:, C_EEI + 256:C_EEI + 768] = 31
    return c


class KCtx:
    pass


def rms_rstd(K, ph, src_ap, src_trk, n, junk, rstd, eps=EPS):
    nc, fw = K.nc, K.fw
    jb, jt = junk
    rb, rt = rstd
    fw.op("act", lambda: nc.scalar.activation(out=jb, in_=src_ap, func=AF.Square, accum_out=rb),
          reads=[src_trk], writes=[jt, rt])
    fw.op("act", lambda: nc.scalar.activation(out=rb, in_=rb, func=AF.Sqrt, scale=1.0 / n, bias=eps),
          reads=[rt], writes=[rt])
    fw.op("dve", lambda: nc.vector.reciprocal(out=rb, in_=rb), reads=[rt], writes=[rt])


def load_row(K, ph, name, src_1d, n, q="sp"):
    t = ph.sb(name, [128, n], F32)
    tr = Trk()
    K.fw.dma(q, t[:], src_1d.partition_broadcast(128), writes=[tr])
    return t, tr


def norm_transpose_group(K, ph, g, xg, t_xg, nwrow, t_nw, xnT, t_xnT, R):
    nc, fw = K.nc, K.fw
    for i in range(4):
        junk = R["junk"].next()
        rstd = R["rstd"].next()
        rms_rstd(K, ph, xg[:, i, :], t_xg[i], D, (junk[0][:], junk[1]), (rstd[0][:], rstd[1]))
        xn, t_xn = R["xn"].next()
        fw.op("dve", lambda: nc.vector.scalar_tensor_tensor(out=xn[:], in0=xg[:, i, :], scalar=rstd[0][:], in1=nwrow[:],
                                                            op0=ALU.mult, op1=ALU.mult),
              reads=[t_xg[i], rstd[1], t_nw], writes=[t_xn])
        pT, t_pT = R["pT"].next()
        for k in range(8):
            fw.op("pe", lambda: nc.tensor.transpose(out=pT[:, k, :], in_=xn[:, k * 128:(k + 1) * 128], identity=K.identb[:]),
                  reads=[t_xn, K.t_const], writes=[t_pT], inc=(k == 7))
        fw.op("act", lambda: nc.scalar.copy(out=xnT[:, :, i * 128:(i + 1) * 128], in_=pT[:]), reads=[t_pT], writes=[t_xnT])


def convert_wi(K, l, which):
    fw = K.fw
    slot = (which - 1) % 2
    wi = (K.ffn1_wi if which == 1 else K.ffn2_wi)[l]
    wi_v = wi.rearrange("(k p) n -> p k n", p=128)
    for j in range(11):
        dst = K.WIB[slot, j].rearrange("p (k a n) -> p k a n", k=8, a=2)
        fw.dma("pool", dst[:, :, 0, :], wi_v[:, :, j * 256:(j + 1) * 256], writes=[K.t_WIB[slot][j]])
        fw.dma("pool", dst[:, :, 1, :], wi_v[:, :, DFF + j * 256:DFF + (j + 1) * 256], writes=[K.t_WIB[slot][j]])
    K.wib_ready[(l, which)] = slot


def token_phase(K, l, which, xin, t_xin, xout, t_xout):
    nc, fw = K.nc, K.fw
    wi = (K.ffn1_wi if which == 1 else K.ffn2_wi)[l]
    wo = (K.ffn1_wo if which == 1 else K.ffn2_wo)[l]
    npre, npost = (0, 1) if which == 1 else (4, 5)
    with Phase(K) as ph:
        nw_pre, t_nwpre = load_row(K, ph, "nwpre", K.norm_w[l, npre], D)
        nw_post, t_nwpost = load_row(K, ph, "nwpost", K.norm_w[l, npost], D)
        wo_sb = ph.sb("wo", [128, 22, D], BF16)
        t_wo = Trk()
        wo_v = wo.rearrange("(k p) n -> p k n", p=128)
        for kk in range(0, 22, 2):
            fw.dma("pool", wo_sb[:, kk:kk + 2, :], wo_v[:, kk:kk + 2, :], writes=[t_wo])
        if which == 2:
            nw3, t_nw3 = load_row(K, ph, "nw3", K.norm_w[l, 3], D)
            wout_sb = ph.sb("wout", [128, 8, D], BF16)
            t_wout = Trk()
            wout_v = K.w_out[l].rearrange("(k p) n -> p k n", p=128)
            for kk in range(0, 8, 2):
                fw.dma("pool", wout_sb[:, kk:kk + 2, :], wout_v[:, kk:kk + 2, :], writes=[t_wout])
            Ry = Rot(ph, "ytile", [128, D], BF16, 2)
            RyT = Rot(ph, "yT", [128, 8, 128], BF16, 2)
        R = {"junk": Rot(ph, "junk", [128, D], F32, 1), "rstd": Rot(ph, "rstd", [128, 1], F32, 4),
             "xn": Rot(ph, "xn", [128, D], BF16, 2), "pT": Rot(ph, "pT", [128, 8, 128], BF16, 2, psum=True)}
        nxg = 2 if which == 1 else 1
        xg_bufs = [(ph.sb("xg", [128, 4, D], F32), [Trk() for _ in range(4)]) for _ in range(nxg)]
        xnT = ph.sb("xnT", [128, 8, 512], BF16)
        t_xnT = Trk()
        aT = ph.sb("aT", [128, 22, 512], BF16)
        t_aT = Trk()
        Rw = Rot(ph, "wipiece", [128, 8, 2, 256], BF16, 3)
        Rpg = Rot(ph, "pg", [128, 512], F32, 2, psum=True)
        Rpu = Rot(ph, "pu", [128, 512], F32, 2, psum=True)
        Rpo = Rot(ph, "po", [128, 512], F32, 2, psum=True)
        Rsg = Rot(ph, "sg", [128, 512], F32, 2)
        Rh = Rot(ph, "h", [128, D], F32, 2)
        wi_v = wi.rearrange("(k p) n -> p k n", p=128)

        wslot = K.wib_ready.get((l, which))

        def load_piece(n):
            j = n % 11
            wb, t_wb = Rw.next()
            if wslot is not None:
                fw.dma("sp", wb[:].rearrange("p k a n -> p (k a n)"), K.WIB[wslot, j], reads=[K.t_WIB[wslot][j]], writes=[t_wb])
            else:
                fw.dma("pool", wb[:, :, 0, :], wi_v[:, :, j * 256:(j + 1) * 256], writes=[t_wb])
                fw.dma("pool", wb[:, :, 1, :], wi_v[:, :, DFF + j * 256:DFF + (j + 1) * 256], writes=[t_wb])
            return wb, t_wb

        pieces = {}
        NP = NG * 11
        pieces[0] = load_piece(0)
        pieces[1] = load_piece(1)

        def load_x(g):
            xg, t_tiles = xg_bufs[g % nxg]
            for i in range(4):
                r0 = g * 512 + i * 128
                fw.dma("sp", xg[:, i, :], xin[r0:r0 + 128, :], reads=[t_xin[g]], writes=[t_tiles[i]])
            return xg, t_tiles

        cur = load_x(0)
        for g in range(NG):
            xg, t_xg = cur
            if which == 2:
                for i in range(4):
                    r0 = g * 512 + i * 128
                    yt, t_yt = Ry.next()
                    fw.dma("sp", yt[:], K.Y[r0:r0 + 128, :], reads=[K.t_Y[g]], writes=[t_yt])
                    pT, t_pT = R["pT"].next()
                    for k in range(8):
                        fw.op("pe", lambda: nc.tensor.transpose(out=pT[:, k, :], in_=yt[:, k * 128:(k + 1) * 128], identity=K.identb[:]),
                              reads=[t_yt, K.t_const], writes=[t_pT], inc=(k == 7))
                    yT, t_yT = RyT.next()
                    fw.op("act", lambda: nc.scalar.copy(out=yT[:], in_=pT[:]), reads=[t_pT], writes=[t_yT])
                    h, t_h = Rh.next()
                    for hf in range(2):
                        po, t_po = Rpo.next()
                        for k in range(8):
                            fw.op("pe", lambda: nc.tensor.matmul(out=po[:], lhsT=yT[:, k, :], rhs=wout_sb[:, k, hf * 512:(hf + 1) * 512],
                                                                 start=(k == 0), stop=(k == 7)),
                                  reads=[t_yT, t_wout], writes=[t_po], inc=(k == 7))
                        fw.op("act", lambda: nc.scalar.copy(out=h[:, hf * 512:(hf + 1) * 512], in_=po[:]), reads=[t_po], writes=[t_h])
                    junk = R["junk"].next()
                    rstd = R["rstd"].next()
                    rms_rstd(K, ph, h[:], t_h, D, (junk[0][:], junk[1]), (rstd[0][:], rstd[1]))
                    fw.op("dve", lambda: nc.vector.scalar_tensor_tensor(out=h[:], in0=h[:], scalar=rstd[0][:], in1=nw3[:],
                                                                        op0=ALU.mult, op1=ALU.mult),
                          reads=[t_h, rstd[1], t_nw3], writes=[t_h])
                    fw.op("dve", lambda: nc.vector.tensor_tensor(out=xg[:, i, :], in0=h[:], in1=xg[:, i, :], op=ALU.add),
                          reads=[t_h, t_xg[i]], writes=[t_xg[i]])
            norm_transpose_group(K, ph, g, xg, t_xg, nw_pre, t_nwpre, xnT, t_xnT, R)
            if g + 1 < NG and nxg == 2:
                cur = load_x(g + 1)
            for j in range(11):
                n = g * 11 + j
                if n + 2 < NP:
                    pieces[n + 2] = load_piece(n + 2)
                wb, t_wb = pieces.pop(n)
                for c in range(2):
                    pg, t_pg = Rpg.next()
                    pu, t_pu = Rpu.next()
                    for k in range(8):
                        fw.op("pe", lambda: nc.tensor.matmul(out=pg[:], lhsT=wb[:, k, 0, c * 128:(c + 1) * 128], rhs=xnT[:, k, :],
                                                             start=(k == 0), stop=(k == 7)),
                              reads=[t_wb, t_xnT], writes=[t_pg], inc=(k == 7))
                    for k in range(8):
                        fw.op("pe", lambda: nc.tensor.matmul(out=pu[:], lhsT=wb[:, k, 1, c * 128:(c + 1) * 128], rhs=xnT[:, k, :],
                                                             start=(k == 0), stop=(k == 7)),
                              reads=[t_wb, t_xnT], writes=[t_pu], inc=(k == 7))
                    sg, t_sg = Rsg.next()
                    fw.op("act", lambda: nc.scalar.activation(out=sg[:], in_=pg[:], func=AF.Silu), reads=[t_pg], writes=[t_sg])
                    fw.op("dve", lambda: nc.vector.tensor_tensor(out=aT[:, j * 2 + c, :], in0=sg[:], in1=pu[:], op=ALU.mult),
                          reads=[t_sg, t_pu], writes=[t_aT])
            for i in range(4):
                r0 = g * 512 + i * 128
                h, t_h = Rh.next()
                for hf in range(2):
                    po, t_po = Rpo.next()
                    for k in range(22):
                        fw.op("pe", lambda: nc.tensor.matmul(out=po[:], lhsT=aT[:, k, i * 128:(i + 1) * 128], rhs=wo_sb[:, k, hf * 512:(hf + 1) * 512],
                                                             start=(k == 0), stop=(k == 21)),
                              reads=[t_aT, t_wo], writes=[t_po], inc=(k == 21))
                    fw.op("act", lambda: nc.scalar.copy(out=h[:, hf * 512:(hf + 1) * 512], in_=po[:]), reads=[t_po], writes=[t_h])
                junk = R["junk"].next()
                rstd = R["rstd"].next()
                rms_rstd(K, ph, h[:], t_h, D, (junk[0][:], junk[1]), (rstd[0][:], rstd[1]))
                fw.op("dve", lambda: nc.vector.scalar_tensor_tensor(out=h[:], in0=h[:], scalar=rstd[0][:], in1=nw_post[:],
                                                                    op0=ALU.mult, op1=ALU.mult),
                      reads=[t_h, rstd[1], t_nwpost], writes=[t_h])
                fw.op("dve", lambda: nc.vector.scalar_tensor_tensor(out=h[:], in0=h[:], scalar=0.5, in1=xg[:, i, :],
                                                                    op0=ALU.mult, op1=ALU.add),
                      reads=[t_h, t_xg[i]], writes=[t_h])
                fw.dma("sp", xout[r0:r0 + 128, :], h[:], reads=[t_h], writes=[t_xout[g]])
            if g + 1 < NG and nxg == 1:
                cur = load_x(g + 1)


def proj_phase(K, l, xsrc=None, t_xsrc=None):
    nc, fw = K.nc, K.fw
    if xsrc is None:
        xsrc, t_xsrc = K.X, K.t_X
    with Phase(K) as ph:
        nw2, t_nw2 = load_row(K, ph, "nw2", K.norm_w[l, 2], D)
        win_sb = ph.sb("win", [128, 8, NIN], BF16)
        t_win = Trk()
        win_v = K.w_in[l].rearrange("(k p) n -> p k n", p=128)
        for k in range(8):
            fw.dma("pool", win_sb[:, k, :], win_v[:, k, :], writes=[t_win])
        if not os.environ.get("NO_WIB"):
            convert_wi(K, l, 2)
            if l + 1 < K.n_layers:
                convert_wi(K, l + 1, 1)
        pp = ph.sb("pp", [128, 28], F32)
        t_pp = Trk()
        fw.dma("sp", pp[:], K.pp[l], writes=[t_pp])
        gb = ph.sb("gb", [128, 8], F32)
        t_gb = Trk()
        fw.dma("sp", gb[:, 0:4], K.ig_b[l].partition_broadcast(128), writes=[t_gb])
        fw.dma("sp", gb[:, 4:8], K.fg_b[l].partition_broadcast(128), writes=[t_gb])
        fw.op("dve", lambda: nc.vector.tensor_scalar(out=gb[:, 0:4], in0=gb[:, 0:4], scalar1=LN8, scalar2=None, op0=ALU.add),
              reads=[t_gb], writes=[t_gb])
        lgr = ph.sb("lgr", [128, 4, 256], F32)
        t_lgr = Trk()
        fw.dma("sp", lgr[:].rearrange("p a b -> p (a b)"), K.lb_logits.rearrange("a b -> (a b)").partition_broadcast(128), writes=[t_lgr])
        fw.op("act", lambda: nc.scalar.activation(out=lgr[:], in_=lgr[:], func=AF.Exp), reads=[t_lgr], writes=[t_lgr])
        lb_row = ph.sb("lb_row", [128, 256], F32)
        oml_row = ph.sb("oml_row", [128, 256], F32)
        tmp_row = ph.sb("tmp_row", [128, 256], F32)
        t_lb = Trk()
        fw.op("dve", lambda: nc.vector.tensor_tensor(out=tmp_row[:], in0=lgr[:, 0, :], in1=lgr[:, 1, :], op=ALU.add), reads=[t_lgr], writes=[t_lb])
        fw.op("dve", lambda: nc.vector.tensor_tensor(out=tmp_row[:], in0=tmp_row[:], in1=lgr[:, 2, :], op=ALU.add), reads=[t_lgr, t_lb], writes=[t_lb])
        fw.op("dve", lambda: nc.vector.tensor_tensor(out=tmp_row[:], in0=tmp_row[:], in1=lgr[:, 3, :], op=ALU.add), reads=[t_lgr, t_lb], writes=[t_lb])
        fw.op("dve", lambda: nc.vector.reciprocal(out=tmp_row[:], in_=tmp_row[:]), reads=[t_lb], writes=[t_lb])
        fw.op("dve", lambda: nc.vector.memset(lb_row[:], 0.0), writes=[t_lb])
        for j in range(1, l + 1):
            fw.op("dve", lambda: nc.vector.tensor_tensor(out=lb_row[:], in0=lb_row[:], in1=lgr[:, j, :], op=ALU.add), reads=[t_lgr, t_lb], writes=[t_lb])
        fw.op("dve", lambda: nc.vector.tensor_tensor(out=lb_row[:], in0=lb_row[:], in1=tmp_row[:], op=ALU.mult), reads=[t_lb], writes=[t_lb])
        fw.op("dve", lambda: nc.vector.tensor_scalar(out=oml_row[:], in0=lb_row[:], scalar1=-1.0, scalar2=1.0, op0=ALU.mult, op1=ALU.add),
              reads=[t_lb], writes=[t_lb])
        lgf = ph.sb("lgf", [128, 2, 4], F32)
        oml_fm = ph.sb("oml_fm", [128, 2], F32)
        tmpf = ph.sb("tmpf", [128, 2], F32)
        t_lf = Trk()
        fw.op("act", lambda: nc.scalar.activation(out=lgf[:], in_=pp[:, 20:28].rearrange("p (a b) -> p a b", a=2), func=AF.Exp),
              reads=[t_pp], writes=[t_lf])
        fw.op("dve", lambda: nc.vector.tensor_reduce(out=tmpf[:], in_=lgf[:], axis=AX.X, op=ALU.add), reads=[t_lf], writes=[t_lf])
        fw.op("dve", lambda: nc.vector.reciprocal(out=tmpf[:], in_=tmpf[:]), reads=[t_lf], writes=[t_lf])
        fw.op("dve", lambda: nc.vector.memset(oml_fm[:], 0.0), writes=[t_lf])
        for j in range(1, l + 1):
            fw.op("dve", lambda: nc.vector.tensor_tensor(out=oml_fm[:], in0=oml_fm[:], in1=lgf[:, :, j], op=ALU.add), reads=[t_lf], writes=[t_lf])
        fw.op("dve", lambda: nc.vector.tensor_tensor(out=oml_fm[:], in0=oml_fm[:], in1=tmpf[:], op=ALU.mult), reads=[t_lf], writes=[t_lf])
        fw.op("dve", lambda: nc.vector.tensor_scalar(out=oml_fm[:], in0=oml_fm[:], scalar1=-1.0, scalar2=1.0, op0=ALU.mult, op1=ALU.add),
              reads=[t_lf], writes=[t_lf])

        R = {"junk": Rot(ph, "junk", [128, D], F32, 1), "rstd": Rot(ph, "rstd", [128, 1], F32, 4),
             "xn": Rot(ph, "xn", [128, D], BF16, 2), "pT": Rot(ph, "pT", [128, 8, 128], BF16, 2, psum=True)}
        xg_bufs = [(ph.sb("xg", [128, 4, D], F32), [Trk() for _ in range(4)]) for _ in range(2)]
        xnT = ph.sb("xnT", [128, 8, 512], BF16)
        t_xnT = Trk()
        xc = ph.sb("xc", [128, 4, 515], F32)
        t_xc = [Trk() for _ in range(4)]
        fw.op("dve", lambda: nc.vector.memset(xc[:], 0.0), writes=t_xc)
        Rpf = Rot(ph, "pf", [128, 512], F32, 2, psum=True)
        Rpt = Rot(ph, "pt", [128, 512], F32, 2, psum=True)
        Rpk = Rot(ph, "pk", [128, 4, 128], BF16, 1, psum=True)
        Racc = Rot(ph, "acc", [128, 512], F32, 2)
        Rob = Rot(ph, "ob", [128, 512], BF16, 3)
        Rkt = Rot(ph, "kt", [128, 4, 128], BF16, 2)
        Rtf = Rot(ph, "tf", [128, 512], F32, 2)
        RtfD = Rot(ph, "tfD", [128, 512], F32, 4)
        Rtb = Rot(ph, "tb", [128, 512], BF16, 3)
        Rg8 = Rot(ph, "g8", [128, 8], F32, 2)
        Rg4 = Rot(ph, "g4", [128, 4], F32, 2)

        def load_x(g):
            xg, t_tiles = xg_bufs[g % 2]
            for i in range(4):
                r0 = g * 512 + i * 128
                fw.dma("sp", xg[:, i, :], xsrc[r0:r0 + 128, :], reads=[t_xsrc[g]], writes=[t_tiles[i]])
            return xg, t_tiles

        cur = load_x(0)
        fchunks = [(ci * 128, "qkm", ci) for ci in range(4)] + [(2568 + 128 * p, "qh", p) for p in range(2)] + \
                  [(1032 + 128 * h, "qd", h) for h in range(4)] + [(1544 + 128 * h, "kd", h) for h in range(4)] + \
                  [(2824 + 128 * p, "fh", p) for p in range(2)]
        for g in range(NG):
            xg, t_xg = cur
            norm_transpose_group(K, ph, g, xg, t_xg, nw2, t_nw2, xnT, t_xnT, R)
            if g + 1 < NG:
                cur = load_x(g + 1)
            cs = slice(g * 512, (g + 1) * 512)
            for (c0, kind, ci) in fchunks:
                pf, t_pf = Rpf.next()
                for k in range(8):
                    fw.op("pe", lambda: nc.tensor.matmul(out=pf[:], lhsT=win_sb[:, k, c0:c0 + 128], rhs=xnT[:, k, :],
                                                         start=(k == 0), stop=(k == 7)),
                          reads=[t_win, t_xnT], writes=[t_pf], inc=(k == 7))
                if kind == "qkm":
                    fw.op("act", lambda: nc.scalar.copy(out=xc[:, ci, 3:515], in_=pf[:]), reads=[t_pf], writes=[t_xc[ci]])
                    acc, t_acc = Racc.next()
                    fw.op("dve", lambda: nc.vector.tensor_scalar(out=acc[:], in0=xc[:, ci, 3:515], scalar1=pp[:, ci * 4 + 3:ci * 4 + 4],
                                                                 scalar2=pp[:, 16 + ci:17 + ci], op0=ALU.mult, op1=ALU.add),
                          reads=[t_xc[ci], t_pp], writes=[t_acc])
                    for j in range(3):
                        fw.op("dve", lambda: nc.vector.scalar_tensor_tensor(out=acc[:], in0=xc[:, ci, j:j + 512], scalar=pp[:, ci * 4 + j:ci * 4 + j + 1],
                                                                            in1=acc[:], op0=ALU.mult, op1=ALU.add),
                              reads=[t_xc[ci], t_pp, t_acc], writes=[t_acc])
                    fw.op("dve", lambda: nc.vector.tensor_copy(out=xc[:, ci, 0:3], in_=xc[:, ci, 512:515]), reads=[t_xc[ci]], writes=[t_xc[ci]])
                    ob, t_ob = Rob.next()
                    fw.op("act", lambda: nc.scalar.activation(out=ob[:], in_=acc[:], func=AF.Silu), reads=[t_acc], writes=[t_ob])
                    fw.dma("sp", K.QKM[ci * 128:(ci + 1) * 128, cs], ob[:], reads=[t_ob], writes=[K.t_QKM[g]])
                    if ci >= 2:
                        pk, t_pk = Rpk.next()
                        for i in range(4):
                            fw.op("pe", lambda: nc.tensor.transpose(out=pk[:, i, :], in_=ob[:, i * 128:(i + 1) * 128], identity=K.identb[:]),
                                  reads=[t_ob, K.t_const], writes=[t_pk])
                        kt, t_kt = Rkt.next()
                        fw.op("dve", lambda: nc.vector.tensor_copy(out=kt[:], in_=pk[:]), reads=[t_pk], writes=[t_kt])
                        fw.dma("sp", K.KMT[cs, (ci - 2) * 128:(ci - 1) * 128].rearrange("(i p) c -> p i c", p=128), kt[:],
                               reads=[t_kt], writes=[K.t_KMT[g]])
                elif kind in ("qd", "kd"):
                    ob, t_ob = Rob.next()
                    if ci % 2 == 0:
                        fw.op("act", lambda: nc.scalar.copy(out=ob[:], in_=pf[:]), reads=[t_pf], writes=[t_ob])
                    else:
                        fw.op("dve", lambda: nc.vector.tensor_copy(out=ob[:], in_=pf[:]), reads=[t_pf], writes=[t_ob])
                    dst, tr = (K.QD, K.t_QD) if kind == "qd" else (K.KD, K.t_KD)
                    fw.dma("sp", dst[ci * 128:(ci + 1) * 128, cs], ob[:], reads=[t_ob], writes=[tr[g]])
                elif kind == "qh":
                    ob, t_ob = Rob.next()
                    fw.op("act", lambda: nc.scalar.activation(out=ob[:], in_=pf[:], func=AF.Silu), reads=[t_pf], writes=[t_ob])
                    fw.dma("sp", K.QH[ci * 128:(ci + 1) * 128, cs], ob[:], reads=[t_ob], writes=[K.t_QH[g]])
                else:
                    acc, t_acc = Racc.next()
                    fw.op("act", lambda: nc.scalar.activation(out=acc[:], in_=pf[:], func=AF.Sigmoid, scale=-1.0), reads=[t_pf], writes=[t_acc])
                    ob, t_ob = Rob.next()
                    fw.op("dve", lambda: nc.vector.tensor_scalar(out=ob[:], in0=acc[:], scalar1=oml_fm[:, ci:ci + 1], scalar2=None, op0=ALU.mult),
                          reads=[t_acc, t_lf], writes=[t_ob])
                    fw.dma("sp", K.KH[ci * 128:(ci + 1) * 128, cs], ob[:], reads=[t_ob], writes=[K.t_KH[g]])
            def tok_mm(i, c0, n):
                pt, t_pt = Rpt.next()
                for k in range(8):
                    fw.op("pe", lambda: nc.tensor.matmul(out=pt[:, 0:n], lhsT=xnT[:, k, i * 128:(i + 1) * 128], rhs=win_sb[:, k, c0:c0 + n],
                                                         start=(k == 0), stop=(k == 7)),
                          reads=[t_win, t_xnT], writes=[t_pt], inc=(k == 7))
                return pt, t_pt
            tfD = []
            for i in range(4):
                rs = slice(g * 512 + i * 128, g * 512 + (i + 1) * 128)
                pt, t_pt = tok_mm(i, 512, 512)
                tb, t_tb = Rtb.next()
                fw.op("dve", lambda: nc.vector.tensor_copy(out=tb[:, 0:256], in_=pt[:, 0:256]), reads=[t_pt], writes=[t_tb])
                fw.dma("sp", K.VM[rs, :], tb[:, 0:256], reads=[t_tb], writes=[K.t_VM[g]])
                tf, t_tf = Rtf.next()
                fw.op("act", lambda: nc.scalar.activation(out=tf[:, 0:256], in_=pt[:, 256:512], func=AF.Sigmoid), reads=[t_pt], writes=[t_tf])
                fw.dma("sp", K.OM[rs, :], tf[:, 0:256], reads=[t_tf], writes=[K.t_OM[g]])
                pt, t_pt = tok_mm(i, 2824, 512)
                tf, t_tf = RtfD.next()
                fw.op("act", lambda: nc.scalar.activation(out=tf[:, 0:256], in_=pt[:, 0:256], func=AF.Sigmoid), reads=[t_pt], writes=[t_tf])
                fw.op("dve", lambda: nc.vector.tensor_tensor(out=tf[:, 0:256], in0=tf[:, 0:256], in1=oml_row[:], op=ALU.mult), reads=[t_tf, t_lb], writes=[t_tf])
                fw.op("dve", lambda: nc.vector.tensor_tensor(out=tf[:, 0:256], in0=tf[:, 0:256], in1=lb_row[:], op=ALU.add), reads=[t_tf, t_lb], writes=[t_tf])
                fw.op("dve", lambda: nc.vector.tensor_scalar(out=tf[:, 256:512], in0=tf[:, 0:256], scalar1=-1.0, scalar2=1.0, op0=ALU.mult, op1=ALU.add),
                      reads=[t_tf], writes=[t_tf])
                fw.dma("sp", K.KHT[rs, :], tf[:, 256:512], reads=[t_tf], writes=[K.t_KHT[g]])
                tb, t_tb = Rtb.next()
                fw.op("dve", lambda: nc.vector.tensor_copy(out=tb[:, 0:256], in_=pt[:, 256:512]), reads=[t_pt], writes=[t_tb])
                fw.dma("sp", K.VH[rs, :], tb[:, 0:256], reads=[t_tb], writes=[K.t_VH[g]])
                tfD.append((tf, t_tf))
            for i in range(4):
                rs = slice(g * 512 + i * 128, g * 512 + (i + 1) * 128)
                tf, t_tf = tfD[i]
                fw.op("act", lambda: nc.scalar.activation(out=tf[:, 0:256], in_=tf[:, 0:256], func=AF.Ln), reads=[t_tf], writes=[t_tf])
                fw.dma("sp", K.LFH[rs, :], tf[:, 0:256], reads=[t_tf], writes=[K.t_LFH[g]])
                pt, t_pt = tok_mm(i, 1024, 8)
                g8, t_g8 = Rg8.next()
                fw.op("dve", lambda: nc.vector.tensor_tensor(out=g8[:], in0=pt[:, 0:8], in1=gb[:], op=ALU.add), reads=[t_pt, t_gb], writes=[t_g8])
                g4, t_g4 = Rg4.next()
                fw.op("act", lambda: nc.scalar.activation(out=g4[:], in_=g8[:, 4:8], func=AF.Exp, scale=-1.0), reads=[t_g8], writes=[t_g4])
                fw.op("act", lambda: nc.scalar.activation(out=g4[:], in_=g4[:], func=AF.Ln, bias=1.0), reads=[t_g4], writes=[t_g4])
                fw.op("dve", lambda: nc.vector.tensor_scalar(out=g8[:, 4:8], in0=g4[:], scalar1=-1.0, scalar2=None, op0=ALU.mult),
                      reads=[t_g4, t_g8], writes=[t_g8])
                fw.dma("sp", K.GM[rs, :], g8[:], reads=[t_g8], writes=[K.t_GM[g]])
                pt, t_pt = tok_mm(i, 2056, 512)
                tb, t_tb = Rtb.next()
                fw.op("act", lambda: nc.scalar.copy(out=tb[:], in_=pt[:]), reads=[t_pt], writes=[t_tb])
                fw.dma("sp", K.VD[rs, :], tb[:], reads=[t_tb], writes=[K.t_VD[g]])
            for i in range(4):
                rs = slice(g * 512 + i * 128, g * 512 + (i + 1) * 128)
                pt, t_pt = tok_mm(i, 3336, 256)
                tf, t_tf = Rtf.next()
                fw.op("act", lambda: nc.scalar.activation(out=tf[:, 0:256], in_=pt[:, 0:256], func=AF.Silu), reads=[t_pt], writes=[t_tf])
                fw.dma("sp", K.GH[rs, :], tf[:, 0:256], reads=[t_tf], writes=[K.t_GH[g]])


def bview(ap, shape):
    return ap.unsqueeze(2).to_broadcast(shape)


def mlstm_phase(K, l):
    nc, fw = K.nc, K.fw
    cf = K.constf
    with Phase(K) as ph:
        qT = ph.sb("mqT", [128, 2, S], BF16)
        kT = ph.sb("mkT", [128, 2, S], BF16)
        ktok = ph.sb("mktok", [128, NT, 256], BF16)
        vaug = ph.sb("mvaug", [128, NT, 4, 66], BF16)
        gm = ph.sb("mgm", [128, NT, 8], F32)
        t_in = [Trk() for _ in range(NG)]
        t_gm = Trk()
        fw.op("pool", lambda: nc.gpsimd.memset(vaug[:], 1.0), writes=t_in)
        for g_ in range(NG):
            fw.dma("sp", gm[:, g_ * 4:(g_ + 1) * 4, :], K.GM[g_ * 512:(g_ + 1) * 512, :].rearrange("(c p) g -> p c g", p=128), reads=[K.t_GM[g_]], writes=[t_gm])
        mnw, t_mnw = load_row(K, ph, "mnw", K.m_nw[l], 256)
        for g in range(NG):
            cs = slice(g * 512, (g + 1) * 512)
            ts = slice(g * 4, (g + 1) * 4)
            for p in range(2):
                fw.dma("sp", qT[:, p, cs], K.QKM[p * 128:(p + 1) * 128, cs], reads=[K.t_QKM[g]], writes=[t_in[g]])
                fw.dma("sp", kT[:, p, cs], K.QKM[256 + p * 128:256 + (p + 1) * 128, cs], reads=[K.t_QKM[g]], writes=[t_in[g]])
            fw.dma("sp", ktok[:, ts, :], K.KMT[cs, :].rearrange("(c p) f -> p c f", p=128), reads=[K.t_KMT[g]], writes=[t_in[g]])
            for c4 in range(4):
                cc_ = g * 4 + c4
                fw.dma("sp", vaug[:, cc_, :, 0:64], K.VM[cc_ * 128:(cc_ + 1) * 128, :].rearrange("p (h e) -> p h e", h=4), reads=[K.t_VM[g]], writes=[t_in[g]])
        lfc = ph.sb("lfc", [128, 128], F32)
        lic = ph.sb("lic", [128, 128], F32)
        eb = ph.sb("eb", [128, 128], F32)
        al = ph.sb("al", [128, 128], F32)
        wa = ph.sb("wa", [128, 128], F32)
        ebL = ph.sb("ebL", [128, 128], F32)
        ebL2 = ph.sb("ebL2", [128, NT, 2], F32)
        t_g = Trk()
        fw.op("dve", lambda: nc.vector.tensor_copy(out=lfc[:].rearrange("p (c h) -> p c h", h=4), in_=gm[:, :, 4:8]), reads=[t_gm], writes=[t_g])
        fw.op("dve", lambda: nc.vector.tensor_copy(out=lic[:].rearrange("p (c h) -> p c h", h=4), in_=gm[:, :, 0:4]), reads=[t_gm], writes=[t_g])
        Rpp = Rot(ph, "mpp", [128, 128], F32, 1, psum=True)
        pp_, t_pp_ = Rpp.next()
        fw.op("pe", lambda: nc.tensor.matmul(out=pp_[:], lhsT=cf[:, C_TRIT:C_TRIT + 128], rhs=lfc[:], start=True, stop=True), reads=[K.t_const, t_g], writes=[t_pp_])
        fw.op("act", lambda: nc.scalar.activation(out=eb[:], in_=pp_[:], func=AF.Exp), reads=[t_pp_], writes=[t_g])
        fw.op("dve", lambda: nc.vector.tensor_tensor(out=al[:], in0=lic[:], in1=pp_[:], op=ALU.subtract), reads=[t_pp_, t_g], writes=[t_g])
        fw.op("act", lambda: nc.scalar.activation(out=al[:], in_=al[:], func=AF.Exp), reads=[t_g], writes=[t_g])
        fw.op("pe", lambda: nc.tensor.matmul(out=pp_[:], lhsT=cf[:, C_TRIU:C_TRIU + 128], rhs=lfc[:], start=True, stop=True), reads=[K.t_const, t_g], writes=[t_pp_])
        fw.op("dve", lambda: nc.vector.tensor_tensor(out=wa[:], in0=lic[:], in1=pp_[:], op=ALU.add), reads=[t_pp_, t_g], writes=[t_g])
        fw.op("act", lambda: nc.scalar.activation(out=wa[:], in_=wa[:], func=AF.Exp), reads=[t_g], writes=[t_g])
        fw.op("pe", lambda: nc.tensor.matmul(out=pp_[:], lhsT=cf[:, C_ONES:C_ONES + 128], rhs=lfc[:], start=True, stop=True), reads=[K.t_const, t_g], writes=[t_pp_])
        fw.op("act", lambda: nc.scalar.activation(out=ebL[:], in_=pp_[:], func=AF.Exp), reads=[t_pp_], writes=[t_g])
        ebL4 = ebL[:].rearrange("p (c q h) -> p c q h", q=2, h=2)
        fw.op("dve", lambda: nc.vector.tensor_copy(out=ebL2[0:64], in_=ebL4[0:64, :, :, 0]), reads=[t_g], writes=[t_g])
        fw.op("dve", lambda: nc.vector.tensor_copy(out=ebL2[64:128], in_=ebL4[64:128, :, :, 1]), reads=[t_g], writes=[t_g])
        Cst = [ph.sb("mC", [128, 132], F32) for _ in range(2)]
        t_C = [Trk() for _ in range(2)]
        RCb = [Rot(ph, "mCb", [128, 132], BF16, 2) for _ in range(2)]
        cb = []
        for p in range(2):
            fw.op("dve", lambda: nc.vector.memset(Cst[p][:], 0.0), writes=[t_C[p]])
            b_, t_b = RCb[p].next()
            fw.op("dve", lambda: nc.vector.memset(b_[:], 0.0), writes=[t_b])
            cb.append((b_, t_b))
        Rps = Rot(ph, "mps", [128, 4, 128], F32, 2, psum=True)
        Rpn = Rot(ph, "mpn", [128, 4, 65], F32, 2, psum=True)
        Rpc = Rot(ph, "mpc", [128, 132], F32, 2, psum=True)
        RscT = Rot(ph, "mscT", [128, 4, 128], BF16, 2)
        Rs4 = Rot(ph, "ms4", [128, 4], F32, 8)
        Rhm = Rot(ph, "mhm", [128, 4, 64], F32, 2)
        Rsq = Rot(ph, "msq", [128, 4, 64], F32, 2)
        Rom = Rot(ph, "mom", [128, 256], F32, 2)
        Ryb = Rot(ph, "myb", [128, 256], BF16, 2)
        Rkw = Rot(ph, "mkw", [128, 256], BF16, 2)
        for c in range(NT):
            g = c // 4
            cs = slice(c * 128, (c + 1) * 128)
            om, t_om = Rom.next()
            fw.dma("sp", om[:], K.OM[cs, :], reads=[K.t_OM[g]], writes=[t_om])
            ps, t_ps = Rps.next()
            for h in range(4):
                p, hp = h // 2, h % 2
                rr = slice(hp * 64, (hp + 1) * 64)
                fw.op("pe", lambda: nc.tensor.matmul(out=ps[:, h, :], lhsT=kT[rr, p, cs], rhs=qT[rr, p, cs], start=True, stop=True),
                      reads=[t_in[g]], writes=[t_ps])
            scT, t_scT = RscT.next()
            for h in range(4):
                fw.op("dve", lambda: nc.vector.scalar_tensor_tensor(out=scT[:, h, :], in0=ps[:, h, :], scalar=al[:, c * 4 + h:c * 4 + h + 1],
                                                                    in1=cf[:, C_MASK:C_MASK + 128], op0=ALU.mult, op1=ALU.mult),
                      reads=[t_ps, t_g, K.t_const], writes=[t_scT])
            pn, t_pn = Rpn.next()
            for h in range(4):
                p, hp = h // 2, h % 2
                rr = slice(hp * 64, (hp + 1) * 64)
                fw.op("pe", lambda: nc.tensor.matmul(out=pn[:, h, :], lhsT=scT[:, h, :], rhs=vaug[:, c, h, 0:65], start=True, stop=False),
                      reads=[t_scT, t_in[g]], writes=[t_pn])
                fw.op("pe", lambda: nc.tensor.matmul(out=pn[:, h, :], lhsT=qT[rr, p, cs], rhs=cb[p][0][rr, hp * 66:hp * 66 + 65], start=False, stop=True),
                      reads=[t_in[g], cb[p][1]], writes=[t_pn])
            eb4 = eb[:, c * 4:(c + 1) * 4]
            d4, t_d4 = Rs4.next()
            fw.op("dve", lambda: nc.vector.tensor_tensor(out=d4[:], in0=pn[:, :, 64], in1=eb4, op=ALU.mult), reads=[t_pn, t_g], writes=[t_d4])
            n4, t_n4 = Rs4.next()
            fw.op("dve", lambda: nc.vector.tensor_scalar(out=n4[:], in0=d4[:], scalar1=-1.0, scalar2=None, op0=ALU.mult), reads=[t_d4], writes=[t_n4])
            fw.op("dve", lambda: nc.vector.scalar_tensor_tensor(out=d4[:], in0=d4[:], scalar=1.0, in1=n4[:], op0=ALU.max, op1=ALU.max), reads=[t_d4, t_n4], writes=[t_d4])
            fw.op("dve", lambda: nc.vector.reciprocal(out=d4[:], in_=d4[:]), reads=[t_d4], writes=[t_d4])
            fw.op("dve", lambda: nc.vector.tensor_tensor(out=d4[:], in0=d4[:], in1=eb4, op=ALU.mult), reads=[t_d4, t_g], writes=[t_d4])
            hm, t_hm = Rhm.next()
            fw.op("dve", lambda: nc.vector.tensor_tensor(out=hm[:], in0=pn[:, :, 0:64], in1=bview(d4[:], [128, 4, 64]), op=ALU.mult),
                  reads=[t_pn, t_d4], writes=[t_hm])
            m4, t_m4 = Rs4.next()
            fw.op("dve", lambda: nc.vector.tensor_reduce(out=m4[:], in_=hm[:], axis=AX.X, op=ALU.add), reads=[t_hm], writes=[t_m4])
            fw.op("dve", lambda: nc.vector.tensor_scalar(out=m4[:], in0=m4[:], scalar1=-1.0 / 64, scalar2=None, op0=ALU.mult), reads=[t_m4], writes=[t_m4])
            fw.op("dve", lambda: nc.vector.tensor_tensor(out=hm[:], in0=hm[:], in1=bview(m4[:], [128, 4, 64]), op=ALU.add), reads=[t_hm, t_m4], writes=[t_hm])
            sq, t_sq = Rsq.next()
            fw.op("pool", lambda: nc.gpsimd.tensor_tensor(out=sq[:], in0=hm[:], in1=hm[:], op=ALU.mult), reads=[t_hm], writes=[t_sq])
            v4, t_v4 = Rs4.next()
            fw.op("dve", lambda: nc.vector.tensor_reduce(out=v4[:], in_=sq[:], axis=AX.X, op=ALU.add), reads=[t_sq], writes=[t_v4])
            fw.op("act", lambda: nc.scalar.activation(out=v4[:], in_=v4[:], func=AF.Ln, scale=1.0 / 64, bias=EPS), reads=[t_v4], writes=[t_v4])
            fw.op("act", lambda: nc.scalar.activation(out=v4[:], in_=v4[:], func=AF.Exp, scale=-0.5), reads=[t_v4], writes=[t_v4])
            fw.op("dve", lambda: nc.vector.tensor_tensor(out=hm[:], in0=hm[:], in1=bview(v4[:], [128, 4, 64]), op=ALU.mult), reads=[t_hm, t_v4], writes=[t_hm])
            hm2 = hm[:].rearrange("p h e -> p (h e)")
            fw.op("pool", lambda: nc.gpsimd.tensor_tensor(out=hm2, in0=hm2, in1=mnw[:], op=ALU.mult), reads=[t_hm, t_mnw], writes=[t_hm])
            yb, t_yb = Ryb.next()
            fw.op("dve", lambda: nc.vector.tensor_tensor(out=yb[:], in0=hm2, in1=om[:], op=ALU.mult), reads=[t_hm, t_om], writes=[t_yb])
            fw.dma("sp", K.Y[cs, 0:256], yb[:], reads=[t_yb], writes=[K.t_Y[g]])
            if c + 1 < NT:
                kw, t_kw = Rkw.next()
                fw.op("dve", lambda: nc.vector.tensor_tensor(out=kw[:].rearrange("p (h e) -> p h e", h=4), in0=ktok[:, c, :].rearrange("p (h e) -> p h e", h=4),
                                                              in1=bview(wa[:, c * 4:(c + 1) * 4], [128, 4, 64]), op=ALU.mult),
                      reads=[t_in[g], t_g], writes=[t_kw])
                for p in range(2):
                    pc, t_pc = Rpc.next()
                    fw.op("pe", lambda: nc.tensor.matmul(out=pc[:], lhsT=kw[:, p * 128:(p + 1) * 128],
                                                         rhs=vaug[:, c, 2 * p:2 * p + 2, :].rearrange("p a b -> p (a b)"), start=True, stop=True),
                          reads=[t_kw, t_in[g]], writes=[t_pc])
                    fw.op("dve", lambda: nc.vector.scalar_tensor_tensor(out=Cst[p][:], in0=Cst[p][:], scalar=ebL2[:, c, p:p + 1], in1=pc[:],
                                                                        op0=ALU.mult, op1=ALU.add),
                          reads=[t_C[p], t_g, t_pc], writes=[t_C[p]])
                    b_, t_b = RCb[p].next()
                    fw.op("act", lambda: nc.scalar.copy(out=b_[:], in_=Cst[p][:]), reads=[t_C[p]], writes=[t_b])
                    cb[p] = (b_, t_b)


def hgrn_phase(K, l):
    nc, fw = K.nc, K.fw
    cf = K.constf
    with Phase(K) as ph:
        qT = ph.sb("hqT", [128, 2, S], BF16)
        kT = ph.sb("hkT", [128, 2, S], BF16)
        lf = ph.sb("hlf", [128, NT, 256], F32)
        kht = ph.sb("hkht", [128, NT, 256], F32)
        vh = ph.sb("hvh", [128, NT, 256], BF16)
        t_in = [Trk() for _ in range(NG)]
        hnw, t_hnw = load_row(K, ph, "hnw", K.h_nw[l], 256)
        for g in range(NG):
            cs = slice(g * 512, (g + 1) * 512)
            ts = slice(g * 4, (g + 1) * 4)
            for p in range(2):
                fw.dma("sp", qT[:, p, cs], K.QH[p * 128:(p + 1) * 128, cs], reads=[K.t_QH[g]], writes=[t_in[g]])
                fw.dma("sp", kT[:, p, cs], K.KH[p * 128:(p + 1) * 128, cs], reads=[K.t_KH[g]], writes=[t_in[g]])
            fw.dma("sp", lf[:, ts, :], K.LFH[cs, :].rearrange("(c p) f -> p c f", p=128), reads=[K.t_LFH[g]], writes=[t_in[g]])
            fw.dma("sp", kht[:, ts, :], K.KHT[cs, :].rearrange("(c p) f -> p c f", p=128), reads=[K.t_KHT[g]], writes=[t_in[g]])
            fw.dma("sp", vh[:, ts, :], K.VH[cs, :].rearrange("(c p) f -> p c f", p=128), reads=[K.t_VH[g]], writes=[t_in[g]])
        Sst = [ph.sb("hS", [128, 128], F32) for _ in range(2)]
        t_S = [Trk() for _ in range(2)]
        RSb = [Rot(ph, "hSb", [128, 128], BF16, 3) for _ in range(2)]
        sb_ = []
        for p in range(2):
            fw.op("dve", lambda: nc.vector.memset(Sst[p][:], 0.0), writes=[t_S[p]])
            b_, t_b = RSb[p].next()
            fw.op("dve", lambda: nc.vector.memset(b_[:], 0.0), writes=[t_b])
            sb_.append((b_, t_b))
        q01 = [[ph.sb("hq01", [128, 2, 128], BF16) for _ in range(2)] for _ in range(2)]
        t_q01 = [[Trk() for _ in range(2)] for _ in range(2)]
        for r in range(2):
            for j in range(2):
                fw.op("pool", lambda: nc.gpsimd.memset(q01[r][j][:], 0.0), writes=[t_q01[r][j]])
        Rpm = Rot(ph, "hpm", [128, 128], F32, 2, psum=True)
        Rpl = Rot(ph, "hpl", [128, 2], F32, 1, psum=True)
        Rprb = Rot(ph, "hprb", [128, 256], F32, 1, psum=True)
        Rpa = Rot(ph, "hpa", [128, 4, 128], F32, 1, psum=True)
        Rpo = Rot(ph, "hpo", [128, 4, 64], F32, 1, psum=True)
        Rpss = Rot(ph, "hpss", [128, 128], F32, 2, psum=True)
        RE = Rot(ph, "hE", [128, 128], F32, 4)
        Rqt = Rot(ph, "hqt", [128, 2, 128], BF16, 2)
        Rkt = Rot(ph, "hkt", [128, 2, 128], BF16, 2)
        Rebl = Rot(ph, "hebl", [128, 2, 2], F32, 2)
        Rwk = Rot(ph, "hwk", [128, 256], F32, 2)
        Rkp = Rot(ph, "hkp", [128, 256], BF16, 2)
        RAT = Rot(ph, "hAT", [128, 4, 128], BF16, 2)
        Ros = Rot(ph, "hos", [128, 4, 64], F32, 2)
        Rsq = Rot(ph, "hsq", [128, 4, 64], F32, 2)
        Rs4 = Rot(ph, "hs4", [128, 4], F32, 4)
        Rgh = Rot(ph, "hgh", [128, 256], F32, 2)
        Ryb = Rot(ph, "hyb", [128, 256], BF16, 2)
        for i in range(NT):
            g = i // 4
            cs = slice(i * 128, (i + 1) * 128)
            gh, t_gh = Rgh.next()
            fw.dma("sp", gh[:], K.GH[cs, :], reads=[K.t_GH[g]], writes=[t_gh])
            qt, t_qt = Rqt.next()
            kt, t_kt = Rkt.next()
            ebl, t_ebl = Rebl.next()
            q0, q1 = q01[i % 2]
            t_q0, t_q1 = t_q01[i % 2]
            for p in range(2):
                lfp = lf[:, i, p * 128:(p + 1) * 128]
                pm, t_pm = Rpm.next()
                fw.op("pe", lambda: nc.tensor.matmul(out=pm[:], lhsT=lfp, rhs=cf[:, C_TRIMID:C_TRIMID + 128], start=True, stop=True),
                      reads=[t_in[g], K.t_const], writes=[t_pm])
                E1, t_E1 = RE.next()
                E2, t_E2 = RE.next()
                fw.op("act", lambda: nc.scalar.activation(out=E1[:], in_=pm[:], func=AF.Exp), reads=[t_pm], writes=[t_E1])
                fw.op("act", lambda: nc.scalar.activation(out=E2[:], in_=pm[:], func=AF.Exp, scale=-1.0), reads=[t_pm], writes=[t_E2])
                fw.op("dve", lambda: nc.vector.tensor_tensor(out=qt[:, p, :], in0=qT[:, p, cs], in1=E1[:], op=ALU.mult), reads=[t_in[g], t_E1], writes=[t_qt])
                fw.op("pool", lambda: nc.gpsimd.tensor_tensor(out=kt[:, p, :], in0=kT[:, p, cs], in1=E2[:], op=ALU.mult), reads=[t_in[g], t_E2], writes=[t_kt])
                pb, t_pb = Rpm.next()
                fw.op("pe", lambda: nc.tensor.matmul(out=pb[:], lhsT=lfp, rhs=cf[:, C_TRITBD:C_TRITBD + 128], start=True, stop=True),
                      reads=[t_in[g], K.t_const], writes=[t_pb])
                E3, t_E3 = RE.next()
                fw.op("act", lambda: nc.scalar.activation(out=E3[:], in_=pb[:], func=AF.Exp), reads=[t_pb], writes=[t_E3])
                fw.op("dve", lambda: nc.vector.tensor_tensor(out=q0[:, p, 0:64], in0=qT[:, p, i * 128:i * 128 + 64], in1=E3[:, 0:64], op=ALU.mult),
                      reads=[t_in[g], t_E3], writes=[t_q0])
                fw.op("dve", lambda: nc.vector.tensor_tensor(out=q1[:, p, 64:128], in0=qT[:, p, i * 128 + 64:(i + 1) * 128], in1=E3[:, 64:128], op=ALU.mult),
                      reads=[t_in[g], t_E3], writes=[t_q1])
                pl, t_pl = Rpl.next()
                fw.op("pe", lambda: nc.tensor.matmul(out=pl[:], lhsT=lfp, rhs=cf[:, C_CHI:C_CHI + 2], start=True, stop=True),
                      reads=[t_in[g], K.t_const], writes=[t_pl])
                fw.op("act", lambda: nc.scalar.activation(out=ebl[:, p, :], in_=pl[:], func=AF.Exp), reads=[t_pl], writes=[t_ebl])
            prb, t_prb = Rprb.next()
            fw.op("pe", lambda: nc.tensor.matmul(out=prb[:], lhsT=cf[:, C_TRIUBD:C_TRIUBD + 128], rhs=lf[:, i, :], start=True, stop=True),
                  reads=[t_in[g], K.t_const], writes=[t_prb])
            wk, t_wk = Rwk.next()
            fw.op("act", lambda: nc.scalar.activation(out=wk[:], in_=prb[:], func=AF.Exp), reads=[t_prb], writes=[t_wk])
            kp, t_kp = Rkp.next()
            fw.op("pool", lambda: nc.gpsimd.tensor_tensor(out=kp[:], in0=kht[:, i, :], in1=wk[:], op=ALU.mult), reads=[t_in[g], t_wk], writes=[t_kp])
            pa, t_pa = Rpa.next()
            for h in range(4):
                p, hp = h // 2, h % 2
                rr = slice(hp * 64, (hp + 1) * 64)
                fw.op("pe", lambda: nc.tensor.matmul(out=pa[:, h, :], lhsT=kt[rr, p, :], rhs=qt[rr, p, :], start=True, stop=True),
                      reads=[t_kt, t_qt], writes=[t_pa])
            AT, t_AT = RAT.next()
            fw.op("dve", lambda: nc.vector.tensor_tensor(out=AT[:], in0=pa[:], in1=cf[:, C_MASKBD:C_MASKBD + 128].unsqueeze(1).to_broadcast([128, 4, 128]), op=ALU.mult),
                  reads=[t_pa, K.t_const], writes=[t_AT])

            def state_update(j):
                rj = slice(j * 64, (j + 1) * 64)
                for p in range(2):
                    pss, t_pss = Rpss.next()
                    fw.op("pe", lambda: nc.tensor.matmul(out=pss[:], lhsT=kp[rj, p * 128:(p + 1) * 128], rhs=vh[rj, i, p * 128:(p + 1) * 128], start=True, stop=True),
                          reads=[t_kp, t_in[g]], writes=[t_pss])
                    fw.op("dve", lambda: nc.vector.scalar_tensor_tensor(out=Sst[p][:], in0=Sst[p][:], scalar=ebl[:, p, j:j + 1], in1=pss[:],
                                                                        op0=ALU.mult, op1=ALU.add),
                          reads=[t_S[p], t_ebl, t_pss], writes=[t_S[p]])
                    b_, t_b = RSb[p].next()
                    fw.op("act", lambda: nc.scalar.copy(out=b_[:], in_=Sst[p][:]), reads=[t_S[p]], writes=[t_b])
                    yield (b_, t_b)
            s0 = list(sb_)
            s1 = list(state_update(0))
            po, t_po = Rpo.next()
            for h in range(4):
                p, hp = h // 2, h % 2
                rr = slice(hp * 64, (hp + 1) * 64)
                cc = slice(hp * 64, (hp + 1) * 64)
                fw.op("pe", lambda: nc.tensor.matmul(out=po[:, h, :], lhsT=AT[:, h, :], rhs=vh[:, i, h * 64:(h + 1) * 64], start=True, stop=False),
                      reads=[t_AT, t_in[g]], writes=[t_po])
                fw.op("pe", lambda: nc.tensor.matmul(out=po[:, h, :], lhsT=q0[rr, p, :], rhs=s0[p][0][rr, cc], start=False, stop=False),
                      reads=[t_q0, s0[p][1]], writes=[t_po])
                fw.op("pe", lambda: nc.tensor.matmul(out=po[:, h, :], lhsT=q1[rr, p, :], rhs=s1[p][0][rr, cc], start=False, stop=True),
                      reads=[t_q1, s1[p][1]], writes=[t_po])
            if i + 1 < NT:
                sb_ = list(state_update(1))
            os_, t_os = Ros.next()
            fw.op("act", lambda: nc.scalar.copy(out=os_[:], in_=po[:]), reads=[t_po], writes=[t_os])
            sq, t_sq = Rsq.next()
            fw.op("pool", lambda: nc.gpsimd.tensor_tensor(out=sq[:], in0=os_[:], in1=os_[:], op=ALU.mult), reads=[t_os], writes=[t_sq])
            v4, t_v4 = Rs4.next()
            fw.op("dve", lambda: nc.vector.tensor_reduce(out=v4[:], in_=sq[:], axis=AX.X, op=ALU.add), reads=[t_sq], writes=[t_v4])
            fw.op("act", lambda: nc.scalar.activation(out=v4[:], in_=v4[:], func=AF.Ln, scale=1.0 / 64, bias=EPS), reads=[t_v4], writes=[t_v4])
            fw.op("act", lambda: nc.scalar.activation(out=v4[:], in_=v4[:], func=AF.Exp, scale=-0.5), reads=[t_v4], writes=[t_v4])
            fw.op("dve", lambda: nc.vector.tensor_tensor(out=os_[:], in0=os_[:], in1=bview(v4[:], [128, 4, 64]), op=ALU.mult), reads=[t_os, t_v4], writes=[t_os])
            o2 = os_[:].rearrange("p h e -> p (h e)")
            fw.op("pool", lambda: nc.gpsimd.tensor_tensor(out=o2, in0=o2, in1=hnw[:], op=ALU.mult), reads=[t_os, t_hnw], writes=[t_os])
            yb, t_yb = Ryb.next()
            fw.op("dve", lambda: nc.vector.tensor_tensor(out=yb[:], in0=o2, in1=gh[:], op=ALU.mult), reads=[t_os, t_gh], writes=[t_yb])
            fw.dma("sp", K.Y[cs, 768:1024], yb[:], reads=[t_yb], writes=[K.t_Y[g]])


def attn_phase(K, l):
    nc, fw = K.nc, K.fw
    cf = K.constf
    lam_init = 0.8 - 0.6 * math.exp(-0.3 * l)
    with Phase(K) as ph:
        lv, t_lv = load_row(K, ph, "lv", K.dlam[l], 256)
        dnw, t_dnw = load_row(K, ph, "dnw", K.d_nw[l], 512)
        rb, t_rb = load_row(K, ph, "rb", K.rel_bias, 128)
        j64 = ph.sb("j64", [128, 2, 64], F32)
        s12 = ph.sb("s12", [128, 2], F32)
        nlam = ph.sb("nlam", [128, 1], F32)
        t_lam = Trk()
        lv4 = lv[:].rearrange("p (a b) -> p a b", a=4)
        fw.op("dve", lambda: nc.vector.tensor_tensor(out=j64[:, 0, :], in0=lv4[:, 0, :], in1=lv4[:, 1, :], op=ALU.mult), reads=[t_lv], writes=[t_lam])
        fw.op("dve", lambda: nc.vector.tensor_tensor(out=j64[:, 1, :], in0=lv4[:, 2, :], in1=lv4[:, 3, :], op=ALU.mult), reads=[t_lv, t_lam], writes=[t_lam])
        fw.op("dve", lambda: nc.vector.tensor_reduce(out=s12[:], in_=j64[:], axis=AX.X, op=ALU.add), reads=[t_lam], writes=[t_lam])
        fw.op("act", lambda: nc.scalar.activation(out=s12[:], in_=s12[:], func=AF.Exp), reads=[t_lam], writes=[t_lam])
        fw.op("dve", lambda: nc.vector.tensor_tensor(out=nlam[:], in0=s12[:, 1:2], in1=s12[:, 0:1], op=ALU.subtract), reads=[t_lam], writes=[t_lam])
        fw.op("dve", lambda: nc.vector.tensor_scalar(out=nlam[:], in0=nlam[:], scalar1=-lam_init, scalar2=None, op0=ALU.add), reads=[t_lam], writes=[t_lam])
        fw.op("dve", lambda: nc.vector.tensor_scalar(out=dnw[:], in0=dnw[:], scalar1=1.0 - lam_init, scalar2=None, op0=ALU.mult), reads=[t_dnw], writes=[t_dnw])
        et = ph.sb("et", [128, 128], F32)
        t_et = Trk()
        fw.op("act", lambda: nc.scalar.activation(out=et[:], in_=rb[:], func=AF.Exp), reads=[t_rb], writes=[t_et])
        EE = ph.sb("EE", [128, 4, 768], F32)
        t_EE = Trk()
        etmp = ph.sb("etmp", [128, 768], F32)
        t_etmp = Trk()
        fw.op("dve", lambda: nc.vector.memset(EE[:], 0.0), writes=[t_EE])
        for b in range(32):
            w = 768 if b == 31 else 256
            for h in range(4):
                fw.op("dve", lambda: nc.vector.tensor_scalar(out=etmp[:, 0:w], in0=cf[:, C_EEI:C_EEI + w], scalar1=float(b), scalar2=et[:, b * 4 + h:b * 4 + h + 1],
                                                             op0=ALU.is_equal, op1=ALU.mult),
                      reads=[K.t_const, t_et, t_etmp], writes=[t_etmp])
                fw.op("dve", lambda: nc.vector.tensor_tensor(out=EE[:, h, 0:w], in0=EE[:, h, 0:w], in1=etmp[:, 0:w], op=ALU.add),
                      reads=[t_etmp, t_EE], writes=[t_EE])
        sets = []
        for r in range(2):
            QT = ph.sb("aQT", [128, S], BF16)
            KT = ph.sb("aKT", [128, S], BF16)
            V = ph.sb("aV", [128, NT, 130], BF16)
            tr = Trk()
            fw.op("pool", lambda: nc.gpsimd.memset(V[:], 1.0), writes=[tr])
            sets.append((QT, KT, V, tr))

        def load_head(h):
            QT, KT, V, tr = sets[h % 2]
            for g in range(NG):
                cs = slice(g * 512, (g + 1) * 512)
                fw.dma("sp", QT[:, cs], K.QD[h * 128:(h + 1) * 128, cs], reads=[K.t_QD[g]], writes=[tr])
                fw.dma("sp", KT[:, cs], K.KD[h * 128:(h + 1) * 128, cs], reads=[K.t_KD[g]], writes=[tr])
                fw.dma("sp", V[:, g * 4:(g + 1) * 4, 0:128], K.VD[cs, h * 128:(h + 1) * 128].rearrange("(c p) e -> p c e", p=128),
                       reads=[K.t_VD[g]], writes=[tr])
        Rs1 = Rot(ph, "as1", [128, 512], F32, 2, psum=True)
        Rs2 = Rot(ph, "as2", [128, 512], F32, 2, psum=True)
        PO = [(ph.ps("aPO", [128, 3, 129], F32), Trk(True)) for _ in range(3)]
        Rp1 = Rot(ph, "ap1", [128, 512], BF16, 3)
        Rp2 = Rot(ph, "ap2", [128, 512], BF16, 3)
        Re = Rot(ph, "ae", [128, 512], F32, 3)
        Rr = Rot(ph, "ar", [128, 2], F32, 4)
        Rt1 = Rot(ph, "at1", [128, 128], F32, 2)
        Rod = Rot(ph, "aod", [128, 128], F32, 2)
        Rsq = Rot(ph, "asq", [128, 128], F32, 2)
        Rss = Rot(ph, "ass", [128, 1], F32, 4)
        Ryb = Rot(ph, "ayb", [128, 128], BF16, 3)

        def acc(j, which):
            idx = which * 4 + j
            po, tp = PO[idx // 3]
            return po[:, idx % 3, :], tp

        load_head(0)
        for h in range(4):
            if h + 1 < 4:
                load_head(h + 1)
            QT, KT, V, t_hd = sets[h % 2]
            tiles = [(Q, kb) for Q in range(8) for kb in range(4 * Q + 4)]

            def S_step(Q, kb):
                j0 = kb - 4 * Q
                jlo = max(j0, 0)
                c0 = jlo * 128
                ks = slice(kb * 128, (kb + 1) * 128)
                qs = slice(Q * 512 + c0, (Q + 1) * 512)
                s1, t_s1 = Rs1.next()
                s2, t_s2 = Rs2.next()
                fw.op("pe", lambda: nc.tensor.matmul(out=s1[:, c0:512], lhsT=KT[0:64, ks], rhs=QT[0:64, qs], start=True, stop=True),
                      reads=[t_hd], writes=[t_s1])
                fw.op("pe", lambda: nc.tensor.matmul(out=s2[:, c0:512], lhsT=KT[64:128, ks], rhs=QT[64:128, qs], start=True, stop=True),
                      reads=[t_hd], writes=[t_s2])
                P1, t_P1 = Rp1.next()
                P2, t_P2 = Rp2.next()
                for (sx, t_sx, Px, t_Px, eng) in ((s1, t_s1, P1, t_P1, "dve"), (s2, t_s2, P2, t_P2, "pool")):
                    if j0 <= -2:
                        fw.op("act", lambda: nc.scalar.activation(out=Px[:], in_=sx[:], func=AF.Exp, scale=0.125, bias=rb[:, 124 + h:125 + h]),
                              reads=[t_sx, t_rb], writes=[t_Px])
                    else:
                        e, t_e = Re.next()
                        eoff = (jlo - j0) * 128
                        ncol = 512 - c0
                        fw.op("act", lambda: nc.scalar.activation(out=e[:, c0:512], in_=sx[:, c0:512], func=AF.Exp, scale=0.125), reads=[t_sx], writes=[t_e])
                        if eng == "dve":
                            fw.op("dve", lambda: nc.vector.tensor_tensor(out=Px[:, c0:512], in0=e[:, c0:512], in1=EE[:, h, eoff:eoff + ncol], op=ALU.mult),
                                  reads=[t_e, t_EE], writes=[t_Px])
                        else:
                            fw.op("pool", lambda: nc.gpsimd.tensor_tensor(out=Px[:, c0:512], in0=e[:, c0:512], in1=EE[:, h, eoff:eoff + ncol], op=ALU.mult),
                                  reads=[t_e, t_EE], writes=[t_Px])
                return (Q, kb, jlo, P1, t_P1, P2, t_P2)

            def A_step(info):
                Q, kb, jlo, P1, t_P1, P2, t_P2 = info
                if kb == 0:
                    for po, tp in PO:
                        fw.op("dve", lambda: nc.vector.memset(po[:], 0.0), writes=[tp])
                for j in range(jlo, 4):
                    for which, (Px, t_Px) in enumerate(((P1, t_P1), (P2, t_P2))):
                        o, t_o = acc(j, which)
                        fw.op("pe", lambda: nc.tensor.matmul(out=o, lhsT=Px[:, j * 128:(j + 1) * 128], rhs=V[:, kb, 0:129], start=False, stop=False,
                                                             skip_group_check=True),
                              reads=[t_Px, t_hd], writes=[t_o], inc=(j == 3 and which == 1))

            def F_step(Q):
                for j in range(4):
                    i = 4 * Q + j
                    o1, t_o1 = acc(j, 0)
                    o2, t_o2 = acc(j, 1)
                    r, t_r = Rr.next()
                    fw.op("dve", lambda: nc.vector.reciprocal(out=r[:, 0:1], in_=o1[:, 128:129]), reads=[t_o1], writes=[t_r])
                    fw.op("dve", lambda: nc.vector.reciprocal(out=r[:, 1:2], in_=o2[:, 128:129]), reads=[t_o2, t_r], writes=[t_r])
                    fw.op("dve", lambda: nc.vector.tensor_tensor(out=r[:, 1:2], in0=r[:, 1:2], in1=nlam[:], op=ALU.mult), reads=[t_r, t_lam], writes=[t_r])
                    t1, t_t1 = Rt1.next()
                    fw.op("dve", lambda: nc.vector.tensor_scalar(out=t1[:], in0=o1[:, 0:128], scalar1=r[:, 0:1], scalar2=None, op0=ALU.mult),
                          reads=[t_o1, t_r], writes=[t_t1])
                    od, t_od = Rod.next()
                    fw.op("dve", lambda: nc.vector.scalar_tensor_tensor(out=od[:], in0=o2[:, 0:128], scalar=r[:, 1:2], in1=t1[:], op0=ALU.mult, op1=ALU.add),
                          reads=[t_o2, t_r, t_t1], writes=[t_od])
                    sq, t_sq = Rsq.next()
                    fw.op("pool", lambda: nc.gpsimd.tensor_tensor(out=sq[:], in0=od[:], in1=od[:], op=ALU.mult), reads=[t_od], writes=[t_sq])
                    ss, t_ss = Rss.next()
                    fw.op("dve", lambda: nc.vector.tensor_reduce(out=ss[:], in_=sq[:], axis=AX.X, op=ALU.add), reads=[t_sq], writes=[t_ss])
                    fw.op("act", lambda: nc.scalar.activation(out=ss[:], in_=ss[:], func=AF.Ln, scale=1.0 / 128, bias=EPS), reads=[t_ss], writes=[t_ss])
                    fw.op("act", lambda: nc.scalar.activation(out=ss[:], in_=ss[:], func=AF.Exp, scale=-0.5), reads=[t_ss], writes=[t_ss])
                    yb, t_yb = Ryb.next()
                    fw.op("dve", lambda: nc.vector.scalar_tensor_tensor(out=yb[:], in0=od[:], scalar=ss[:], in1=dnw[:, h * 128:(h + 1) * 128], op0=ALU.mult, op1=ALU.mult),
                          reads=[t_od, t_ss, t_dnw], writes=[t_yb])
                    fw.dma("sp", K.Y[i * 128:(i + 1) * 128, 256 + h * 128:256 + (h + 1) * 128], yb[:], reads=[t_yb], writes=[K.t_Y[i // 4]])

            prev = None
            for (Q, kb) in tiles:
                info = S_step(Q, kb)
                if prev is not None:
                    A_step(prev)
                    if prev[1] == 4 * prev[0] + 3:
                        F_step(prev[0])
                prev = info
            A_step(prev)
            F_step(prev[0])


KCtx.attn_phase = staticmethod(attn_phase)


def mixers(K, l, stop_after):
    MX = os.environ.get("MIXERS", "MHD")
    K.fw.pe_sync = True
    if "M" in MX:
        mlstm_phase(K, l)
    if "H" in MX:
        hgrn_phase(K, l)
    K.fw.pe_sync = False
    if "D" in MX and hasattr(K, "attn_phase"):
        K.attn_phase(K, l)


KCtx.mixers = staticmethod(mixers)


def build(n_layers=DEPTH, debug=False, stop_after=None):
    nc = bass.Bass("TRN2", target_bir_lowering=False)
    K = KCtx()
    K.nc = nc
    dt = nc.dram_tensor
    K.x = dt("x", [S, D], F32, kind="ExternalInput").ap()
    K.norm_w = dt("norm_w", [DEPTH, 6, D], F32, kind="ExternalInput").ap()
    K.ffn1_wi = dt("ffn1_wi", [DEPTH, D, 2 * DFF], F32, kind="ExternalInput").ap()
    K.ffn1_wo = dt("ffn1_wo", [DEPTH, DFF, D], F32, kind="ExternalInput").ap()
    K.ffn2_wi = dt("ffn2_wi", [DEPTH, D, 2 * DFF], F32, kind="ExternalInput").ap()
    K.ffn2_wo = dt("ffn2_wo", [DEPTH, DFF, D], F32, kind="ExternalInput").ap()
    K.w_in = dt("w_in", [DEPTH, D, NIN], F32, kind="ExternalInput").ap()
    K.w_out = dt("w_out", [DEPTH, D, D], F32, kind="ExternalInput").ap()
    K.consts = dt("consts", [128, NCONST], F32, kind="ExternalInput").ap()
    K.pp = dt("pp", [DEPTH, 128, 28], F32, kind="ExternalInput").ap()
    K.ig_b = dt("mlstm_igate_b", [DEPTH, 4], F32, kind="ExternalInput").ap()
    K.fg_b = dt("mlstm_fgate_b", [DEPTH, 4], F32, kind="ExternalInput").ap()
    K.m_nw = dt("mlstm_norm_w", [DEPTH, 256], F32, kind="ExternalInput").ap()
    K.dlam = dt("diff_lambda", [DEPTH, 256], F32, kind="ExternalInput").ap()
    K.d_nw = dt("diff_norm_w", [DEPTH, 512], F32, kind="ExternalInput").ap()
    K.rel_bias = dt("rel_bias", [128], F32, kind="ExternalInput").ap()
    K.lb_logits = dt("hgrn_lb_logits", [DEPTH, 256], F32, kind="ExternalInput").ap()
    K.h_nw = dt("hgrn_norm_w", [DEPTH, 256], F32, kind="ExternalInput").ap()
    K.out = dt("out", [S, D], F32, kind="ExternalOutput").ap()
    dbg = set(debug) if debug else set()

    def scratch(name, shape, dtp):
        kind = "ExternalOutput" if name in dbg else "Internal"
        ap = dt(name, shape, dtp, kind=kind).ap()
        return ap, [Trk() for _ in range(NG)]
    K.X, K.t_X = scratch("Xs", [S, D], F32)
    K.Y, K.t_Y = scratch("Ys", [S, D], BF16)
    K.QKM, K.t_QKM = scratch("QKM", [512, S], BF16)
    K.KMT, K.t_KMT = scratch("KMT", [S, 256], BF16)
    K.VM, K.t_VM = scratch("VM", [S, 256], BF16)
    K.OM, K.t_OM = scratch("OM", [S, 256], F32)
    K.GM, K.t_GM = scratch("GM", [S, 8], F32)
    K.QD, K.t_QD = scratch("QD", [512, S], BF16)
    K.KD, K.t_KD = scratch("KD", [512, S], BF16)
    K.VD, K.t_VD = scratch("VD", [S, 512], BF16)
    K.QH, K.t_QH = scratch("QH", [256, S], BF16)
    K.KH, K.t_KH = scratch("KH", [256, S], BF16)
    K.LFH, K.t_LFH = scratch("LFH", [S, 256], F32)
    K.KHT, K.t_KHT = scratch("KHT", [S, 256], F32)
    K.VH, K.t_VH = scratch("VH", [S, 256], BF16)
    K.GH, K.t_GH = scratch("GH", [S, 256], F32)
    K.t_x = [Trk() for _ in range(NG)]
    K.t_out = [Trk() for _ in range(NG)]
    K.WIB = dt("WIB", [2, 11, 128, 4096], BF16, kind="Internal").ap()
    K.t_WIB = [[Trk() for _ in range(11)] for _ in range(2)]
    K.wib_ready = {}
    K.n_layers = n_layers
    with ExitStack() as st:
        fw = FW(nc, st)
        K.fw = fw
        K.constf = st.enter_context(nc.sbuf_tensor("constf", [128, NCONST], F32))
        K.identb = st.enter_context(nc.sbuf_tensor("identb", [128, 128], BF16))
        K.t_const = Trk()
        fw.dma("sp", K.constf[:], K.consts, writes=[K.t_const])
        fw.op("dve", lambda: nc.vector.tensor_copy(out=K.identb[:], in_=K.constf[:, C_IDENT:C_IDENT + 128]),
              reads=[K.t_const], writes=[K.t_const])
        for l in range(n_layers):
            xin, t_xin = (K.x, K.t_x) if l == 0 else (K.X, K.t_X)
            if os.environ.get('SIM_PROJ_ONLY'):
                proj_phase(K, l, K.x, K.t_x)
                if stop_after[1] >= 3:
                    K.mixers(K, l, stop_after)
                break
            token_phase(K, l, 1, xin, t_xin, K.X, K.t_X)
            if stop_after == (l, 1):
                break
            proj_phase(K, l)
            if stop_after == (l, 2):
                break
            if hasattr(K, "mixers"):
                K.mixers(K, l, stop_after)
            if stop_after is not None and stop_after[0] == l and stop_after[1] == 3:
                break
            last = (l == n_layers - 1)
            token_phase(K, l, 2, K.X, K.t_X, K.out if last else K.X, K.t_out if last else K.t_X)
            if stop_after == (l, 4):
                break
        fw.barrier()
    K.counts = {k: v.count for k, v in fw.engs.items()}
    print("instr counts", K.counts, "dmas", fw.dma_count)
    return nc


def make_in_map(inputs, c, consts):
    f = lambda a: np.ascontiguousarray(a, dtype=np.float32)
    m = {"x": f(inputs["x"][c]), "consts": consts}
    for k in ["norm_w", "ffn1_wi", "ffn1_wo", "ffn2_wi", "ffn2_wo", "w_in", "w_out", "mlstm_igate_b", "mlstm_fgate_b",
              "mlstm_norm_w", "diff_norm_w", "hgrn_lb_logits", "hgrn_norm_w"]:
        m[k] = f(inputs[k])
    m["diff_lambda"] = f(inputs["diff_lambda"]).reshape(DEPTH, 256)
    m["rel_bias"] = f(inputs["rel_bias"]).reshape(128)
    pp = np.zeros((DEPTH, 128, 28), np.float32)
    cw = f(inputs["mlstm_conv_w"])
    cb = f(inputs["mlstm_conv_b"])
    lg = f(inputs["hgrn_lb_logits"])
    for l in range(DEPTH):
        pp[l, :, 0:16] = cw[l].reshape(4, 4, 128).transpose(2, 1, 0).reshape(128, 16)
        pp[l, :, 16:20] = cb[l].reshape(4, 128).T
        pp[l, :, 20:28] = lg.reshape(DEPTH, 2, 128).transpose(2, 1, 0).reshape(128, 8)
    m["pp"] = pp
    return m


_NC_CACHE = {}


def kernel(**inputs):
    nc = _NC_CACHE.get("nc")
    if nc is None:
        nc = build()
        _NC_CACHE["nc"] = nc
    consts = make_consts()
    in_maps = [make_in_map(inputs, c, consts) for c in range(8)]
    res = run_bass_kernel_spmd(nc, in_maps, core_ids=list(range(8)))
    return np.stack([res.results[c]["out"] for c in range(8)], axis=0)
```
